# Optimizing a Trainium2 kernel written in Bass

```python
import math
import jax, jax.numpy as jnp
from jax import lax
import numpy as np


D_MODEL = 1024
BATCH = 1
SEQ = 16384
DEPTH = 2

N_MIXERS = 2
N_A_LAYERS = (DEPTH + 1) // 2
N_B_LAYERS = DEPTH // 2
N_SUBLAYERS = 3
RMS_EPS = 1e-6

D_FF = 2816

GM_D = 3 * D_MODEL
GM_GROUPS = 16
GM_GROUP_DIM = GM_D // GM_GROUPS
GM_CHUNK = 128

MB_HEADS = 16
MB_HEAD_DIM = D_MODEL // MB_HEADS
MB_BLOCK = 256
MB_TOPK = 3
MB_QBLOCK = 128
MB_SCALE = MB_HEAD_DIM ** -0.5

T5_NUM_BUCKETS = 32
T5_MAX_EXACT = T5_NUM_BUCKETS // 2
T5_MAX_DISTANCE = 128

NEG_INF = -1e30

kernel_name = "hybrid_gmlp_moba_macaron_adaln"


def rmsnorm(x, g):
    xf = x.astype(jnp.float32)
    y = xf * lax.rsqrt(jnp.mean(xf * xf, axis=-1, keepdims=True) + RMS_EPS)
    return (y * g.astype(jnp.float32)).astype(x.dtype)


def adaln(x, g, shift, scale):
    return rmsnorm(x, g) * (1 + scale[:, None, :]) + shift[:, None, :]


def swiglu(h, w_in, w_out):
    gate, up = jnp.split(h @ w_in, 2, axis=-1)
    return (jax.nn.silu(gate) * up) @ w_out


def t5_bucket(rel):
    n = jnp.maximum(rel, 0)
    is_small = n < T5_MAX_EXACT
    nf = jnp.maximum(n, T5_MAX_EXACT).astype(jnp.float32)
    large = T5_MAX_EXACT + (jnp.log(nf / T5_MAX_EXACT)
                            / math.log(T5_MAX_DISTANCE / T5_MAX_EXACT)
                            * (T5_NUM_BUCKETS - T5_MAX_EXACT)).astype(jnp.int32)
    large = jnp.minimum(large, T5_NUM_BUCKETS - 1)
    return jnp.where(is_small, n, large)


def gmlp_mixer(h, w_in, v_norm, w_s, b_s, w_out):
    B, S, _ = h.shape
    z = jax.nn.gelu(h @ w_in, approximate=False)
    u, v = jnp.split(z, 2, axis=-1)
    v = rmsnorm(v, v_norm).reshape(B, S // GM_CHUNK, GM_CHUNK, GM_GROUPS, GM_GROUP_DIM)
    causal = jnp.tril(jnp.ones((GM_CHUNK, GM_CHUNK), dtype=bool))
    w_sm = jnp.where(causal, w_s, 0)
    sv = jnp.einsum("gts,bcsgd->bctgd", w_sm, v) + b_s.T[:, :, None]
    return (u * sv.reshape(B, S, GM_D)) @ w_out


def moba_mixer(h, w_qkv, w_o, rel_bias):
    B, S, _ = h.shape
    qkv = (h @ w_qkv).reshape(B, S, 3, MB_HEADS, MB_HEAD_DIM)
    q = qkv[:, :, 0].transpose(0, 2, 1, 3)
    k = qkv[:, :, 1].transpose(0, 2, 1, 3)
    v = qkv[:, :, 2].transpose(0, 2, 1, 3)
    nb = -(-S // MB_BLOCK)
    pad = ((0, 0), (0, 0), (0, nb * MB_BLOCK - S), (0, 0))
    k = jnp.pad(k, pad)
    v = jnp.pad(v, pad)
    k_blocks = k.reshape(B, MB_HEADS, nb, MB_BLOCK, MB_HEAD_DIM)
    v_blocks = v.reshape(B, MB_HEADS, nb, MB_BLOCK, MB_HEAD_DIM)
    k_mean = jnp.mean(k_blocks.astype(jnp.float32), axis=3)
    topk = min(MB_TOPK, nb)
    b_ix = jnp.arange(B)[:, None, None, None]
    h_ix = jnp.arange(MB_HEADS)[None, :, None, None]
    blk_ar = jnp.arange(nb)
    slot_ar = jnp.arange(topk)
    key_ar = jnp.arange(MB_BLOCK)
    q_ar = jnp.arange(MB_QBLOCK)

    def one_query_block(qb):
        p0 = qb * MB_QBLOCK
        j = p0 // MB_BLOCK
        q_blk = lax.dynamic_slice_in_dim(q, p0, MB_QBLOCK, axis=2)
        q_pos = p0 + q_ar
        gate = jnp.einsum("bhqd,bhnd->bhqn", q_blk.astype(jnp.float32), k_mean)
        gate = jnp.where(blk_ar < j, gate, NEG_INF)
        _, idx = lax.top_k(gate, topk)
        k_sel = k_blocks[b_ix, h_ix, idx]
        v_sel = v_blocks[b_ix, h_ix, idx]
        logit_sel = jnp.einsum("bhqd,bhqrkd->bhqrk", q_blk, k_sel).astype(jnp.float32) * MB_SCALE
        k_pos_sel = idx[..., None] * MB_BLOCK + key_ar
        bucket_sel = t5_bucket(q_pos[:, None, None] - k_pos_sel)
        logit_sel = logit_sel + rel_bias[bucket_sel, h_ix[..., None]].astype(jnp.float32)
        logit_sel = jnp.where((slot_ar < j)[:, None], logit_sel, NEG_INF)
        k_own = lax.dynamic_slice_in_dim(k, j * MB_BLOCK, MB_BLOCK, axis=2)
        v_own = lax.dynamic_slice_in_dim(v, j * MB_BLOCK, MB_BLOCK, axis=2)
        logit_own = jnp.einsum("bhqd,bhkd->bhqk", q_blk, k_own).astype(jnp.float32) * MB_SCALE
        rel_own = q_pos[:, None] - (j * MB_BLOCK + key_ar)[None, :]
        logit_own = logit_own + rel_bias[t5_bucket(rel_own)].transpose(2, 0, 1).astype(jnp.float32)
        logit_own = jnp.where(rel_own >= 0, logit_own, NEG_INF)
        logits = jnp.concatenate(
            [logit_sel.reshape(B, MB_HEADS, MB_QBLOCK, topk * MB_BLOCK), logit_own], axis=-1)
        p = jax.nn.softmax(logits, axis=-1).astype(v.dtype)
        p_sel = p[..., : topk * MB_BLOCK].reshape(B, MB_HEADS, MB_QBLOCK, topk, MB_BLOCK)
        p_own = p[..., topk * MB_BLOCK:]
        return (jnp.einsum("bhqrk,bhqrkd->bhqd", p_sel, v_sel)
                + jnp.einsum("bhqk,bhkd->bhqd", p_own, v_own))

    o = lax.map(one_query_block, jnp.arange(S // MB_QBLOCK))
    o = o.transpose(1, 0, 3, 2, 4).reshape(B, S, MB_HEADS * MB_HEAD_DIM)
    return o @ w_o


def setup_inputs(seed: int = 0) -> dict:
    key = jax.random.key(seed)
    ks = jax.random.split(key, 16)
    f32 = jnp.float32

    def nrm(k, shape, scale):
        return jax.random.normal(k, shape, f32) * scale

    return {
        "x": nrm(ks[0], (BATCH, SEQ, D_MODEL), 1.0),
        "c": nrm(ks[1], (BATCH, D_MODEL), 1.0),
        "rel_bias": nrm(ks[2], (T5_NUM_BUCKETS, MB_HEADS), 0.5),
        "mod_w": nrm(ks[3], (DEPTH, D_MODEL, N_SUBLAYERS * 3 * D_MODEL), D_MODEL ** -0.5),
        "mod_b": nrm(ks[4], (DEPTH, N_SUBLAYERS * 3 * D_MODEL), 0.02),
        "norm_g": 1.0 + nrm(ks[5], (DEPTH, N_SUBLAYERS, D_MODEL), 0.02),
        "ffn_w_in": nrm(ks[6], (DEPTH, 2, D_MODEL, 2 * D_FF), D_MODEL ** -0.5),
        "ffn_w_out": nrm(ks[7], (DEPTH, 2, D_FF, D_MODEL), D_FF ** -0.5),
        "gmlp_w_in": nrm(ks[8], (N_A_LAYERS, D_MODEL, 2 * GM_D), D_MODEL ** -0.5),
        "gmlp_v_norm": 1.0 + nrm(ks[9], (N_A_LAYERS, GM_D), 0.02),
        "gmlp_w_s": nrm(ks[10], (N_A_LAYERS, GM_GROUPS, GM_CHUNK, GM_CHUNK), GM_CHUNK ** -0.5),
        "gmlp_b_s": 1.0 + nrm(ks[11], (N_A_LAYERS, GM_GROUPS, GM_CHUNK), 0.02),
        "gmlp_w_out": nrm(ks[12], (N_A_LAYERS, GM_D, D_MODEL), GM_D ** -0.5),
        "moba_w_qkv": nrm(ks[13], (N_B_LAYERS, D_MODEL, 3 * D_MODEL), D_MODEL ** -0.5),
        "moba_w_o": nrm(ks[14], (N_B_LAYERS, D_MODEL, D_MODEL), D_MODEL ** -0.5),
        "final_norm": 1.0 + nrm(ks[15], (D_MODEL,), 0.02),
    }


def reference(x, c, rel_bias, mod_w, mod_b, norm_g, ffn_w_in, ffn_w_out,
              gmlp_w_in, gmlp_v_norm, gmlp_w_s, gmlp_b_s, gmlp_w_out,
              moba_w_qkv, moba_w_o, final_norm):
    B = x.shape[0]
    c_act = jax.nn.silu(c)
    for i in range(DEPTH):
        mod = (c_act @ mod_w[i] + mod_b[i]).reshape(B, N_SUBLAYERS, 3, D_MODEL)
        h = adaln(x, norm_g[i, 0], mod[:, 0, 0], mod[:, 0, 1])
        x = x + 0.5 * mod[:, 0, 2][:, None, :] * swiglu(h, ffn_w_in[i, 0], ffn_w_out[i, 0])
        h = adaln(x, norm_g[i, 1], mod[:, 1, 0], mod[:, 1, 1])
        li = i // N_MIXERS
        if i % N_MIXERS == 0:
            y = gmlp_mixer(h, gmlp_w_in[li], gmlp_v_norm[li], gmlp_w_s[li], gmlp_b_s[li], gmlp_w_out[li])
        else:
            y = moba_mixer(h, moba_w_qkv[li], moba_w_o[li], rel_bias)
        x = x + mod[:, 1, 2][:, None, :] * y
        h = adaln(x, norm_g[i, 2], mod[:, 2, 0], mod[:, 2, 1])
        x = x + 0.5 * mod[:, 2, 2][:, None, :] * swiglu(h, ffn_w_in[i, 1], ffn_w_out[i, 1])
    return rmsnorm(x, final_norm)
```

```python
import math
import numpy as np
import ml_dtypes
import concourse.bass as bass
import concourse.mybir as mybir
from concourse.bass_utils import run_bass_kernel_spmd

F32 = mybir.dt.float32
BF16 = mybir.dt.bfloat16
AF = mybir.ActivationFunctionType
ALU = mybir.AluOpType
AX = mybir.AxisListType

import os
NOAG = bool(os.environ.get('NOAG'))
MOBA_FAKE = bool(os.environ.get('MOBA_FAKE'))
NCORES = 8
D = 1024
SEQ = 16384
TOK = SEQ // NCORES
NT = TOK // 128
DFF = 2816
NFF = DFF // 128
GMD = 3072
EPS = 1e-6
FFPARTS = [list(range(0, 6)), list(range(6, 12)), list(range(12, 17)), list(range(17, 22))]


def _NOP(eng):
    return eng.nop()


class Op:
    __slots__ = ("eng", "fn", "waits", "is_dma", "sem", "val", "needs_inc", "rank", "inc")


class Prog:
    ENGS = ("pe", "act", "dve", "pool", "sp")

    def __init__(self):
        self.ops = {e: [] for e in self.ENGS}
        self.last_w = {}
        self.readers = {}
        self.dma_cnt = {}

    def op(self, eng, fn, r=(), w=(), dma=None, inc=16):
        o = Op()
        o.eng = eng
        o.fn = fn
        o.is_dma = dma is not None
        o.needs_inc = False
        o.waits = []
        o.sem = None
        o.val = 0
        o.rank = 0
        o.inc = inc
        deps = {}
        for k in r:
            d = self.last_w.get(k)
            if d is not None:
                deps[id(d)] = (d, True)
        for k in w:
            d = self.last_w.get(k)
            if d is not None and id(d) not in deps:
                deps[id(d)] = (d, False)
            rd = self.readers.get(k)
            if rd:
                for x in rd[0].values():
                    if id(x) not in deps:
                        deps[id(x)] = (x, False)
                for x in rd[1]:
                    if id(x) not in deps:
                        deps[id(x)] = (x, False)
        for d, raw in deps.values():
            if d.is_dma:
                o.waits.append(("dma", d.sem, d.val))
            elif d.eng == eng:
                if eng != "pe":
                    d.needs_inc = True
                    o.waits.append(("eng", d))
            else:
                d.needs_inc = True
                o.waits.append(("eng", d))
        if dma is not None:
            self.dma_cnt[dma] = self.dma_cnt.get(dma, 0) + inc
            o.inc = inc
            o.sem = dma
            o.val = self.dma_cnt[dma]
        for k in w:
            self.last_w[k] = o
            self.readers[k] = [{}, []]
        for k in r:
            if k in w:
                continue
            rd = self.readers.setdefault(k, [{}, []])
            if o.is_dma:
                rd[1].append(o)
            else:
                rd[0][eng] = o
        self.ops[eng].append(o)
        return o

    def barrier(self):
        lasts = {}
        for e in self.ENGS:
            for o in reversed(self.ops[e]):
                if not o.is_dma and o.fn is not _NOP:
                    lasts[e] = o
                    break
        dmas = [o for e in self.ENGS for o in self.ops[e]
                if o.is_dma and o.inc != 1 and o.val == self.dma_cnt[o.sem]]
        for e in self.ENGS:
            o = Op()
            o.eng = e
            o.fn = _NOP
            o.is_dma = False
            o.needs_inc = False
            o.sem = None
            o.val = 0
            o.rank = 0
            o.inc = 16
            o.waits = []
            for f, d in lasts.items():
                if f != e:
                    d.needs_inc = True
                    o.waits.append(("eng", d))
            for d in dmas:
                o.waits.append(("dma", d.sem, d.val))
            self.ops[e].append(o)
        self.last_w = {k: o for k, o in self.last_w.items() if o.is_dma and o.inc == 1}
        self.readers = {}

    def emit(self, nc, block, stack):
        engsem = {e: stack.enter_context(nc.semaphore(f"s_{e}")) for e in self.ENGS}
        dmasem = {n: stack.enter_context(nc.semaphore(f"d_{n}")) for n in self.dma_cnt}
        for e in self.ENGS:
            c = 0
            for o in self.ops[e]:
                if (not o.is_dma) and o.needs_inc:
                    c += 1
                    o.rank = c
        prog = self

        def run(e, engobj):
            waited = {}
            for o in prog.ops[e]:
                red = {}
                for w in o.waits:
                    kk = ("d", w[1]) if w[0] == "dma" else ("e", w[1].eng)
                    vv = w[2] if w[0] == "dma" else w[1].rank
                    if kk not in red or vv > red[kk][0]:
                        red[kk] = (vv, w)
                for vv, w in red.values():
                    if w[0] == "dma":
                        key = ("d", w[1])
                        val = w[2]
                        sem = dmasem[w[1]]
                    else:
                        d = w[1]
                        key = ("e", d.eng)
                        val = d.rank
                        sem = engsem[d.eng]
                    if waited.get(key, 0) >= val:
                        continue
                    waited[key] = val
                    engobj.wait_ge(sem, val)
                ins = o.fn(engobj)
                if o.is_dma:
                    if o.inc == 1:
                        ins.then_inc(dmasem[o.sem])
                    else:
                        ins.then_inc(dmasem[o.sem], o.inc)
                elif o.needs_inc:
                    ins.then_inc(engsem[e], 1)

        @block.tensor
        def _(eng):
            run("pe", eng)

        @block.scalar
        def _(eng):
            run("act", eng)

        @block.vector
        def _(eng):
            run("dve", eng)

        @block.gpsimd
        def _(eng):
            run("pool", eng)

        @block.sync
        def _(eng):
            run("sp", eng)


class SBAlloc:
    def __init__(self, nc):
        self.nc = nc
        self.off = 16640
        self.n = 0
        self.peak = 0

    def alloc(self, name, shape, dtype):
        esz = 4 if dtype == F32 else 2
        size = esz
        for s in shape[1:]:
            size *= s
        off = (self.off + 63) // 64 * 64
        self.off = off + size
        self.peak = max(self.peak, self.off)
        self.n += 1
        assert self.off <= 229376, (name, self.off)
        return self.nc.alloc_sbuf_tensor_at(f"{name}_{self.n}", list(shape), dtype, offset=off)

    def mark(self):
        return self.off

    def release(self, m):
        self.off = m


def build_program(stop_after=None):
    nc = bass.Bass("TRN2", target_bir_lowering=False)
    P = Prog()
    sb = SBAlloc(nc)

    def K(name, *idx):
        return (name,) + idx

    def din(name, shape, dt=F32):
        return nc.dram_tensor(name, list(shape), dt, kind="ExternalInput").ap()

    x_in = din("x", [TOK, D])
    c_in = din("c", [8, 128])
    RG = [list(range(NCORES))]

    def gathered(name, R, C):
        if NOAG:
            return din(name, [R, C])
        sh = din(name, [R // NCORES, C])
        loc = nc.dram_tensor(name + "_loc", [R // NCORES, C], F32).ap()
        full = nc.dram_tensor(name + "_full", [R, C], F32, addr_space="Shared").ap()
        P.op("sp", lambda e: e.dma_start(out=loc, in_=sh), r=[], w=[K(name, "loc")], dma=name + "_l")
        P.op("pool", lambda e: e.collective_compute("AllGather", ALU.bypass, replica_groups=RG,
                                                    ins=[loc.opt()], outs=[full.opt()]),
             r=[K(name, "loc")], w=[K(name)], dma=name + "_g", inc=1)
        return full

    mod_b = din("mod_b", [2 * 72, 128])
    norm_g = din("norm_g", [48, 128])
    final_norm = din("final_norm", [1, D])
    mb_ind_in = din("mb_ind", [64, SEQ], BF16)
    mb_T_in = din("mb_T", [8, 128, 256])
    mb_cm_in = din("mb_cm", [2, 128, 256])
    mb_cmneg_in = din("mb_cmneg", [2, 128, 256])
    mb_rb31_in = din("mb_rb31", [128, 2])
    gm_ws_in = din("gmlp_w_s", [16, 128, 128])
    gm_bs_in = din("gmlp_b_s", [16, 128])
    gm_vn_in = din("gmlp_v_norm", [24, 128])
    gm_sel_in = din("gm_sel", [16, GMD])
    gm_mask_in = din("gm_mask", [128, 128])
    ident_in = din("ident", [128, 128])
    y_out = nc.dram_tensor("y", [TOK, D], F32, kind="ExternalOutput").ap()

    X = sb.alloc("X", [128, NT, D], F32)
    ident_f = sb.alloc("identf", [128, 128], F32)
    ident_b = sb.alloc("identb", [128, 128], BF16)
    ones_f = sb.alloc("onesf", [128, 128], F32)
    modT = sb.alloc("modT", [128, 144], F32)
    modbT = sb.alloc("modbT", [128, 144], F32)
    gT = sb.alloc("gT", [128, 48], F32)
    Acol = sb.alloc("Acol", [128, 48], F32)
    cT = sb.alloc("cT", [128, 8], F32)
    ssq = sb.alloc("ssq", [128, NT], F32)
    rstd = sb.alloc("rstd", [128, NT], F32)
    rtmp = sb.alloc("rtmp", [128, NT], F32)
    gate_b = sb.alloc("gateb", [128, D], F32)
    gdiag = sb.alloc("gdiag", [128, D], F32)
    rows = sb.alloc("rows", [128, 128], F32)
    junk = sb.alloc("junk", [128, D], BF16)

    ps = [nc.alloc_psum_tensor(f"ps{i}", [128, 512], F32) for i in range(6)]
    ps.append(ps[5])
    psTb = [nc.alloc_psum_tensor(f"psT{i}", [128, 1024], BF16) for i in range(2)]

    def dma(q, out, in_, r, w, sem):
        P.op(q, lambda e: e.dma_start(out=out, in_=in_), r=r, w=w, dma=sem)

    def transpose_small(rows_n, src_dram, dst, dst_key, tag):
        dma("sp", rows[0:rows_n, :], src_dram, [], [K("rows")], f"rows")
        P.op("pe", lambda e: e.transpose(out=ps[6][:, 0:rows_n], in_=rows[0:rows_n, :],
                                         identity=ident_f[0:rows_n, 0:rows_n]),
             r=[K("rows"), K("identf")], w=[K("ps", 5)])
        P.op("dve", lambda e: e.tensor_copy(out=dst, in_=ps[6][:, 0:rows_n]),
             r=[K("ps", 5)], w=[dst_key])

    dma("sp", ident_f[:, :], ident_in, [], [K("identf")], "c0")
    P.op("dve", lambda e: e.tensor_copy(out=ident_b[:, :], in_=ident_f[:, :]),
         r=[K("identf")], w=[K("identb")])
    P.op("pool", lambda e: e.memset(ones_f[:, :], 1.0), w=[K("onesf")])

    for t in range(NT):
        dma("sp" if t % 2 == 0 else "act", X[:, t, :], x_in[t * 128:(t + 1) * 128, :],
            [], [K("X", t)], f"x{t}")

    prep_mark = sb.mark()
    stF = [sb.alloc(f"stF{i}", [128, 6144], F32) for i in range(2)]
    stB = [sb.alloc(f"stB{i}", [128, 6144], BF16) for i in range(2)]
    prep_cnt = [0]

    def gathered_bf(name, R, C):
        if NOAG:
            return din(name, [R, C], BF16)
        sh = din(name, [R // NCORES, C])
        n = (R // NCORES) * C // 128
        loc = nc.dram_tensor(name + "_loc", [R // NCORES, C], BF16).ap()
        full = nc.dram_tensor(name + "_full", [R, C], BF16, addr_space="Shared").ap()
        i = prep_cnt[0] % 2
        prep_cnt[0] += 1
        shf = sh.rearrange("r c -> (r c)").rearrange("(p f) -> p f", p=128)
        locf = loc.rearrange("r c -> (r c)").rearrange("(p f) -> p f", p=128)
        dma("sp", stF[i][:, 0:n], shf, [], [K("stF", i)], f"stF{i}")
        a = (n * 9 // 20) // 64 * 64
        b = (n * 16 // 20) // 64 * 64
        P.op("act", lambda e: e.copy(out=stB[i][:, 0:a], in_=stF[i][:, 0:a]), r=[K("stF", i)], w=[K("stB", i, 0)])
        P.op("dve", lambda e: e.tensor_copy(out=stB[i][:, a:b], in_=stF[i][:, a:b]), r=[K("stF", i)], w=[K("stB", i, 1)])
        P.op("pool", lambda e: e.tensor_copy(out=stB[i][:, b:n], in_=stF[i][:, b:n]), r=[K("stF", i)], w=[K("stB", i, 2)])
        dma("act", locf, stB[i][:, 0:n], [K("stB", i, 0), K("stB", i, 1), K("stB", i, 2)], [K(name, "loc")], f"stB{i}")
        P.op("pool", lambda e: e.collective_compute("AllGather", ALU.bypass, replica_groups=RG,
                                                    ins=[loc.opt()], outs=[full.opt()]),
             r=[K(name, "loc")], w=[K(name)], dma=name + "_g", inc=1)
        return full

    dbg = stop_after[1] if (stop_after and stop_after[0] == "dbg") else 99
    mod_w = [None, None]
    ffn_w_in = {}
    ffn_w_out = {}
    mod_w[0] = gathered("mod_w0", D, 9216)
    ffn_w_in[(0, 0)] = gathered_bf("ffn_w_in00", D, 2 * DFF)
    ffn_w_out[(0, 0)] = gathered_bf("ffn_w_out00", DFF, D)
    gm_w_in = gathered_bf("gmlp_w_in", D, 2 * GMD)
    gm_w_out = gathered_bf("gmlp_w_out", GMD, D)
    ffn_w_in[(0, 1)] = gathered_bf("ffn_w_in01", D, 2 * DFF)
    ffn_w_out[(0, 1)] = gathered_bf("ffn_w_out01", DFF, D)
    mod_w[1] = gathered("mod_w1", D, 9216)
    ffn_w_in[(1, 0)] = gathered_bf("ffn_w_in10", D, 2 * DFF)
    ffn_w_out[(1, 0)] = gathered_bf("ffn_w_out10", DFF, D)
    mb_w_qkv = gathered_bf("moba_w_qkv", D, 3 * D)
    mb_w_o = gathered_bf("moba_w_o", D, D)
    ffn_w_in[(1, 1)] = gathered_bf("ffn_w_in11", D, 2 * DFF)
    ffn_w_out[(1, 1)] = gathered_bf("ffn_w_out11", DFF, D)
    P.barrier()
    sb.release(prep_mark)

    transpose_small(8, c_in, cT[:, :], K("cT"), "c")
    P.op("act", lambda e: e.activation(out=cT[:, :], in_=cT[:, :], func=AF.Silu),
         r=[K("cT")], w=[K("cT")])
    for half in range(2):
        transpose_small(72, mod_b[half * 72:(half + 1) * 72, :], modbT[:, half * 72:(half + 1) * 72],
                        K("modbT", half), "mb")
    transpose_small(48, norm_g, gT[:, :], K("gT"), "g")

    eps_t = sb.alloc("eps", [128, 1], F32)
    EPS_AP = eps_t[:, 0:1]
    P.op("pool", lambda e: e.memset(eps_t[:, :], EPS), w=[K("eps")])

    attn_mark = sb.mark()
    WST = [sb.alloc(f"wst{i}", [128, 8, 256], F32) for i in range(2)]
    cnt = {"wst": 0, "wp": 0, "sg": 0, "tmp": 0, "psA": 0, "psO": 0, "psT": 0, "wos": 0}

    def mod_piece(l, n):
        i = cnt["wst"] % 2
        cnt["wst"] += 1
        src = mod_w[l].rearrange("(k p) n -> p k n", p=128)[:, :, n * 256:(n + 1) * 256]
        dma("sp", WST[i][:, :, :], src, [K(f"mod_w{l}")], [K("wst", i, 0), K("wst", i, 1)], f"wst{i}a")
        for j in range(2):
            for k in range(8):
                P.op("pe", lambda e, k=k, j=j, i=i: e.matmul(
                    ps[6][:, j:j + 1], lhsT=WST[i][:, k, j * 128:(j + 1) * 128], rhs=cT[:, k:k + 1],
                    start=(k == 0), stop=(k == 7)),
                     r=[K("wst", i, j), K("cT")], w=[K("ps", 5)])
        col0 = l * 72 + 2 * n
        P.op("dve", lambda e: e.tensor_tensor(out=modT[:, col0:col0 + 2], in0=ps[6][:, 0:2],
                                              in1=modbT[:, col0:col0 + 2], op=ALU.add),
             r=[K("ps", 5), K("modbT", l)], w=[K("modT", col0), K("modT", col0 + 1)])

    def mod_sublayer(l, s):
        for n in range(12):
            mod_piece(l, s * 12 + n)
        base = l * 72 + s * 24
        a0 = (l * 3 + s) * 8
        P.op("dve", lambda e: e.scalar_tensor_tensor(
            out=Acol[:, a0:a0 + 8], in0=modT[:, base + 8:base + 16], scalar=1.0,
            in1=gT[:, a0:a0 + 8], op0=ALU.add, op1=ALU.mult),
             r=[K("modT", base + 8 + i) for i in range(8)] + [K("gT")], w=[K("Acol", l, s)])

    def gate_bcast(l, s, mul):
        base = l * 72 + s * 24 + 16
        for c in range(8):
            P.op("dve", lambda e, c=c: e.tensor_scalar(
                out=gdiag[:, c * 128:(c + 1) * 128], in0=ident_f[:, :], scalar1=modT[:, base + c:base + c + 1],
                scalar2=float(mul), op0=ALU.mult, op1=ALU.mult),
                 r=[K("identf"), K("modT", base + c)], w=[K("gdiag", c)])
        for h in range(2):
            P.op("pe", lambda e, h=h: e.matmul(ps[6][:, :], lhsT=ones_f[:, :], rhs=gdiag[:, h * 512:(h + 1) * 512],
                                               start=True, stop=True),
                 r=[K("onesf")] + [K("gdiag", c) for c in range(h * 4, h * 4 + 4)], w=[K("ps", 5)])
            P.op("act", lambda e, h=h: e.copy(out=gate_b[:, h * 512:(h + 1) * 512], in_=ps[6][:, :]),
                 r=[K("ps", 5)], w=[K("gateb", h)])

    for s in range(3):
        mod_sublayer(0, s)

    hT = sb.alloc("hT", [128, 8, TOK], BF16)
    xn = sb.alloc("xn", [128, 4, D], BF16)
    phase_mark = sb.mark()

    def rms_stats():
        for t in range(NT):
            P.op("act", lambda e, t=t: e.activation(out=junk[:, :], in_=X[:, t, :], func=AF.Square,
                                                    accum_out=ssq[:, t:t + 1]),
                 r=[K("X", t)], w=[K("junk"), K("ssq")])
        P.op("act", lambda e: e.activation(out=rtmp[:, :], in_=ssq[:, :], func=AF.Sqrt, scale=1.0 / D, bias=EPS_AP),
             r=[K("ssq"), K("eps")], w=[K("rtmp")])
        P.op("dve", lambda e: e.reciprocal(out=rstd[:, :], in_=rtmp[:, :]), r=[K("rtmp")], w=[K("rstd")])

    def norm_to_hT(l, s):
        rms_stats()
        for tg in range(NT // 4):
            norm_group(l, s, tg, hT, tg)

    def norm_group(l, s, tg, hT, tgd):
        a0 = (l * 3 + s) * 8
        b0 = l * 72 + s * 24
        if True:
            for tt in range(4):
                t = tg * 4 + tt
                if tt % 2 == 0:
                    P.op("dve", lambda e, t=t, tt=tt: e.tensor_scalar(
                        out=xn[:, tt, :], in0=X[:, t, :], scalar1=rstd[:, t:t + 1], scalar2=None, op0=ALU.mult),
                         r=[K("X", t), K("rstd")], w=[K("xn", tt)])
                else:
                    P.op("act", lambda e, t=t, tt=tt: e.activation(
                        out=xn[:, tt, :], in_=X[:, t, :], func=AF.Copy, scale=rstd[:, t:t + 1]),
                         r=[K("X", t), K("rstd")], w=[K("xn", tt)])
            for c in range(8):
                j = cnt["psT"] % 2
                cnt["psT"] += 1
                for tt in range(4):
                    P.op("pe", lambda e, tt=tt, c=c, j=j: e.transpose(
                        out=psTb[j][:, tt * 128:(tt + 1) * 128],
                        in_=xn[:, tt, c * 128:(c + 1) * 128], identity=ident_b[:, :]),
                         r=[K("xn", tt), K("identb")], w=[K("psT", j)])
                if c % 2 == 0:
                    P.op("dve", lambda e, c=c, j=j, tg=tgd: e.tensor_scalar(
                        out=hT[:, c, tg * 512:(tg + 1) * 512], in0=psTb[j][:, 0:512],
                        scalar1=Acol[:, a0 + c:a0 + c + 1], scalar2=modT[:, b0 + c:b0 + c + 1],
                        op0=ALU.mult, op1=ALU.add),
                         r=[K("psT", j), K("Acol", l, s), K("modT", b0 + c)], w=[K("hT", c, tgd)])
                else:
                    P.op("act", lambda e, c=c, j=j, tg=tgd: e.activation(
                        out=hT[:, c, tg * 512:(tg + 1) * 512], in_=psTb[j][:, 0:512],
                        func=AF.Identity, scale=Acol[:, a0 + c:a0 + c + 1], bias=modT[:, b0 + c:b0 + c + 1]),
                         r=[K("psT", j), K("Acol", l, s), K("modT", b0 + c)], w=[K("hT", c, tgd)])

    def residual_matmul(lhs_tile, lhs_key, nk, rhs_tile, rhs_key, tmp, tlist=None, t0=0, tk="tmp"):
        for t in (tlist if tlist is not None else range(NT)):
            tl = t - t0
            for dh in range(2):
                po = 4 + cnt["psO"] % 2
                cnt["psO"] += 1
                for fi in range(nk):
                    P.op("pe", lambda e, fi=fi, tl=tl, dh=dh, po=po: e.matmul(
                        ps[po][:, :], lhsT=(lhs_tile(fi, tl) if callable(lhs_tile) else lhs_tile[:, fi, tl * 128:(tl + 1) * 128]),
                        rhs=rhs_tile[:, fi, dh * 512:(dh + 1) * 512], start=(fi == 0), stop=(fi == nk - 1)),
                         r=[lhs_key(fi, t // 4), rhs_key(fi)], w=[K("ps", po)])
                j = cnt["tmp"] % 2
                cnt["tmp"] += 1
                P.op("dve", lambda e, po=po, j=j, dh=dh: e.tensor_tensor(
                    out=tmp[j][:, :], in0=ps[po][:, :], in1=gate_b[:, dh * 512:(dh + 1) * 512], op=ALU.mult),
                     r=[K("ps", po), K("gateb", dh)], w=[K(tk, j)])
                P.op("pool", lambda e, j=j, t=t, dh=dh: e.tensor_tensor(
                    out=X[:, t, dh * 512:(dh + 1) * 512], in0=X[:, t, dh * 512:(dh + 1) * 512], in1=tmp[j][:, :],
                    op=ALU.add),
                     r=[K(tk, j), K("X", t)], w=[K("X", t)])

    def ffn(l, s2):
        s = 0 if s2 == 0 else 2
        sb.release(phase_mark)
        aT = sb.alloc("aT", [128, 6, TOK], BF16)
        Wp = [sb.alloc(f"wp{i}", [128, 8, 256], BF16) for i in range(3)]
        Wo = sb.alloc("wo", [128, 6, D], BF16)
        sg = [sb.alloc(f"sg{i}", [128, 512], BF16) for i in range(2)]
        tmp = [sb.alloc(f"tmp{i}", [128, 512], F32) for i in range(2)]
        norm_to_hT(l, s)
        gate_bcast(l, s, 0.5)
        w_in = ffn_w_in[(l, s2)].rearrange("(k p) n -> p k n", p=128)
        w_out = ffn_w_out[(l, s2)].rearrange("(f p) d -> p f d", p=128)
        kin = K(f"ffn_w_in{l}{s2}")
        kout = K(f"ffn_w_out{l}{s2}")
        for part in FFPARTS:
            nf = len(part)
            for fi, f in enumerate(part):
                dma("act", Wo[:, fi, :], w_out[:, f, :], [kout], [K("wo", fi)], f"wo{fi}")
            for fi, f in enumerate(part):
                i = cnt["wp"] % 3
                cnt["wp"] += 1
                dma("sp", Wp[i][:, :, 0:128], w_in[:, :, f * 128:(f + 1) * 128], [kin], [K("wp", i, 0)], f"wp{i}a")
                dma("sp", Wp[i][:, :, 128:256], w_in[:, :, DFF + f * 128:DFF + (f + 1) * 128], [kin],
                    [K("wp", i, 1)], f"wp{i}b")
                for tg in range(4):
                    a = cnt["psA"] % 2
                    cnt["psA"] += 1
                    for gu in range(2):
                        pb = a * 2 + gu
                        for k in range(8):
                            P.op("pe", lambda e, k=k, i=i, gu=gu, tg=tg, pb=pb: e.matmul(
                                ps[pb][:, :], lhsT=Wp[i][:, k, gu * 128:(gu + 1) * 128],
                                rhs=hT[:, k, tg * 512:(tg + 1) * 512], start=(k == 0), stop=(k == 7)),
                                 r=[K("wp", i, gu), K("hT", k, tg)], w=[K("ps", pb)])
                    j = cnt["sg"] % 2
                    cnt["sg"] += 1
                    P.op("act", lambda e, a=a, j=j: e.activation(out=sg[j][:, :], in_=ps[a * 2][:, :], func=AF.Silu),
                         r=[K("ps", a * 2)], w=[K("sg", j)])
                    P.op("dve", lambda e, a=a, j=j, fi=fi, tg=tg: e.tensor_tensor(
                        out=aT[:, fi, tg * 512:(tg + 1) * 512], in0=sg[j][:, :], in1=ps[a * 2 + 1][:, :], op=ALU.mult),
                         r=[K("sg", j), K("ps", a * 2 + 1)], w=[K("aT", fi, tg)])
            residual_matmul(aT, lambda fi, tg: K("aT", fi, tg), nf, Wo, lambda fi: K("wo", fi), tmp)
        P.barrier()

    def gmlp():
        l, s = 0, 1
        sb.release(phase_mark)
        vtok = sb.alloc("vtok", [128, 4, GMD], BF16)

        def prodT(c):
            return hT[:, c // 3, 512 + (c % 3) * 512: 512 + (c % 3 + 1) * 512]
        Wp = [sb.alloc(f"gwp{i}", [128, 8, 256], BF16) for i in range(3)]
        wsT = sb.alloc("wsT", [128, 16, 128], BF16)
        bsT = sb.alloc("bsT", [128, 24, 128], BF16)
        uT = [sb.alloc(f"uT{i}", [128, 512], BF16) for i in range(2)]
        t1 = [sb.alloc(f"t1{i}", [128, 512], F32) for i in range(2)]
        Wo = sb.alloc("gwo", [128, 6, D], BF16)
        vnT = sb.alloc("vnT", [128, 24], F32)
        ssqv = sb.alloc("ssqv", [128, 4], F32)
        rstdv = sb.alloc("rstdv", [128, 4], F32)
        hTg = hT
        transpose_small(24, gm_vn_in, vnT[:, :], K("vnT"), "vn")
        wsS = WST[0]
        dma("sp", wsS[:, :, :].rearrange("p a (b c) -> p (a b) c", b=2), gm_ws_in.rearrange("g t s -> t g s"),
            [], [K("wst", 0, 0), K("wst", 0, 1)], "wst0a")
        dma("sp", WST[1][:, 0, 0:128], gm_mask_in, [], [K("wst", 1, 0), K("wst", 1, 1)], "wst1a")
        for g in range(16):
            P.op("pe", lambda e, g=g: e.transpose(out=ps[5][:, 0:128], in_=wsS[:, g // 2, (g % 2) * 128:(g % 2 + 1) * 128],
                                                  identity=ident_f[:, :]),
                 r=[K("wst", 0, 0), K("wst", 0, 1), K("identf")], w=[K("ps", 5)])
            P.op("dve", lambda e, g=g: e.tensor_tensor(out=wsT[:, g, :], in0=ps[5][:, 0:128], in1=WST[1][:, 0, 0:128],
                                                       op=ALU.mult),
                 r=[K("ps", 5), K("wst", 1, 0)], w=[K("wsT")])
        selS = t1[0]
        dma("sp", rows[0:16, :], gm_bs_in, [], [K("rows")], "rows")
        for c in range(24):
            dma("sp", selS[0:16, 0:128], gm_sel_in[:, c * 128:(c + 1) * 128], [], [K("t1", 0)], "sel")
            P.op("pe", lambda e: e.matmul(ps[5][:, 0:128], lhsT=selS[0:16, 0:128], rhs=rows[0:16, :], start=True, stop=True),
                 r=[K("t1", 0), K("rows")], w=[K("ps", 5)])
            P.op("dve", lambda e, c=c: e.tensor_copy(out=bsT[:, c, :], in_=ps[5][:, 0:128]),
                 r=[K("ps", 5)], w=[K("bsT")])
        gate_bcast(l, s, 1.0)
        rms_stats()
        w_in = gm_w_in.rearrange("(k p) n -> p k n", p=128)
        w_out = gm_w_out.rearrange("(f p) d -> p f d", p=128)
        kin, kout = K("gmlp_w_in"), K("gmlp_w_out")
        wpc = [0]

        def load_piece(col0):
            i = wpc[0] % 3
            wpc[0] += 1
            dma("sp", Wp[i][:, :, :], w_in[:, :, col0:col0 + 256], [kin], [K("gwp", i)], f"gwp{i}")
            return i

        for tg in range(NT // 4):
            norm_group(l, s, tg, hTg, 0)
            for vb in range(12):
                i = load_piece(GMD + vb * 256)
                for tt in range(4):
                    pb = cnt["psA"] % 4
                    cnt["psA"] += 1
                    for k in range(8):
                        P.op("pe", lambda e, k=k, i=i, tt=tt, pb=pb: e.matmul(
                            ps[pb][:, 0:256], lhsT=hTg[:, k, tt * 128:(tt + 1) * 128], rhs=Wp[i][:, k, :],
                            start=(k == 0), stop=(k == 7)),
                             r=[K("gwp", i), K("hT", k, 0)], w=[K("ps", pb)])
                    P.op("act", lambda e, pb=pb, tt=tt, vb=vb: e.activation(
                        out=vtok[:, tt, vb * 256:(vb + 1) * 256], in_=ps[pb][:, 0:256], func=AF.Gelu),
                         r=[K("ps", pb)], w=[K("vtok", tt, vb)])
            for tt in range(4):
                P.op("act", lambda e, tt=tt: e.activation(out=junk[:, 0:1024], in_=vtok[:, tt, 0:1024], func=AF.Square,
                                                          accum_out=ssqv[:, tt:tt + 1]),
                     r=[K("vtok", tt, vb) for vb in range(4)], w=[K("junk"), K("ssqv", tt)])
                for part in range(1, 3):
                    P.op("act", lambda e, tt=tt, part=part: e.activation(
                        out=junk[:, 0:1024], in_=vtok[:, tt, part * 1024:(part + 1) * 1024], func=AF.Square,
                        accum_out=rtmp[:, part:part + 1]),
                         r=[K("vtok", tt, vb) for vb in range(part * 4, part * 4 + 4)], w=[K("junk"), K("rtmp")])
                    P.op("dve", lambda e, tt=tt, part=part: e.tensor_tensor(
                        out=ssqv[:, tt:tt + 1], in0=ssqv[:, tt:tt + 1], in1=rtmp[:, part:part + 1], op=ALU.add),
                         r=[K("ssqv", tt), K("rtmp")], w=[K("ssqv", tt)])
            P.op("act", lambda e: e.activation(out=rstdv[:, :], in_=ssqv[:, :], func=AF.Sqrt, scale=1.0 / GMD, bias=EPS_AP),
                 r=[K("ssqv", tt) for tt in range(4)] + [K("eps")], w=[K("rstdv")])
            P.op("dve", lambda e: e.reciprocal(out=rstdv[:, :], in_=rstdv[:, :]), r=[K("rstdv")], w=[K("rstdv")])
            for tt in range(4):
                P.op("dve", lambda e, tt=tt: e.tensor_scalar(
                    out=vtok[:, tt, :], in0=vtok[:, tt, :], scalar1=rstdv[:, tt:tt + 1], scalar2=None, op0=ALU.mult),
                     r=[K("vtok", tt, vb) for vb in range(12)] + [K("rstdv")], w=[K("vtok", tt, vb) for vb in range(12)])
            for up in range(12):
                i = load_piece(up * 256)
                for cc in range(2):
                    c = up * 2 + cc
                    pu = cnt["psA"] % 4
                    cnt["psA"] += 1
                    for k in range(8):
                        P.op("pe", lambda e, k=k, i=i, cc=cc, pu=pu: e.matmul(
                            ps[pu][:, :], lhsT=Wp[i][:, k, cc * 128:(cc + 1) * 128], rhs=hTg[:, k, 0:512],
                            start=(k == 0), stop=(k == 7)),
                             r=[K("gwp", i), K("hT", k, 0)], w=[K("ps", pu)])
                    j = cnt["sg"] % 2
                    cnt["sg"] += 1
                    P.op("act", lambda e, pu=pu, j=j: e.activation(out=uT[j][:, :], in_=ps[pu][:, :], func=AF.Gelu),
                         r=[K("ps", pu)], w=[K("uT", j)])
                    pss = cnt["psA"] % 4
                    cnt["psA"] += 1
                    f0 = c * 128
                    if c % 3 == 1:
                        segs = [(f0 // 192, 0, 64), (f0 // 192 + 1, 64, 128)]
                    else:
                        segs = [(f0 // 192, 0, 128)]
                    for tt in range(4):
                        for (g, p0, p1) in segs:
                            P.op("pe", lambda e, g=g, p0=p0, p1=p1, tt=tt, pss=pss, f0=f0: e.matmul(
                                ps[pss][p0:p1, tt * 128:(tt + 1) * 128], lhsT=vtok[:, tt, f0 + p0:f0 + p1],
                                rhs=wsT[:, g, :], start=True, stop=True),
                                 r=[K("vtok", tt, f0 // 256), K("wsT")], w=[K("ps", pss)])
                    for tt in range(4):
                        P.op("dve", lambda e, tt=tt, pss=pss, c=c, j=j: e.scalar_tensor_tensor(
                            out=t1[j][:, tt * 128:(tt + 1) * 128], in0=ps[pss][:, tt * 128:(tt + 1) * 128],
                            scalar=vnT[:, c:c + 1], in1=bsT[:, c, :], op0=ALU.mult, op1=ALU.add),
                             r=[K("ps", pss), K("vnT"), K("bsT")], w=[K("t1", j)])
                    P.op("pool", lambda e, j=j, c=c: e.tensor_tensor(out=prodT(c), in0=t1[j][:, :], in1=uT[j][:, :],
                                                                    op=ALU.mult),
                         r=[K("t1", j), K("uT", j)], w=[K("prodT", c)])
            for part in range(4):
                for fi in range(6):
                    f = part * 6 + fi
                    dma("act", Wo[:, fi, :], w_out[:, f, :], [kout], [K("gwo", fi)], f"wo{fi}")
                residual_matmul(lambda fi, tl, part=part: prodT(part * 6 + fi)[:, tl * 128:(tl + 1) * 128],
                                lambda fi, tgx, part=part: K("prodT", part * 6 + fi),
                                6, Wo, lambda fi: K("gwo", fi), t1, tlist=range(tg * 4, tg * 4 + 4), t0=tg * 4, tk="t1")
        P.barrier()

    NJ = int(os.environ.get("MOBA_NJ", "64"))
    MBIG = 262144.0

    def moba():
        l, s = 1, 1
        if not MOBA_FAKE:
            qk_loc = nc.dram_tensor("qk_loc", [2048, TOK], BF16).ap()
            qk_full = nc.dram_tensor("qk_full", [NCORES * 2048, TOK], BF16, addr_space="Shared").ap()
            v_loc = nc.dram_tensor("v_loc", [TOK, D], BF16).ap()
            v_full = nc.dram_tensor("v_full", [SEQ, D], BF16, addr_space="Shared").ap()
            o_full = nc.dram_tensor("o_full", [NCORES * SEQ, 128], BF16, addr_space="Shared").ap()
        else:
            qk_full = din("qk_full", [NCORES * 2048, TOK], BF16)
            v_full = din("v_full", [SEQ, D], BF16)
        o_loc = nc.dram_tensor("o_loc", [SEQ, 128], BF16, kind=("ExternalOutput" if MOBA_FAKE else "Internal")).ap()

        if not MOBA_FAKE:
            sb.release(phase_mark)
            Wp = [sb.alloc(f"mwp{i}", [128, 8, 256], BF16) for i in range(2)]
            stg = [sb.alloc(f"stg{i}", [128, 512], BF16) for i in range(2)]
            norm_to_hT(l, s)
            w_in = mb_w_qkv.rearrange("(k p) n -> p k n", p=128)
            kin = K("moba_w_qkv")
            stc = [0]
            loc_keys = []
            for p in range(12):
                i = p % 2
                dma("act", Wp[i][:, :, :], w_in[:, :, p * 256:(p + 1) * 256], [kin], [K("mwp", i)], f"gwp{i}")
                if p < 8:
                    for cc in range(2):
                        fc = p * 2 + cc
                        for tg in range(4):
                            pb = cnt["psA"] % 4
                            cnt["psA"] += 1
                            for k in range(8):
                                P.op("pe", lambda e, k=k, i=i, cc=cc, tg=tg, pb=pb: e.matmul(
                                    ps[pb][:, :], lhsT=Wp[i][:, k, cc * 128:(cc + 1) * 128],
                                    rhs=hT[:, k, tg * 512:(tg + 1) * 512], start=(k == 0), stop=(k == 7)),
                                     r=[K("mwp", i), K("hT", k, tg)], w=[K("ps", pb)])
                            j = stc[0] % 2
                            stc[0] += 1
                            P.op("act", lambda e, pb=pb, j=j: e.copy(out=stg[j][:, :], in_=ps[pb][:, :]),
                                 r=[K("ps", pb)], w=[K("stg", j)])
                            kk = K("qk_loc", fc, tg)
                            loc_keys.append(kk)
                            dma("sp", qk_loc[fc * 128:(fc + 1) * 128, tg * 512:(tg + 1) * 512], stg[j][:, :],
                                [K("stg", j)], [kk], f"stg{j}")
                else:
                    for t in range(NT):
                        pb = cnt["psA"] % 4
                        cnt["psA"] += 1
                        for k in range(8):
                            P.op("pe", lambda e, k=k, i=i, t=t, pb=pb: e.matmul(
                                ps[pb][:, 0:256], lhsT=hT[:, k, t * 128:(t + 1) * 128], rhs=Wp[i][:, k, :],
                                start=(k == 0), stop=(k == 7)),
                                 r=[K("mwp", i), K("hT", k, t // 4)], w=[K("ps", pb)])
                        j = stc[0] % 2
                        stc[0] += 1
                        P.op("act", lambda e, pb=pb, j=j: e.copy(out=stg[j][:, 0:256], in_=ps[pb][:, 0:256]),
                             r=[K("ps", pb)], w=[K("stg", j)])
                        kk = K("v_loc", p, t)
                        loc_keys.append(kk)
                        dma("sp", v_loc[t * 128:(t + 1) * 128, (p - 8) * 256:(p - 7) * 256], stg[j][:, 0:256],
                            [K("stg", j)], [kk], f"stg{j}")
            P.op("pool", lambda e: e.collective_compute("AllGather", ALU.bypass, replica_groups=RG,
                                                        ins=[qk_loc.opt()], outs=[qk_full.opt()]),
                 r=[k_ for k_ in loc_keys if k_[0] == "qk_loc"], w=[K("qk_full")], dma="qk_g", inc=1)
            P.op("pool", lambda e: e.collective_compute("AllGather", ALU.bypass, replica_groups=RG,
                                                        ins=[v_loc.opt()], outs=[v_full.opt()]),
                 r=[k_ for k_ in loc_keys if k_[0] == "v_loc"], w=[K("v_full")], dma="v_g", inc=1)
            P.barrier()

        sb.release(attn_mark)
        KA = sb.alloc("KA", [128, SEQ], BF16)
        QA = sb.alloc("QA", [128, SEQ], BF16)
        VA = sb.alloc("VA", [128, 128, 130], BF16)
        Ost = sb.alloc("Ost", [128, 128, 64], BF16)
        PT = [sb.alloc(f"PT{i}", [128, 512], BF16) for i in range(3)]
        Tb = sb.alloc("Tb", [128, 8, 256], BF16)
        Traw = sb.alloc("Traw", [128, 256], F32)
        Tcm = sb.alloc("Tcm", [128, 2, 256], F32)
        Tcn = sb.alloc("Tcn", [128, 2, 256], F32)
        rb31 = sb.alloc("rb31", [128, 2], F32)
        negb = sb.alloc("negb", [128, 1], F32)
        km = sb.alloc("km", [128, 64], F32)
        kmh = sb.alloc("kmh", [128, 64], BF16)
        kml = sb.alloc("kml", [128, 64], BF16)
        kmt = sb.alloc("kmt", [128, 64], F32)
        gsb = [sb.alloc(f"gsb{i}", [128, 64], F32) for i in range(2)]
        m8 = [sb.alloc(f"m8{i}", [128, 8], F32) for i in range(2)]
        mbw = [sb.alloc(f"mbw{i}", [128, 128], F32) for i in range(2)]
        rl = [sb.alloc(f"rl{i}", [128, 1], F32) for i in range(2)]

        pid_cache = {}

        def pidx(e):
            if id(e) not in pid_cache:
                pid_cache[id(e)] = e.snap(e.partition_id())
            return pid_cache[id(e)]

        dma("sp", KA[64:128, :], mb_ind_in, [], [K("KAind")], "kaind")
        P.op("pool", lambda e: e.memset(VA[:, :, 0:1], 1.0), w=[K("VA1")])
        P.op("pool", lambda e: e.memset(VA[:, :, 129:130], 1.0), w=[K("VA1")])
        P.op("pool", lambda e: e.memset(negb[:, :], -MBIG), w=[K("negb")])
        for i in range(2):
            P.op("pool", lambda e, i=i: e.memset(mbw[i][:, :], 0.0), w=[K("mbw", i)])
        dma("sp", rb31[:, :], mb_rb31_in, [], [K("rb31")], "rb31")
        dma("sp", Tcm[:, :, :], mb_cm_in.rearrange("a p q -> p a q"), [], [K("Tcm")], "tcm")
        dma("sp", Tcn[:, :, :], mb_cmneg_in.rearrange("a p q -> p a q"), [], [K("Tcn")], "tcn")
        P.op("sp", lambda e: e.dma_start(
            out=VA[:, :, 1:129],
            in_=v_full.rearrange("(hb p) c -> p hb c", p=128)[:, :, bass.ds(pidx(e) * 128, 128)]),
             r=[K("v_full")], w=[K("VA")], dma="va")
        for ti in range(8):
            hh, kind, kh = ti // 4, (ti // 2) % 2, ti % 2
            dma("sp", Traw[:, :], mb_T_in[ti], [], [K("Traw")], "traw")
            if kind == 0:
                P.op("dve", lambda e, ti=ti, hh=hh: e.tensor_scalar(
                    out=Tb[:, ti, :], in0=Traw[:, :], scalar1=rb31[:, hh:hh + 1], scalar2=8.0,
                    op0=ALU.subtract, op1=ALU.mult), r=[K("Traw"), K("rb31")], w=[K("Tb", ti)])
            else:
                P.op("dve", lambda e, ti=ti, hh=hh: e.tensor_scalar(
                    out=Traw[:, :], in0=Traw[:, :], scalar1=rb31[:, hh:hh + 1], scalar2=8.0,
                    op0=ALU.subtract, op1=ALU.mult), r=[K("Traw"), K("rb31")], w=[K("Traw")])
                P.op("dve", lambda e, kh=kh: e.tensor_tensor(out=Traw[:, :], in0=Traw[:, :], in1=Tcm[:, kh, :], op=ALU.mult),
                     r=[K("Traw"), K("Tcm")], w=[K("Traw")])
                P.op("dve", lambda e, ti=ti, kh=kh: e.tensor_tensor(out=Tb[:, ti, :], in0=Traw[:, :], in1=Tcn[:, kh, :],
                                                                  op=ALU.add),
                     r=[K("Traw"), K("Tcn")], w=[K("Tb", ti)])

        qk_v = qk_full.rearrange("(r f) t -> f r t", f=2048)
        for hh in range(2):
            P.op("sp", lambda e, hh=hh: e.dma_start(
                out=KA[0:64, :].rearrange("p (r t) -> p r t", r=NCORES),
                in_=qk_v[bass.ds(1024 + pidx(e) * 128 + hh * 64, 64), :, :]),
                 r=[K("qk_full")], w=[K("KA")], dma="ka")
            P.op("act", lambda e, hh=hh: e.dma_start(
                out=QA[0:64, :].rearrange("p (r t) -> p r t", r=NCORES),
                in_=qk_v[bass.ds(pidx(e) * 128 + hh * 64, 64), :, :]),
                 r=[K("qk_full")], w=[K("QAq")], dma="qa")
            P.op("dve", lambda e: e.tensor_reduce(out=km[0:64, :], in_=KA[0:64, :].rearrange("p (n k) -> p n k", k=256),
                                                  axis=AX.X, op=ALU.add), r=[K("KA")], w=[K("km")])
            P.op("dve", lambda e: e.tensor_copy(out=kmh[0:64, :], in_=km[0:64, :]), r=[K("km")], w=[K("kmh")])
            P.op("dve", lambda e: e.tensor_copy(out=kmt[0:64, :], in_=kmh[0:64, :]), r=[K("kmh")], w=[K("kmt")])
            P.op("dve", lambda e: e.tensor_tensor(out=kmt[0:64, :], in0=km[0:64, :], in1=kmt[0:64, :], op=ALU.subtract),
                 r=[K("km"), K("kmt")], w=[K("kmt")])
            P.op("dve", lambda e: e.tensor_copy(out=kml[0:64, :], in_=kmt[0:64, :]), r=[K("kmt")], w=[K("kml")])
            for i in range(2):
                P.op("pool", lambda e, i=i: e.memset(gsb[i][:, :], -1e30), w=[K("gsb", i)])
            for qc in range(2 * NJ):
                J = qc // 2
                g = qc % 2
                if J >= 4:
                    P.op("pe", lambda e, qc=qc: e.matmul(ps[4][:, 0:64], lhsT=QA[0:64, qc * 128:(qc + 1) * 128],
                                                         rhs=kmh[0:64, :], start=True, stop=False),
                         r=[K("QAq"), K("kmh")], w=[K("ps", 4)])
                    P.op("pe", lambda e, qc=qc: e.matmul(ps[4][:, 0:64], lhsT=QA[0:64, qc * 128:(qc + 1) * 128],
                                                         rhs=kml[0:64, :], start=False, stop=True),
                         r=[K("QAq"), K("kml")], w=[K("ps", 4)])
                    P.op("dve", lambda e, g=g, J=J: e.tensor_copy(out=gsb[g][:, 0:J], in_=ps[4][:, 0:J]),
                         r=[K("ps", 4)], w=[K("gsb", g)])
                    P.op("dve", lambda e, g=g, J=J: e.max(out=m8[g][:, :], in_=gsb[g][:, 0:max(J, 8)]),
                         r=[K("gsb", g)], w=[K("m8", g)])
                    P.op("dve", lambda e, g=g: e.tensor_scalar(
                        out=mbw[g][:, 64:128], in0=gsb[g][:, :], scalar1=m8[g][:, 2:3], scalar2=MBIG,
                        op0=ALU.is_ge, op1=ALU.mult), r=[K("gsb", g), K("m8", g)], w=[K("mbw", g)])
                    P.op("dve", lambda e, g=g, J=J: e.memset(mbw[g][:, 64 + J:65 + J], MBIG), r=[], w=[K("mbw", g)])
                else:
                    P.op("dve", lambda e, g=g: e.memset(mbw[g][:, 64:128], 0.0), w=[K("mbw", g)])
                    P.op("dve", lambda e, g=g, J=J: e.memset(mbw[g][:, 64:65 + J], MBIG), w=[K("mbw", g)])
                P.op("pe", lambda e, g=g: e.transpose(out=ps[5][:, 0:128], in_=mbw[g][:, :], identity=ident_f[:, :]),
                     r=[K("mbw", g), K("identf")], w=[K("ps", 5)])
                P.op("act", lambda e, qc=qc: e.activation(
                    out=QA[64:128, qc * 128:(qc + 1) * 128], in_=ps[5][64:128, 0:128], func=AF.Identity,
                    bias=negb[64:128, 0:1], scale=1.0), r=[K("ps", 5), K("negb")], w=[K("QAm", qc)])
            vo = 0 if hh == 0 else 65
            dcol = 0 if hh == 0 else 64
            v0 = 1 if hh == 0 else 0
            units = [(J, n) for J in range(NJ) for n in range(J + 1)]

            def emit_qk(u):
                J, n = units[u]
                b = u % 2
                pj = u % 3
                kind = 1 if n == J else (0 if n == J - 1 else -1)
                for kh in range(2):
                    P.op("pe", lambda e, n=n, kh=kh, J=J, b=b, kind=kind: e.matmul(
                        ps[b][:, kh * 256:(kh + 1) * 256], lhsT=KA[:, n * 256 + kh * 128:n * 256 + (kh + 1) * 128],
                        rhs=QA[:, J * 256:(J + 1) * 256], start=True, stop=(kind < 0)),
                         r=[K("KA"), K("KAind"), K("QAq"), K("QAm", 2 * J), K("QAm", 2 * J + 1)], w=[K("ps", b)])
                    if kind >= 0:
                        ti = hh * 4 + kind * 2 + kh
                        P.op("pe", lambda e, kh=kh, b=b, ti=ti: e.matmul(
                            ps[b][:, kh * 256:(kh + 1) * 256], lhsT=ident_b[:, :], rhs=Tb[:, ti, :],
                            start=False, stop=True), r=[K("identb"), K("Tb", ti)], w=[K("ps", b)])
                P.op("act", lambda e, b=b, pj=pj: e.activation(out=PT[pj][:, :], in_=ps[b][:, :], func=AF.Exp,
                                                               scale=0.125),
                     r=[K("ps", b)], w=[K("PT", pj)])

            def emit_pv(u):
                J, n = units[u]
                pj = u % 3
                ob = 2 * (J % 2)
                for q2 in range(2):
                    for kh in range(2):
                        P.op("pe", lambda e, q2=q2, kh=kh, n=n, J=J, pj=pj, ob=ob, vo=vo: e.matmul(
                            ps[2 + ob + q2][:, 0:65], lhsT=PT[pj][:, kh * 256 + q2 * 128:kh * 256 + (q2 + 1) * 128],
                            rhs=VA[:, n * 2 + kh, vo:vo + 65], start=(n == 0 and kh == 0),
                            stop=(n == J and kh == 1)),
                             r=[K("PT", pj), K("VA"), K("VA1")], w=[K("ps", 2 + ob + q2)])
                if n == J:
                    for q2 in range(2):
                        pb = 2 + ob + q2
                        rj = q2
                        P.op("dve", lambda e, pb=pb, rj=rj, dcol=dcol: e.reciprocal(out=rl[rj][:, :],
                                                                                   in_=ps[pb][:, dcol:dcol + 1]),
                             r=[K("ps", pb)], w=[K("rl", rj)])
                        P.op("dve", lambda e, pb=pb, rj=rj, J=J, q2=q2, v0=v0: e.tensor_scalar(
                            out=Ost[:, 2 * J + q2, :], in0=ps[pb][:, v0:v0 + 64], scalar1=rl[rj][:, 0:1], scalar2=None,
                            op0=ALU.mult), r=[K("ps", pb), K("rl", rj)], w=[K("Ost")])

            for u in range(len(units) + 1):
                if u < len(units):
                    emit_qk(u)
                if u >= 1:
                    emit_pv(u - 1)
            dma("sp", o_loc.rearrange("(qc p) c -> p qc c", p=128)[:, 0:2 * NJ, hh * 64:(hh + 1) * 64], Ost[:, 0:2 * NJ, :],
                [K("Ost")], [K("o_loc", hh)], "ost")
        if MOBA_FAKE:
            P.op("sp", _NOP, r=[K("o_loc", 0), K("o_loc", 1)], w=[])
            return
        P.op("pool", lambda e: e.collective_compute("AllGather", ALU.bypass, replica_groups=RG,
                                                    ins=[o_loc.opt()], outs=[o_full.opt()]),
             r=[K("o_loc", 0), K("o_loc", 1)], w=[K("o_full")], dma="o_g", inc=1)
        P.barrier()

        sb.release(phase_mark)
        Om = sb.alloc("Om", [128, 4, D], BF16)
        Wo = sb.alloc("mwo", [128, 8, D], BF16)
        tmp = [sb.alloc(f"mtmp{i}", [128, 512], F32) for i in range(2)]
        gate_bcast(l, s, 1.0)
        w_o = mb_w_o.rearrange("(f p) d -> p f d", p=128)
        for f0 in range(8):
            dma("act", Wo[:, f0, :], w_o[:, f0, :], [K("moba_w_o")], [K("mwo", f0)], f"mwo{f0}")
        o_mine = nc.dram_tensor("o_mine", [NCORES, TOK, 128], BF16).ap()
        P.op("sp", lambda e: e.dma_start(
            out=o_mine, in_=o_full.rearrange("(r t) c -> r t c", r=NCORES)[:, bass.ds(pidx(e) * TOK, TOK), :]),
             r=[K("o_full")], w=[K("o_mine")], dma="omine")
        for tg in range(NT // 4):
            for a in range(4):
                dma("sp" if a % 2 == 0 else "act", Om[:, a, :].rearrange("p (r c) -> p r c", r=NCORES),
                    o_mine[:, tg * 512 + a * 128:tg * 512 + (a + 1) * 128, :].rearrange("r p c -> p r c"),
                    [K("o_mine")], [K("Om", a)], f"om{a}")
            for c in range(8):
                j = cnt["psT"] % 2
                cnt["psT"] += 1
                for tt in range(4):
                    P.op("pe", lambda e, tt=tt, c=c, j=j: e.transpose(
                        out=psTb[j][:, tt * 128:(tt + 1) * 128], in_=Om[:, tt, c * 128:(c + 1) * 128],
                        identity=ident_b[:, :]), r=[K("Om", tt), K("identb")], w=[K("psT", j)])
                if c % 2 == 0:
                    P.op("dve", lambda e, c=c, j=j, tg=tg: e.tensor_copy(out=hT[:, c, tg * 512:(tg + 1) * 512],
                                                                         in_=psTb[j][:, 0:512]),
                         r=[K("psT", j)], w=[K("hT", c, tg)])
                else:
                    P.op("act", lambda e, c=c, j=j, tg=tg: e.copy(out=hT[:, c, tg * 512:(tg + 1) * 512],
                                                                  in_=psTb[j][:, 0:512]),
                         r=[K("psT", j)], w=[K("hT", c, tg)])
        residual_matmul(hT, lambda fi, tgx: K("hT", fi, tgx), 8, Wo, lambda fi: K("mwo", fi), tmp)
        P.barrier()

    def write_out(final):
        sb.release(phase_mark)
        xnf = [sb.alloc(f"xnf{i}", [128, D], F32) for i in range(2)]
        if final:
            rms_stats()
            dma("sp", gdiag[:, :], final_norm.to_broadcast([128, D]), [], [K("gdiag", c) for c in range(8)], "fn")
        for t in range(NT):
            if final:
                j = t % 2
                P.op("dve", lambda e, t=t, j=j: e.tensor_scalar(
                    out=xnf[j][:, :], in0=X[:, t, :], scalar1=rstd[:, t:t + 1], scalar2=None, op0=ALU.mult),
                     r=[K("X", t), K("rstd")], w=[K("xnf", j)])
                P.op("pool", lambda e, t=t, j=j: e.tensor_tensor(
                    out=xnf[j][:, :], in0=xnf[j][:, :], in1=gdiag[:, :], op=ALU.mult),
                     r=[K("xnf", j)] + [K("gdiag", c) for c in range(8)], w=[K("xnf", j)])
                dma("sp", y_out[t * 128:(t + 1) * 128, :], xnf[j][:, :], [K("xnf", j)], [K("y", t)], f"y{j}")
            else:
                dma("sp", y_out[t * 128:(t + 1) * 128, :], X[:, t, :], [K("X", t)], [K("y", t)], f"y{t % 2}")
        P.op("sp", _NOP, r=[K("y", t) for t in range(NT)], w=[])

    if MOBA_FAKE:
        P.barrier()
        moba()
        return nc, P
    P.barrier()
    done = False
    stages = [("ffn", 0, 0), ("mix", 0), ("ffn", 0, 1), ("ffn", 1, 0), ("mix", 1), ("ffn", 1, 1)]
    if dbg < 99:
        if dbg >= 3:
            sb.release(phase_mark)
            norm_to_hT(0, 0)
            gate_bcast(0, 0, 0.5)
        if dbg == 4:
            for t in range(NT):
                P.op("dve", lambda e, t=t: e.tensor_copy(out=X[:, t, 0:512], in_=hT[:, t % 8, 0:512]),
                     r=[K("hT", t % 8, 0)], w=[K("X", t)])
        write_out(False)
        return nc, P
    for st in stages:
        if st == ("ffn", 1, 0):
            for s_ in range(3):
                mod_sublayer(1, s_)
        if st[0] == "ffn":
            ffn(st[1], st[2])
        elif st == ("mix", 0):
            gmlp()
        elif st == ("mix", 1):
            moba()
        if stop_after == st:
            write_out(False)
            done = True
            break
    if not done:
        write_out(True)

    return nc, P


def emit_program(nc, P):
    from contextlib import ExitStack
    with ExitStack() as stack:
        block = stack.enter_context(nc.Block())
        P.emit(nc, block, stack)
    return nc


_ID = np.eye(128, dtype=np.float32)
_SEL = (np.arange(GMD)[None, :] // 192 == np.arange(16)[:, None]).astype(np.float32)
_MASK = (np.arange(128)[:, None] <= np.arange(128)[None, :]).astype(np.float32)


def _t5_bucket(rel):
    n = np.maximum(rel, 0)
    is_small = n < 16
    nf = np.maximum(n, 16).astype(np.float32)
    large = 16 + (np.log(nf / np.float32(16)) / np.float32(math.log(128 / 16)) * np.float32(16)).astype(np.int32)
    large = np.minimum(large, 31)
    return np.where(is_small, n, large)


def _moba_consts(rel_bias, core):
    rb = np.asarray(rel_bias, np.float32)
    kk = np.arange(128)[:, None]
    qq = np.arange(256)[None, :]
    T = np.zeros((8, 128, 256), np.float32)
    cm = np.zeros((2, 128, 256), np.float32)
    for hh in range(2):
        h = 2 * core + hh
        for kind in range(2):
            for kh in range(2):
                rel = qq - (kk + kh * 128) + (256 if kind == 0 else 0)
                T[hh * 4 + kind * 2 + kh] = rb[_t5_bucket(rel), h]
                if kind == 1:
                    cm[kh] = (rel >= 0).astype(np.float32)
    cmneg = (cm - 1.0) * 262144.0
    rb31 = np.broadcast_to(rb[31, 2 * core:2 * core + 2][None, :], (128, 2)).astype(np.float32).copy()
    return T, cm, cmneg, rb31


_IND = np.repeat(np.eye(64, dtype=np.float32), 256, axis=1).astype(ml_dtypes.bfloat16)


def make_in_maps(inputs, names=None):
    x = np.ascontiguousarray(np.asarray(inputs["x"], dtype=np.float32).reshape(SEQ, D))
    shared = {
        "c": np.ascontiguousarray(np.asarray(inputs["c"], np.float32).reshape(8, 128)),
        "mod_b": np.ascontiguousarray(np.asarray(inputs["mod_b"], np.float32).reshape(144, 128)),
        "norm_g": np.ascontiguousarray(np.asarray(inputs["norm_g"], np.float32).reshape(48, 128)),
        "final_norm": np.ascontiguousarray(np.asarray(inputs["final_norm"], np.float32).reshape(1, D)),
        "ident": _ID,
        "gmlp_w_s": np.ascontiguousarray(np.asarray(inputs["gmlp_w_s"], np.float32).reshape(16, 128, 128)),
        "gmlp_b_s": np.ascontiguousarray(np.asarray(inputs["gmlp_b_s"], np.float32).reshape(16, 128)),
        "gmlp_v_norm": np.ascontiguousarray(np.asarray(inputs["gmlp_v_norm"], np.float32).reshape(24, 128)),
        "gm_sel": _SEL,
        "gm_mask": _MASK,
    }
    big = {}
    for l in range(2):
        big[f"mod_w{l}"] = np.asarray(inputs["mod_w"], np.float32)[l]
        for s2 in range(2):
            big[f"ffn_w_in{l}{s2}"] = np.asarray(inputs["ffn_w_in"], np.float32)[l, s2]
            big[f"ffn_w_out{l}{s2}"] = np.asarray(inputs["ffn_w_out"], np.float32)[l, s2]
    big["gmlp_w_in"] = np.asarray(inputs["gmlp_w_in"], np.float32)[0]
    big["gmlp_w_out"] = np.asarray(inputs["gmlp_w_out"], np.float32)[0]
    big["moba_w_qkv"] = np.asarray(inputs["moba_w_qkv"], np.float32)[0]
    big["moba_w_o"] = np.asarray(inputs["moba_w_o"], np.float32)[0]
    in_maps = []
    for i in range(NCORES):
        m = dict(shared)
        m["x"] = np.ascontiguousarray(x[i * TOK:(i + 1) * TOK])
        T, cm, cmneg, rb31 = _moba_consts(inputs["rel_bias"], i)
        m["mb_T"], m["mb_cm"], m["mb_cmneg"], m["mb_rb31"] = T, cm, cmneg, rb31
        m["mb_ind"] = _IND
        for name, w in big.items():
            if names is not None and name not in names:
                continue
            r = w.shape[0] // NCORES
            m[name] = np.ascontiguousarray(w[i * r:(i + 1) * r])
        in_maps.append(m)
    return in_maps


def run(inputs, stop_after=None, trace=False):
    nc, P = build_program(stop_after)
    emit_program(nc, P)
    names = {t.name for t in nc.main_func.allocations if hasattr(t, "name")} if False else None
    in_maps = make_in_maps(inputs, None if stop_after is None or stop_after[0] != "dbg" else ({"mod_w0"} if stop_after[1] > 0 else set()))
    res = run_bass_kernel_spmd(nc, in_maps, core_ids=list(range(NCORES)), trace=trace)
    out = np.concatenate([np.asarray(r["y"]) for r in res.results], axis=0)
    return out.reshape(1, SEQ, D).astype(np.float32), res


def kernel(**inputs):
    out, _ = run(inputs)
    return out
```

```python
import math
import numpy as np
import ml_dtypes
import concourse.bass as bass
import concourse.mybir as mybir
from concourse.bass_utils import run_bass_kernel_spmd

F32 = mybir.dt.float32
BF16 = mybir.dt.bfloat16
AF = mybir.ActivationFunctionType
ALU = mybir.AluOpType
AX = mybir.AxisListType

import os
NOAG = bool(os.environ.get('NOAG'))
MOBA_FAKE = bool(os.environ.get('MOBA_FAKE'))
NCORES = 8
D = 1024
SEQ = 16384
TOK = SEQ // NCORES
NT = TOK // 128
DFF = 2816
NFF = DFF // 128
GMD = 3072
EPS = 1e-6
FFPARTS = [list(range(0, 6)), list(range(6, 12)), list(range(12, 17)), list(range(17, 22))]


def _NOP(eng):
    return eng.nop()


class Op:
    __slots__ = ("eng", "fn", "waits", "is_dma", "sem", "val", "needs_inc", "rank", "inc")


class Prog:
    ENGS = ("pe", "act", "dve", "pool", "sp")

    def __init__(self):
        self.ops = {e: [] for e in self.ENGS}
        self.last_w = {}
        self.readers = {}
        self.dma_cnt = {}

    def op(self, eng, fn, r=(), w=(), dma=None, inc=16):
        o = Op()
        o.eng = eng
        o.fn = fn
        o.is_dma = dma is not None
        o.needs_inc = False
        o.waits = []
        o.sem = None
        o.val = 0
        o.rank = 0
        o.inc = inc
        deps = {}
        for k in r:
            d = self.last_w.get(k)
            if d is not None:
                deps[id(d)] = (d, True)
        for k in w:
            d = self.last_w.get(k)
            if d is not None and id(d) not in deps:
                deps[id(d)] = (d, False)
            rd = self.readers.get(k)
            if rd:
                for x in rd[0].values():
                    if id(x) not in deps:
                        deps[id(x)] = (x, False)
                for x in rd[1]:
                    if id(x) not in deps:
                        deps[id(x)] = (x, False)
        for d, raw in deps.values():
            if d.is_dma:
                o.waits.append(("dma", d.sem, d.val))
            elif d.eng == eng:
                if eng != "pe":
                    d.needs_inc = True
                    o.waits.append(("eng", d))
            else:
                d.needs_inc = True
                o.waits.append(("eng", d))
        if dma is not None:
            self.dma_cnt[dma] = self.dma_cnt.get(dma, 0) + inc
            o.inc = inc
            o.sem = dma
            o.val = self.dma_cnt[dma]
        for k in w:
            self.last_w[k] = o
            self.readers[k] = [{}, []]
        for k in r:
            if k in w:
                continue
            rd = self.readers.setdefault(k, [{}, []])
            if o.is_dma:
                rd[1].append(o)
            else:
                rd[0][eng] = o
        self.ops[eng].append(o)
        return o

    def barrier(self):
        lasts = {}
        for e in self.ENGS:
            for o in reversed(self.ops[e]):
                if not o.is_dma and o.fn is not _NOP:
                    lasts[e] = o
                    break
        dmas = [o for e in self.ENGS for o in self.ops[e]
                if o.is_dma and o.inc != 1 and o.val == self.dma_cnt[o.sem]]
        for e in self.ENGS:
            o = Op()
            o.eng = e
            o.fn = _NOP
            o.is_dma = False
            o.needs_inc = False
            o.sem = None
            o.val = 0
            o.rank = 0
            o.inc = 16
            o.waits = []
            for f, d in lasts.items():
                if f != e:
                    d.needs_inc = True
                    o.waits.append(("eng", d))
            for d in dmas:
                o.waits.append(("dma", d.sem, d.val))
            self.ops[e].append(o)
        self.last_w = {k: o for k, o in self.last_w.items() if o.is_dma and o.inc == 1}
        self.readers = {}

    def emit(self, nc, block, stack):
        engsem = {e: stack.enter_context(nc.semaphore(f"s_{e}")) for e in self.ENGS}
        dmasem = {n: stack.enter_context(nc.semaphore(f"d_{n}")) for n in self.dma_cnt}
        for e in self.ENGS:
            c = 0
            for o in self.ops[e]:
                if (not o.is_dma) and o.needs_inc:
                    c += 1
                    o.rank = c
        prog = self

        def run(e, engobj):
            waited = {}
            for o in prog.ops[e]:
                red = {}
                for w in o.waits:
                    kk = ("d", w[1]) if w[0] == "dma" else ("e", w[1].eng)
                    vv = w[2] if w[0] == "dma" else w[1].rank
                    if kk not in red or vv > red[kk][0]:
                        red[kk] = (vv, w)
                for vv, w in red.values():
                    if w[0] == "dma":
                        key = ("d", w[1])
                        val = w[2]
                        sem = dmasem[w[1]]
                    else:
                        d = w[1]
                        key = ("e", d.eng)
                        val = d.rank
                        sem = engsem[d.eng]
                    if waited.get(key, 0) >= val:
                        continue
                    waited[key] = val
                    engobj.wait_ge(sem, val)
                ins = o.fn(engobj)
                if o.is_dma:
                    if o.inc == 1:
                        ins.then_inc(dmasem[o.sem])
                    else:
                        ins.then_inc(dmasem[o.sem], o.inc)
                elif o.needs_inc:
                    ins.then_inc(engsem[e], 1)

        @block.tensor
        def _(eng):
            run("pe", eng)

        @block.scalar
        def _(eng):
            run("act", eng)

        @block.vector
        def _(eng):
            run("dve", eng)

        @block.gpsimd
        def _(eng):
            run("pool", eng)

        @block.sync
        def _(eng):
            run("sp", eng)


class SBAlloc:
    def __init__(self, nc):
        self.nc = nc
        self.off = 16640
        self.n = 0
        self.peak = 0

    def alloc(self, name, shape, dtype):
        esz = 4 if dtype == F32 else 2
        size = esz
        for s in shape[1:]:
            size *= s
        off = (self.off + 63) // 64 * 64
        self.off = off + size
        self.peak = max(self.peak, self.off)
        self.n += 1
        assert self.off <= 229376, (name, self.off)
        return self.nc.alloc_sbuf_tensor_at(f"{name}_{self.n}", list(shape), dtype, offset=off)

    def mark(self):
        return self.off

    def release(self, m):
        self.off = m


def build_program(stop_after=None):
    nc = bass.Bass("TRN2", target_bir_lowering=False)
    P = Prog()
    sb = SBAlloc(nc)

    def K(name, *idx):
        return (name,) + idx

    def din(name, shape, dt=F32):
        return nc.dram_tensor(name, list(shape), dt, kind="ExternalInput").ap()

    x_in = din("x", [TOK, D])
    c_in = din("c", [8, 128])
    RG = [list(range(NCORES))]

    def gathered(name, R, C):
        if NOAG:
            return din(name, [R, C])
        sh = din(name, [R // NCORES, C])
        loc = nc.dram_tensor(name + "_loc", [R // NCORES, C], F32).ap()
        full = nc.dram_tensor(name + "_full", [R, C], F32, addr_space="Shared").ap()
        P.op("sp", lambda e: e.dma_start(out=loc, in_=sh), r=[], w=[K(name, "loc")], dma=name + "_l")
        P.op("pool", lambda e: e.collective_compute("AllGather", ALU.bypass, replica_groups=RG,
                                                    ins=[loc.opt()], outs=[full.opt()]),
             r=[K(name, "loc")], w=[K(name)], dma=name + "_g", inc=1)
        return full

    mod_b = din("mod_b", [2 * 72, 128])
    norm_g = din("norm_g", [48, 128])
    final_norm = din("final_norm", [1, D])
    mb_ind_in = din("mb_ind", [64, SEQ], BF16)
    mb_T_in = din("mb_T", [8, 128, 256])
    mb_cm_in = din("mb_cm", [2, 128, 256])
    mb_cmneg_in = din("mb_cmneg", [2, 128, 256])
    mb_rb31_in = din("mb_rb31", [128, 2])
    gm_ws_in = din("gmlp_w_s", [16, 128, 128])
    gm_bs_in = din("gmlp_b_s", [16, 128])
    gm_vn_in = din("gmlp_v_norm", [24, 128])
    gm_sel_in = din("gm_sel", [16, GMD])
    gm_mask_in = din("gm_mask", [128, 128])
    ident_in = din("ident", [128, 128])
    y_out = nc.dram_tensor("y", [TOK, D], F32, kind="ExternalOutput").ap()

    X = sb.alloc("X", [128, NT, D], F32)
    ident_f = sb.alloc("identf", [128, 128], F32)
    ident_b = sb.alloc("identb", [128, 128], BF16)
    ones_f = sb.alloc("onesf", [128, 128], F32)
    modT = sb.alloc("modT", [128, 144], F32)
    modbT = sb.alloc("modbT", [128, 144], F32)
    gT = sb.alloc("gT", [128, 48], F32)
    Acol = sb.alloc("Acol", [128, 48], F32)
    cT = sb.alloc("cT", [128, 8], F32)
    ssq = sb.alloc("ssq", [128, NT], F32)
    rstd = sb.alloc("rstd", [128, NT], F32)
    rtmp = sb.alloc("rtmp", [128, NT], F32)
    gate_b = sb.alloc("gateb", [128, D], F32)
    gdiag = sb.alloc("gdiag", [128, D], F32)
    rows = sb.alloc("rows", [128, 128], F32)
    junk = sb.alloc("junk", [128, D], BF16)

    PQ = [nc.alloc_psum_tensor(f"pq{i}", [128, 1024], F32) for i in range(4)]
    ps = [PQ[i // 2][:, (i % 2) * 512:(i % 2 + 1) * 512] for i in range(8)]
    _pq3b = PQ[3].bitcast(BF16)
    psTb = [_pq3b[:, i * 1024:(i + 1) * 1024] for i in range(2)]

    def dma(q, out, in_, r, w, sem):
        P.op(q, lambda e: e.dma_start(out=out, in_=in_), r=r, w=w, dma=sem)

    def transpose_small(rows_n, src_dram, dst, dst_key, tag):
        dma("sp", rows[0:rows_n, :], src_dram, [], [K("rows")], f"rows")
        P.op("pe", lambda e: e.transpose(out=ps[6][:, 0:rows_n], in_=rows[0:rows_n, :],
                                         identity=ident_f[0:rows_n, 0:rows_n]),
             r=[K("rows"), K("identf")], w=[K("ps", 5)])
        P.op("dve", lambda e: e.tensor_copy(out=dst, in_=ps[6][:, 0:rows_n]),
             r=[K("ps", 5)], w=[dst_key])

    dma("sp", ident_f[:, :], ident_in, [], [K("identf")], "c0")
    P.op("dve", lambda e: e.tensor_copy(out=ident_b[:, :], in_=ident_f[:, :]),
         r=[K("identf")], w=[K("identb")])
    P.op("pool", lambda e: e.memset(ones_f[:, :], 1.0), w=[K("onesf")])

    for t in range(NT):
        dma("sp" if t % 2 == 0 else "act", X[:, t, :], x_in[t * 128:(t + 1) * 128, :],
            [], [K("X", t)], f"x{t}")

    prep_mark = sb.mark()
    stF = [sb.alloc(f"stF{i}", [128, 6144], F32) for i in range(2)]
    stB = [sb.alloc(f"stB{i}", [128, 6144], BF16) for i in range(2)]
    prep_cnt = [0]

    def gathered_bf(name, R, C):
        if NOAG:
            return din(name, [R, C], BF16)
        sh = din(name, [R // NCORES, C])
        n = (R // NCORES) * C // 128
        loc = nc.dram_tensor(name + "_loc", [R // NCORES, C], BF16).ap()
        full = nc.dram_tensor(name + "_full", [R, C], BF16, addr_space="Shared").ap()
        i = prep_cnt[0] % 2
        prep_cnt[0] += 1
        shf = sh.rearrange("r c -> (r c)").rearrange("(p f) -> p f", p=128)
        locf = loc.rearrange("r c -> (r c)").rearrange("(p f) -> p f", p=128)
        dma("sp", stF[i][:, 0:n], shf, [], [K("stF", i)], f"stF{i}")
        a = (n * 9 // 20) // 64 * 64
        b = (n * 16 // 20) // 64 * 64
        P.op("act", lambda e: e.copy(out=stB[i][:, 0:a], in_=stF[i][:, 0:a]), r=[K("stF", i)], w=[K("stB", i, 0)])
        P.op("dve", lambda e: e.tensor_copy(out=stB[i][:, a:b], in_=stF[i][:, a:b]), r=[K("stF", i)], w=[K("stB", i, 1)])
        P.op("pool", lambda e: e.tensor_copy(out=stB[i][:, b:n], in_=stF[i][:, b:n]), r=[K("stF", i)], w=[K("stB", i, 2)])
        dma("act", locf, stB[i][:, 0:n], [K("stB", i, 0), K("stB", i, 1), K("stB", i, 2)], [K(name, "loc")], f"stB{i}")
        P.op("pool", lambda e: e.collective_compute("AllGather", ALU.bypass, replica_groups=RG,
                                                    ins=[loc.opt()], outs=[full.opt()]),
             r=[K(name, "loc")], w=[K(name)], dma=name + "_g", inc=1)
        return full

    dbg = stop_after[1] if (stop_after and stop_after[0] == "dbg") else 99
    mod_w = [None, None]
    ffn_w_in = {}
    ffn_w_out = {}
    mod_w[0] = gathered("mod_w0", D, 9216)
    ffn_w_in[(0, 0)] = gathered_bf("ffn_w_in00", D, 2 * DFF)
    ffn_w_out[(0, 0)] = gathered_bf("ffn_w_out00", DFF, D)
    gm_w_in = gathered_bf("gmlp_w_in", D, 2 * GMD)
    gm_w_out = gathered_bf("gmlp_w_out", GMD, D)
    ffn_w_in[(0, 1)] = gathered_bf("ffn_w_in01", D, 2 * DFF)
    ffn_w_out[(0, 1)] = gathered_bf("ffn_w_out01", DFF, D)
    mod_w[1] = gathered("mod_w1", D, 9216)
    ffn_w_in[(1, 0)] = gathered_bf("ffn_w_in10", D, 2 * DFF)
    ffn_w_out[(1, 0)] = gathered_bf("ffn_w_out10", DFF, D)
    mb_w_qkv = gathered_bf("moba_w_qkv", D, 3 * D)
    mb_w_o = gathered_bf("moba_w_o", D, D)
    ffn_w_in[(1, 1)] = gathered_bf("ffn_w_in11", D, 2 * DFF)
    ffn_w_out[(1, 1)] = gathered_bf("ffn_w_out11", DFF, D)
    P.barrier()
    sb.release(prep_mark)

    transpose_small(8, c_in, cT[:, :], K("cT"), "c")
    P.op("act", lambda e: e.activation(out=cT[:, :], in_=cT[:, :], func=AF.Silu),
         r=[K("cT")], w=[K("cT")])
    for half in range(2):
        transpose_small(72, mod_b[half * 72:(half + 1) * 72, :], modbT[:, half * 72:(half + 1) * 72],
                        K("modbT", half), "mb")
    transpose_small(48, norm_g, gT[:, :], K("gT"), "g")

    eps_t = sb.alloc("eps", [128, 1], F32)
    EPS_AP = eps_t[:, 0:1]
    P.op("pool", lambda e: e.memset(eps_t[:, :], EPS), w=[K("eps")])

    attn_mark = sb.mark()
    WST = [sb.alloc(f"wst{i}", [128, 8, 256], F32) for i in range(2)]
    cnt = {"wst": 0, "wp": 0, "sg": 0, "tmp": 0, "psA": 0, "psO": 0, "psT": 0, "wos": 0}

    def mod_piece(l, n):
        i = cnt["wst"] % 2
        cnt["wst"] += 1
        src = mod_w[l].rearrange("(k p) n -> p k n", p=128)[:, :, n * 256:(n + 1) * 256]
        dma("sp", WST[i][:, :, :], src, [K(f"mod_w{l}")], [K("wst", i, 0), K("wst", i, 1)], f"wst{i}a")
        for j in range(2):
            for k in range(8):
                P.op("pe", lambda e, k=k, j=j, i=i: e.matmul(
                    ps[6][:, j:j + 1], lhsT=WST[i][:, k, j * 128:(j + 1) * 128], rhs=cT[:, k:k + 1],
                    start=(k == 0), stop=(k == 7)),
                     r=[K("wst", i, j), K("cT")], w=[K("ps", 5)])
        col0 = l * 72 + 2 * n
        P.op("dve", lambda e: e.tensor_tensor(out=modT[:, col0:col0 + 2], in0=ps[6][:, 0:2],
                                              in1=modbT[:, col0:col0 + 2], op=ALU.add),
             r=[K("ps", 5), K("modbT", l)], w=[K("modT", col0), K("modT", col0 + 1)])

    def mod_sublayer(l, s):
        for n in range(12):
            mod_piece(l, s * 12 + n)
        base = l * 72 + s * 24
        a0 = (l * 3 + s) * 8
        P.op("dve", lambda e: e.scalar_tensor_tensor(
            out=Acol[:, a0:a0 + 8], in0=modT[:, base + 8:base + 16], scalar=1.0,
            in1=gT[:, a0:a0 + 8], op0=ALU.add, op1=ALU.mult),
             r=[K("modT", base + 8 + i) for i in range(8)] + [K("gT")], w=[K("Acol", l, s)])

    def gate_bcast(l, s, mul):
        base = l * 72 + s * 24 + 16
        for c in range(8):
            P.op("dve", lambda e, c=c: e.tensor_scalar(
                out=gdiag[:, c * 128:(c + 1) * 128], in0=ident_f[:, :], scalar1=modT[:, base + c:base + c + 1],
                scalar2=float(mul), op0=ALU.mult, op1=ALU.mult),
                 r=[K("identf"), K("modT", base + c)], w=[K("gdiag", c)])
        for h in range(2):
            P.op("pe", lambda e, h=h: e.matmul(ps[6][:, :], lhsT=ones_f[:, :], rhs=gdiag[:, h * 512:(h + 1) * 512],
                                               start=True, stop=True),
                 r=[K("onesf")] + [K("gdiag", c) for c in range(h * 4, h * 4 + 4)], w=[K("ps", 5)])
            P.op("act", lambda e, h=h: e.copy(out=gate_b[:, h * 512:(h + 1) * 512], in_=ps[6][:, :]),
                 r=[K("ps", 5)], w=[K("gateb", h)])

    for s in range(3):
        mod_sublayer(0, s)

    hT = sb.alloc("hT", [128, 8, TOK], BF16)
    xn = sb.alloc("xn", [128, 4, D], BF16)
    phase_mark = sb.mark()

    def rms_stats():
        for t in range(NT):
            P.op("act", lambda e, t=t: e.activation(out=junk[:, :], in_=X[:, t, :], func=AF.Square,
                                                    accum_out=ssq[:, t:t + 1]),
                 r=[K("X", t)], w=[K("junk"), K("ssq")])
        P.op("act", lambda e: e.activation(out=rtmp[:, :], in_=ssq[:, :], func=AF.Sqrt, scale=1.0 / D, bias=EPS_AP),
             r=[K("ssq"), K("eps")], w=[K("rtmp")])
        P.op("dve", lambda e: e.reciprocal(out=rstd[:, :], in_=rtmp[:, :]), r=[K("rtmp")], w=[K("rstd")])

    def norm_to_hT(l, s):
        rms_stats()
        for tg in range(NT // 4):
            norm_group(l, s, tg, hT, tg)

    def norm_group(l, s, tg, hT, tgd):
        a0 = (l * 3 + s) * 8
        b0 = l * 72 + s * 24
        if True:
            for tt in range(4):
                t = tg * 4 + tt
                if tt % 2 == 0:
                    P.op("dve", lambda e, t=t, tt=tt: e.tensor_scalar(
                        out=xn[:, tt, :], in0=X[:, t, :], scalar1=rstd[:, t:t + 1], scalar2=None, op0=ALU.mult),
                         r=[K("X", t), K("rstd")], w=[K("xn", tt)])
                else:
                    P.op("act", lambda e, t=t, tt=tt: e.activation(
                        out=xn[:, tt, :], in_=X[:, t, :], func=AF.Copy, scale=rstd[:, t:t + 1]),
                         r=[K("X", t), K("rstd")], w=[K("xn", tt)])
            for c in range(8):
                j = cnt["psT"] % 2
                cnt["psT"] += 1
                for tt in range(4):
                    P.op("pe", lambda e, tt=tt, c=c, j=j: e.transpose(
                        out=psTb[j][:, tt * 128:(tt + 1) * 128],
                        in_=xn[:, tt, c * 128:(c + 1) * 128], identity=ident_b[:, :]),
                         r=[K("xn", tt), K("identb")], w=[K("psT", j)])
                if c % 2 == 0:
                    P.op("dve", lambda e, c=c, j=j, tg=tgd: e.tensor_scalar(
                        out=hT[:, c, tg * 512:(tg + 1) * 512], in0=psTb[j][:, 0:512],
                        scalar1=Acol[:, a0 + c:a0 + c + 1], scalar2=modT[:, b0 + c:b0 + c + 1],
                        op0=ALU.mult, op1=ALU.add),
                         r=[K("psT", j), K("Acol", l, s), K("modT", b0 + c)], w=[K("hT", c, tgd)])
                else:
                    P.op("act", lambda e, c=c, j=j, tg=tgd: e.activation(
                        out=hT[:, c, tg * 512:(tg + 1) * 512], in_=psTb[j][:, 0:512],
                        func=AF.Identity, scale=Acol[:, a0 + c:a0 + c + 1], bias=modT[:, b0 + c:b0 + c + 1]),
                         r=[K("psT", j), K("Acol", l, s), K("modT", b0 + c)], w=[K("hT", c, tgd)])

    def residual_matmul(lhs_tile, lhs_key, nk, rhs_tile, rhs_key, tmp, tlist=None, t0=0, tk="tmp"):
        for t in (tlist if tlist is not None else range(NT)):
            tl = t - t0
            for dh in range(2):
                po = 4 + cnt["psO"] % 2
                cnt["psO"] += 1
                for fi in range(nk):
                    P.op("pe", lambda e, fi=fi, tl=tl, dh=dh, po=po: e.matmul(
                        ps[po][:, :], lhsT=(lhs_tile(fi, tl) if callable(lhs_tile) else lhs_tile[:, fi, tl * 128:(tl + 1) * 128]),
                        rhs=rhs_tile[:, fi, dh * 512:(dh + 1) * 512], start=(fi == 0), stop=(fi == nk - 1)),
                         r=[lhs_key(fi, t // 4), rhs_key(fi)], w=[K("ps", po)])
                j = cnt["tmp"] % 2
                cnt["tmp"] += 1
                P.op("dve", lambda e, po=po, j=j, dh=dh: e.tensor_tensor(
                    out=tmp[j][:, :], in0=ps[po][:, :], in1=gate_b[:, dh * 512:(dh + 1) * 512], op=ALU.mult),
                     r=[K("ps", po), K("gateb", dh)], w=[K(tk, j)])
                P.op("pool", lambda e, j=j, t=t, dh=dh: e.tensor_tensor(
                    out=X[:, t, dh * 512:(dh + 1) * 512], in0=X[:, t, dh * 512:(dh + 1) * 512], in1=tmp[j][:, :],
                    op=ALU.add),
                     r=[K(tk, j), K("X", t)], w=[K("X", t)])

    def ffn(l, s2):
        s = 0 if s2 == 0 else 2
        sb.release(phase_mark)
        aT = sb.alloc("aT", [128, 6, TOK], BF16)
        Wp = [sb.alloc(f"wp{i}", [128, 8, 256], BF16) for i in range(3)]
        Wo = sb.alloc("wo", [128, 6, D], BF16)
        sg = [sb.alloc(f"sg{i}", [128, 512], BF16) for i in range(2)]
        tmp = [sb.alloc(f"tmp{i}", [128, 512], F32) for i in range(2)]
        norm_to_hT(l, s)
        gate_bcast(l, s, 0.5)
        w_in = ffn_w_in[(l, s2)].rearrange("(k p) n -> p k n", p=128)
        w_out = ffn_w_out[(l, s2)].rearrange("(f p) d -> p f d", p=128)
        kin = K(f"ffn_w_in{l}{s2}")
        kout = K(f"ffn_w_out{l}{s2}")
        for part in FFPARTS:
            nf = len(part)
            for fi, f in enumerate(part):
                dma("act", Wo[:, fi, :], w_out[:, f, :], [kout], [K("wo", fi)], f"wo{fi}")
            for fi, f in enumerate(part):
                i = cnt["wp"] % 3
                cnt["wp"] += 1
                dma("sp", Wp[i][:, :, 0:128], w_in[:, :, f * 128:(f + 1) * 128], [kin], [K("wp", i, 0)], f"wp{i}a")
                dma("sp", Wp[i][:, :, 128:256], w_in[:, :, DFF + f * 128:DFF + (f + 1) * 128], [kin],
                    [K("wp", i, 1)], f"wp{i}b")
                for tg in range(4):
                    a = cnt["psA"] % 2
                    cnt["psA"] += 1
                    for gu in range(2):
                        pb = a * 2 + gu
                        for k in range(8):
                            P.op("pe", lambda e, k=k, i=i, gu=gu, tg=tg, pb=pb: e.matmul(
                                ps[pb][:, :], lhsT=Wp[i][:, k, gu * 128:(gu + 1) * 128],
                                rhs=hT[:, k, tg * 512:(tg + 1) * 512], start=(k == 0), stop=(k == 7)),
                                 r=[K("wp", i, gu), K("hT", k, tg)], w=[K("ps", pb)])
                    j = cnt["sg"] % 2
                    cnt["sg"] += 1
                    P.op("act", lambda e, a=a, j=j: e.activation(out=sg[j][:, :], in_=ps[a * 2][:, :], func=AF.Silu),
                         r=[K("ps", a * 2)], w=[K("sg", j)])
                    P.op("dve", lambda e, a=a, j=j, fi=fi, tg=tg: e.tensor_tensor(
                        out=aT[:, fi, tg * 512:(tg + 1) * 512], in0=sg[j][:, :], in1=ps[a * 2 + 1][:, :], op=ALU.mult),
                         r=[K("sg", j), K("ps", a * 2 + 1)], w=[K("aT", fi, tg)])
            residual_matmul(aT, lambda fi, tg: K("aT", fi, tg), nf, Wo, lambda fi: K("wo", fi), tmp)
        P.barrier()

    def gmlp():
        l, s = 0, 1
        sb.release(phase_mark)
        vtok = sb.alloc("vtok", [128, 4, GMD], BF16)

        def prodT(c):
            return hT[:, c // 3, 512 + (c % 3) * 512: 512 + (c % 3 + 1) * 512]
        Wp = [sb.alloc(f"gwp{i}", [128, 8, 256], BF16) for i in range(3)]
        wsT = sb.alloc("wsT", [128, 16, 128], BF16)
        bsT = sb.alloc("bsT", [128, 24, 128], BF16)
        uT = [sb.alloc(f"uT{i}", [128, 512], BF16) for i in range(2)]
        t1 = [sb.alloc(f"t1{i}", [128, 512], F32) for i in range(2)]
        Wo = sb.alloc("gwo", [128, 6, D], BF16)
        vnT = sb.alloc("vnT", [128, 24], F32)
        ssqv = sb.alloc("ssqv", [128, 4], F32)
        rstdv = sb.alloc("rstdv", [128, 4], F32)
        hTg = hT
        transpose_small(24, gm_vn_in, vnT[:, :], K("vnT"), "vn")
        wsS = WST[0]
        dma("sp", wsS[:, :, :].rearrange("p a (b c) -> p (a b) c", b=2), gm_ws_in.rearrange("g t s -> t g s"),
            [], [K("wst", 0, 0), K("wst", 0, 1)], "wst0a")
        dma("sp", WST[1][:, 0, 0:128], gm_mask_in, [], [K("wst", 1, 0), K("wst", 1, 1)], "wst1a")
        for g in range(16):
            P.op("pe", lambda e, g=g: e.transpose(out=ps[5][:, 0:128], in_=wsS[:, g // 2, (g % 2) * 128:(g % 2 + 1) * 128],
                                                  identity=ident_f[:, :]),
                 r=[K("wst", 0, 0), K("wst", 0, 1), K("identf")], w=[K("ps", 5)])
            P.op("dve", lambda e, g=g: e.tensor_tensor(out=wsT[:, g, :], in0=ps[5][:, 0:128], in1=WST[1][:, 0, 0:128],
                                                       op=ALU.mult),
                 r=[K("ps", 5), K("wst", 1, 0)], w=[K("wsT")])
        selS = t1[0]
        dma("sp", rows[0:16, :], gm_bs_in, [], [K("rows")], "rows")
        for c in range(24):
            dma("sp", selS[0:16, 0:128], gm_sel_in[:, c * 128:(c + 1) * 128], [], [K("t1", 0)], "sel")
            P.op("pe", lambda e: e.matmul(ps[5][:, 0:128], lhsT=selS[0:16, 0:128], rhs=rows[0:16, :], start=True, stop=True),
                 r=[K("t1", 0), K("rows")], w=[K("ps", 5)])
            P.op("dve", lambda e, c=c: e.tensor_copy(out=bsT[:, c, :], in_=ps[5][:, 0:128]),
                 r=[K("ps", 5)], w=[K("bsT")])
        gate_bcast(l, s, 1.0)
        rms_stats()
        w_in = gm_w_in.rearrange("(k p) n -> p k n", p=128)
        w_out = gm_w_out.rearrange("(f p) d -> p f d", p=128)
        kin, kout = K("gmlp_w_in"), K("gmlp_w_out")
        wpc = [0]

        def load_piece(col0):
            i = wpc[0] % 3
            wpc[0] += 1
            dma("sp", Wp[i][:, :, :], w_in[:, :, col0:col0 + 256], [kin], [K("gwp", i)], f"gwp{i}")
            return i

        for tg in range(NT // 4):
            norm_group(l, s, tg, hTg, 0)
            for vb in range(12):
                i = load_piece(GMD + vb * 256)
                for tt in range(4):
                    pb = cnt["psA"] % 4
                    cnt["psA"] += 1
                    for k in range(8):
                        P.op("pe", lambda e, k=k, i=i, tt=tt, pb=pb: e.matmul(
                            ps[pb][:, 0:256], lhsT=hTg[:, k, tt * 128:(tt + 1) * 128], rhs=Wp[i][:, k, :],
                            start=(k == 0), stop=(k == 7)),
                             r=[K("gwp", i), K("hT", k, 0)], w=[K("ps", pb)])
                    P.op("act", lambda e, pb=pb, tt=tt, vb=vb: e.activation(
                        out=vtok[:, tt, vb * 256:(vb + 1) * 256], in_=ps[pb][:, 0:256], func=AF.Gelu),
                         r=[K("ps", pb)], w=[K("vtok", tt, vb)])
            for tt in range(4):
                P.op("act", lambda e, tt=tt: e.activation(out=junk[:, 0:1024], in_=vtok[:, tt, 0:1024], func=AF.Square,
                                                          accum_out=ssqv[:, tt:tt + 1]),
                     r=[K("vtok", tt, vb) for vb in range(4)], w=[K("junk"), K("ssqv", tt)])
                for part in range(1, 3):
                    P.op("act", lambda e, tt=tt, part=part: e.activation(
                        out=junk[:, 0:1024], in_=vtok[:, tt, part * 1024:(part + 1) * 1024], func=AF.Square,
                        accum_out=rtmp[:, part:part + 1]),
                         r=[K("vtok", tt, vb) for vb in range(part * 4, part * 4 + 4)], w=[K("junk"), K("rtmp")])
                    P.op("dve", lambda e, tt=tt, part=part: e.tensor_tensor(
                        out=ssqv[:, tt:tt + 1], in0=ssqv[:, tt:tt + 1], in1=rtmp[:, part:part + 1], op=ALU.add),
                         r=[K("ssqv", tt), K("rtmp")], w=[K("ssqv", tt)])
            P.op("act", lambda e: e.activation(out=rstdv[:, :], in_=ssqv[:, :], func=AF.Sqrt, scale=1.0 / GMD, bias=EPS_AP),
                 r=[K("ssqv", tt) for tt in range(4)] + [K("eps")], w=[K("rstdv")])
            P.op("dve", lambda e: e.reciprocal(out=rstdv[:, :], in_=rstdv[:, :]), r=[K("rstdv")], w=[K("rstdv")])
            for tt in range(4):
                P.op("dve", lambda e, tt=tt: e.tensor_scalar(
                    out=vtok[:, tt, :], in0=vtok[:, tt, :], scalar1=rstdv[:, tt:tt + 1], scalar2=None, op0=ALU.mult),
                     r=[K("vtok", tt, vb) for vb in range(12)] + [K("rstdv")], w=[K("vtok", tt, vb) for vb in range(12)])
            for up in range(12):
                i = load_piece(up * 256)
                for cc in range(2):
                    c = up * 2 + cc
                    pu = cnt["psA"] % 4
                    cnt["psA"] += 1
                    for k in range(8):
                        P.op("pe", lambda e, k=k, i=i, cc=cc, pu=pu: e.matmul(
                            ps[pu][:, :], lhsT=Wp[i][:, k, cc * 128:(cc + 1) * 128], rhs=hTg[:, k, 0:512],
                            start=(k == 0), stop=(k == 7)),
                             r=[K("gwp", i), K("hT", k, 0)], w=[K("ps", pu)])
                    j = cnt["sg"] % 2
                    cnt["sg"] += 1
                    P.op("act", lambda e, pu=pu, j=j: e.activation(out=uT[j][:, :], in_=ps[pu][:, :], func=AF.Gelu),
                         r=[K("ps", pu)], w=[K("uT", j)])
                    pss = cnt["psA"] % 4
                    cnt["psA"] += 1
                    f0 = c * 128
                    if c % 3 == 1:
                        segs = [(f0 // 192, 0, 64), (f0 // 192 + 1, 64, 128)]
                    else:
                        segs = [(f0 // 192, 0, 128)]
                    for tt in range(4):
                        for (g, p0, p1) in segs:
                            P.op("pe", lambda e, g=g, p0=p0, p1=p1, tt=tt, pss=pss, f0=f0: e.matmul(
                                ps[pss][p0:p1, tt * 128:(tt + 1) * 128], lhsT=vtok[:, tt, f0 + p0:f0 + p1],
                                rhs=wsT[:, g, :], start=True, stop=True),
                                 r=[K("vtok", tt, f0 // 256), K("wsT")], w=[K("ps", pss)])
                    for tt in range(4):
                        P.op("dve", lambda e, tt=tt, pss=pss, c=c, j=j: e.scalar_tensor_tensor(
                            out=t1[j][:, tt * 128:(tt + 1) * 128], in0=ps[pss][:, tt * 128:(tt + 1) * 128],
                            scalar=vnT[:, c:c + 1], in1=bsT[:, c, :], op0=ALU.mult, op1=ALU.add),
                             r=[K("ps", pss), K("vnT"), K("bsT")], w=[K("t1", j)])
                    P.op("pool", lambda e, j=j, c=c: e.tensor_tensor(out=prodT(c), in0=t1[j][:, :], in1=uT[j][:, :],
                                                                    op=ALU.mult),
                         r=[K("t1", j), K("uT", j)], w=[K("prodT", c)])
            for part in range(4):
                for fi in range(6):
                    f = part * 6 + fi
                    dma("act", Wo[:, fi, :], w_out[:, f, :], [kout], [K("gwo", fi)], f"wo{fi}")
                residual_matmul(lambda fi, tl, part=part: prodT(part * 6 + fi)[:, tl * 128:(tl + 1) * 128],
                                lambda fi, tgx, part=part: K("prodT", part * 6 + fi),
                                6, Wo, lambda fi: K("gwo", fi), t1, tlist=range(tg * 4, tg * 4 + 4), t0=tg * 4, tk="t1")
        P.barrier()

    NJ = int(os.environ.get("MOBA_NJ", "64"))
    MBIG = 262144.0

    def moba():
        l, s = 1, 1
        if not MOBA_FAKE:
            qk_loc = nc.dram_tensor("qk_loc", [2048, TOK], BF16).ap()
            qk_full = nc.dram_tensor("qk_full", [NCORES * 2048, TOK], BF16, addr_space="Shared").ap()
            v_loc = nc.dram_tensor("v_loc", [TOK, D], BF16).ap()
            v_full = nc.dram_tensor("v_full", [SEQ, D], BF16, addr_space="Shared").ap()
            o_full = nc.dram_tensor("o_full", [NCORES * SEQ, 128], BF16, addr_space="Shared").ap()
        else:
            qk_full = din("qk_full", [NCORES * 2048, TOK], BF16)
            v_full = din("v_full", [SEQ, D], BF16)
        o_loc = nc.dram_tensor("o_loc", [SEQ, 128], BF16, kind=("ExternalOutput" if MOBA_FAKE else "Internal")).ap()

        if not MOBA_FAKE:
            sb.release(phase_mark)
            Wp = [sb.alloc(f"mwp{i}", [128, 8, 256], BF16) for i in range(2)]
            stg = [sb.alloc(f"stg{i}", [128, 512], BF16) for i in range(2)]
            norm_to_hT(l, s)
            w_in = mb_w_qkv.rearrange("(k p) n -> p k n", p=128)
            kin = K("moba_w_qkv")
            stc = [0]
            loc_keys = []
            for p in range(12):
                i = p % 2
                dma("act", Wp[i][:, :, :], w_in[:, :, p * 256:(p + 1) * 256], [kin], [K("mwp", i)], f"gwp{i}")
                if p < 8:
                    for cc in range(2):
                        fc = p * 2 + cc
                        for tg in range(4):
                            pb = cnt["psA"] % 4
                            cnt["psA"] += 1
                            for k in range(8):
                                P.op("pe", lambda e, k=k, i=i, cc=cc, tg=tg, pb=pb: e.matmul(
                                    ps[pb][:, :], lhsT=Wp[i][:, k, cc * 128:(cc + 1) * 128],
                                    rhs=hT[:, k, tg * 512:(tg + 1) * 512], start=(k == 0), stop=(k == 7)),
                                     r=[K("mwp", i), K("hT", k, tg)], w=[K("ps", pb)])
                            j = stc[0] % 2
                            stc[0] += 1
                            P.op("act", lambda e, pb=pb, j=j: e.copy(out=stg[j][:, :], in_=ps[pb][:, :]),
                                 r=[K("ps", pb)], w=[K("stg", j)])
                            kk = K("qk_loc", fc, tg)
                            loc_keys.append(kk)
                            dma("sp", qk_loc[fc * 128:(fc + 1) * 128, tg * 512:(tg + 1) * 512], stg[j][:, :],
                                [K("stg", j)], [kk], f"stg{j}")
                else:
                    for t in range(NT):
                        pb = cnt["psA"] % 4
                        cnt["psA"] += 1
                        for k in range(8):
                            P.op("pe", lambda e, k=k, i=i, t=t, pb=pb: e.matmul(
                                ps[pb][:, 0:256], lhsT=hT[:, k, t * 128:(t + 1) * 128], rhs=Wp[i][:, k, :],
                                start=(k == 0), stop=(k == 7)),
                                 r=[K("mwp", i), K("hT", k, t // 4)], w=[K("ps", pb)])
                        j = stc[0] % 2
                        stc[0] += 1
                        P.op("act", lambda e, pb=pb, j=j: e.copy(out=stg[j][:, 0:256], in_=ps[pb][:, 0:256]),
                             r=[K("ps", pb)], w=[K("stg", j)])
                        kk = K("v_loc", p, t)
                        loc_keys.append(kk)
                        dma("sp", v_loc[t * 128:(t + 1) * 128, (p - 8) * 256:(p - 7) * 256], stg[j][:, 0:256],
                            [K("stg", j)], [kk], f"stg{j}")
            P.op("pool", lambda e: e.collective_compute("AllGather", ALU.bypass, replica_groups=RG,
                                                        ins=[qk_loc.opt()], outs=[qk_full.opt()]),
                 r=[k_ for k_ in loc_keys if k_[0] == "qk_loc"], w=[K("qk_full")], dma="qk_g", inc=1)
            P.op("pool", lambda e: e.collective_compute("AllGather", ALU.bypass, replica_groups=RG,
                                                        ins=[v_loc.opt()], outs=[v_full.opt()]),
                 r=[k_ for k_ in loc_keys if k_[0] == "v_loc"], w=[K("v_full")], dma="v_g", inc=1)
            P.barrier()

        sb.release(attn_mark)
        KA = sb.alloc("KA", [128, SEQ], BF16)
        QA = sb.alloc("QA", [128, SEQ], BF16)
        VA = sb.alloc("VA", [128, 128, 130], BF16)
        Ost = sb.alloc("Ost", [128, 128, 64], BF16)
        PT = [sb.alloc(f"PT{i}", [128, 1024], BF16) for i in range(3)]
        Tb = sb.alloc("Tb", [128, 8, 256], BF16)
        Traw = sb.alloc("Traw", [128, 256], F32)
        Tcm = sb.alloc("Tcm", [128, 2, 256], F32)
        Tcn = sb.alloc("Tcn", [128, 2, 256], F32)
        rb31 = sb.alloc("rb31", [128, 2], F32)
        negb = sb.alloc("negb", [128, 1], F32)
        km = sb.alloc("km", [128, 64], F32)
        kmh = sb.alloc("kmh", [128, 64], BF16)
        kml = sb.alloc("kml", [128, 64], BF16)
        kmt = sb.alloc("kmt", [128, 64], F32)
        gsb = [sb.alloc(f"gsb{i}", [128, 64], F32) for i in range(2)]
        m8 = [sb.alloc(f"m8{i}", [128, 8], F32) for i in range(2)]
        mbw = [sb.alloc(f"mbw{i}", [128, 128], F32) for i in range(2)]
        rl = [sb.alloc(f"rl{i}", [128, 1], F32) for i in range(2)]

        pid_cache = {}

        def pidx(e):
            if id(e) not in pid_cache:
                pid_cache[id(e)] = e.snap(e.partition_id())
            return pid_cache[id(e)]

        dma("sp", KA[64:128, :], mb_ind_in, [], [K("KAind")], "kaind")
        P.op("pool", lambda e: e.memset(VA[:, :, 0:1], 1.0), w=[K("VA1")])
        P.op("pool", lambda e: e.memset(VA[:, :, 129:130], 1.0), w=[K("VA1")])
        P.op("pool", lambda e: e.memset(negb[:, :], -MBIG), w=[K("negb")])
        for i in range(2):
            P.op("pool", lambda e, i=i: e.memset(mbw[i][:, :], 0.0), w=[K("mbw", i)])
        dma("sp", rb31[:, :], mb_rb31_in, [], [K("rb31")], "rb31")
        dma("sp", Tcm[:, :, :], mb_cm_in.rearrange("a p q -> p a q"), [], [K("Tcm")], "tcm")
        dma("sp", Tcn[:, :, :], mb_cmneg_in.rearrange("a p q -> p a q"), [], [K("Tcn")], "tcn")
        P.op("sp", lambda e: e.dma_start(
            out=VA[:, :, 1:129],
            in_=v_full.rearrange("(hb p) c -> p hb c", p=128)[:, :, bass.ds(pidx(e) * 128, 128)]),
             r=[K("v_full")], w=[K("VA")], dma="va")
        for ti in range(8):
            hh, kind, kh = ti // 4, (ti // 2) % 2, ti % 2
            dma("sp", Traw[:, :], mb_T_in[ti], [], [K("Traw")], "traw")
            if kind == 0:
                P.op("dve", lambda e, ti=ti, hh=hh: e.tensor_scalar(
                    out=Tb[:, ti, :], in0=Traw[:, :], scalar1=rb31[:, hh:hh + 1], scalar2=8.0,
                    op0=ALU.subtract, op1=ALU.mult), r=[K("Traw"), K("rb31")], w=[K("Tb", ti)])
            else:
                P.op("dve", lambda e, ti=ti, hh=hh: e.tensor_scalar(
                    out=Traw[:, :], in0=Traw[:, :], scalar1=rb31[:, hh:hh + 1], scalar2=8.0,
                    op0=ALU.subtract, op1=ALU.mult), r=[K("Traw"), K("rb31")], w=[K("Traw")])
                P.op("dve", lambda e, kh=kh: e.tensor_tensor(out=Traw[:, :], in0=Traw[:, :], in1=Tcm[:, kh, :], op=ALU.mult),
                     r=[K("Traw"), K("Tcm")], w=[K("Traw")])
                P.op("dve", lambda e, ti=ti, kh=kh: e.tensor_tensor(out=Tb[:, ti, :], in0=Traw[:, :], in1=Tcn[:, kh, :],
                                                                  op=ALU.add),
                     r=[K("Traw"), K("Tcn")], w=[K("Tb", ti)])

        qk_v = qk_full.rearrange("(r f) t -> f r t", f=2048)
        for hh in range(2):
            P.op("sp", lambda e, hh=hh: e.dma_start(
                out=KA[0:64, :].rearrange("p (r t) -> p r t", r=NCORES),
                in_=qk_v[bass.ds(1024 + pidx(e) * 128 + hh * 64, 64), :, :]),
                 r=[K("qk_full")], w=[K("KA")], dma="ka")
            P.op("act", lambda e, hh=hh: e.dma_start(
                out=QA[0:64, :].rearrange("p (r t) -> p r t", r=NCORES),
                in_=qk_v[bass.ds(pidx(e) * 128 + hh * 64, 64), :, :]),
                 r=[K("qk_full")], w=[K("QAq")], dma="qa")
            P.op("dve", lambda e: e.tensor_reduce(out=km[0:64, :], in_=KA[0:64, :].rearrange("p (n k) -> p n k", k=256),
                                                  axis=AX.X, op=ALU.add), r=[K("KA")], w=[K("km")])
            P.op("dve", lambda e: e.tensor_copy(out=kmh[0:64, :], in_=km[0:64, :]), r=[K("km")], w=[K("kmh")])
            P.op("dve", lambda e: e.tensor_copy(out=kmt[0:64, :], in_=kmh[0:64, :]), r=[K("kmh")], w=[K("kmt")])
            P.op("dve", lambda e: e.tensor_tensor(out=kmt[0:64, :], in0=km[0:64, :], in1=kmt[0:64, :], op=ALU.subtract),
                 r=[K("km"), K("kmt")], w=[K("kmt")])
            P.op("dve", lambda e: e.tensor_copy(out=kml[0:64, :], in_=kmt[0:64, :]), r=[K("kmt")], w=[K("kml")])
            for i in range(2):
                P.op("pool", lambda e, i=i: e.memset(gsb[i][:, :], -1e30), w=[K("gsb", i)])
            for qc in range(2 * NJ):
                J = qc // 2
                g = qc % 2
                if J >= 4:
                    P.op("pe", lambda e, qc=qc: e.matmul(ps[4][:, 0:64], lhsT=QA[0:64, qc * 128:(qc + 1) * 128],
                                                         rhs=kmh[0:64, :], start=True, stop=False),
                         r=[K("QAq"), K("kmh")], w=[K("ps", 4)])
                    P.op("pe", lambda e, qc=qc: e.matmul(ps[4][:, 0:64], lhsT=QA[0:64, qc * 128:(qc + 1) * 128],
                                                         rhs=kml[0:64, :], start=False, stop=True),
                         r=[K("QAq"), K("kml")], w=[K("ps", 4)])
                    P.op("dve", lambda e, g=g, J=J: e.tensor_copy(out=gsb[g][:, 0:J], in_=ps[4][:, 0:J]),
                         r=[K("ps", 4)], w=[K("gsb", g)])
                    P.op("dve", lambda e, g=g, J=J: e.max(out=m8[g][:, :], in_=gsb[g][:, 0:max(J, 8)]),
                         r=[K("gsb", g)], w=[K("m8", g)])
                    P.op("dve", lambda e, g=g: e.tensor_scalar(
                        out=mbw[g][:, 64:128], in0=gsb[g][:, :], scalar1=m8[g][:, 2:3], scalar2=MBIG,
                        op0=ALU.is_ge, op1=ALU.mult), r=[K("gsb", g), K("m8", g)], w=[K("mbw", g)])
                    P.op("dve", lambda e, g=g, J=J: e.memset(mbw[g][:, 64 + J:65 + J], MBIG), r=[], w=[K("mbw", g)])
                else:
                    P.op("dve", lambda e, g=g: e.memset(mbw[g][:, 64:128], 0.0), w=[K("mbw", g)])
                    P.op("dve", lambda e, g=g, J=J: e.memset(mbw[g][:, 64:65 + J], MBIG), w=[K("mbw", g)])
                P.op("pe", lambda e, g=g: e.transpose(out=ps[5][:, 0:128], in_=mbw[g][:, :], identity=ident_f[:, :]),
                     r=[K("mbw", g), K("identf")], w=[K("ps", 5)])
                P.op("act", lambda e, qc=qc: e.activation(
                    out=QA[64:128, qc * 128:(qc + 1) * 128], in_=ps[5][64:128, 0:128], func=AF.Identity,
                    bias=negb[64:128, 0:1], scale=1.0), r=[K("ps", 5), K("negb")], w=[K("QAm", qc)])
            vo = 0 if hh == 0 else 65
            dcol = 0 if hh == 0 else 64
            v0 = 1 if hh == 0 else 0
            units = [(J, n) for J in range(NJ) for n in range(J + 1)]

            pairs = [units[i:i + 2] for i in range(0, len(units), 2)]

            def emit_qk(p):
                Dp = PQ[p % 2]
                bank = 2 * (p % 2)
                pj = p % 3
                for ui, (J, n) in enumerate(pairs[p]):
                    kind = 1 if n == J else (0 if n == J - 1 else -1)
                    for kh in range(2):
                        c0 = ui * 512 + kh * 256
                        P.op("pe", lambda e, n=n, kh=kh, J=J, Dp=Dp, c0=c0, kind=kind: e.matmul(
                            Dp[:, c0:c0 + 256], lhsT=KA[:, n * 256 + kh * 128:n * 256 + (kh + 1) * 128],
                            rhs=QA[:, J * 256:(J + 1) * 256], start=True, stop=(kind < 0)),
                             r=[K("KA"), K("KAind"), K("QAq"), K("QAm", 2 * J), K("QAm", 2 * J + 1)],
                             w=[K("ps", bank + ui)])
                        if kind >= 0:
                            ti = hh * 4 + kind * 2 + kh
                            P.op("pe", lambda e, Dp=Dp, c0=c0, ti=ti: e.matmul(
                                Dp[:, c0:c0 + 256], lhsT=ident_b[:, :], rhs=Tb[:, ti, :],
                                start=False, stop=True), r=[K("identb"), K("Tb", ti)], w=[K("ps", bank + ui)])
                wd = 512 * len(pairs[p])
                P.op("act", lambda e, Dp=Dp, pj=pj, wd=wd: e.activation(out=PT[pj][:, 0:wd], in_=Dp[:, 0:wd], func=AF.Exp,
                                                                       scale=0.125),
                     r=[K("ps", bank + ui) for ui in range(len(pairs[p]))], w=[K("PT", pj)])

            def emit_pv(p):
                pj = p % 3
                for ui, (J, n) in enumerate(pairs[p]):
                    ob = 2 * (J % 2)
                    for q2 in range(2):
                        for kh in range(2):
                            c0 = ui * 512 + kh * 256 + q2 * 128
                            P.op("pe", lambda e, q2=q2, kh=kh, n=n, J=J, pj=pj, ob=ob, vo=vo, c0=c0: e.matmul(
                                ps[4 + ob + q2][:, 0:65], lhsT=PT[pj][:, c0:c0 + 128],
                                rhs=VA[:, n * 2 + kh, vo:vo + 65], start=(n == 0 and kh == 0),
                                stop=(n == J and kh == 1)),
                                 r=[K("PT", pj), K("VA"), K("VA1")], w=[K("ps", 4 + ob + q2)])
                    if n == J:
                        for q2 in range(2):
                            pb = 4 + ob + q2
                            rj = q2
                            P.op("dve", lambda e, pb=pb, rj=rj, dcol=dcol: e.reciprocal(out=rl[rj][:, :],
                                                                                       in_=ps[pb][:, dcol:dcol + 1]),
                                 r=[K("ps", pb)], w=[K("rl", rj)])
                            P.op("dve", lambda e, pb=pb, rj=rj, J=J, q2=q2, v0=v0: e.tensor_scalar(
                                out=Ost[:, 2 * J + q2, :], in0=ps[pb][:, v0:v0 + 64], scalar1=rl[rj][:, 0:1],
                                scalar2=None, op0=ALU.mult), r=[K("ps", pb), K("rl", rj)], w=[K("Ost")])

            for p in range(len(pairs) + 1):
                if p < len(pairs):
                    emit_qk(p)
                if p >= 1:
                    emit_pv(p - 1)
            dma("sp", o_loc.rearrange("(qc p) c -> p qc c", p=128)[:, 0:2 * NJ, hh * 64:(hh + 1) * 64], Ost[:, 0:2 * NJ, :],
                [K("Ost")], [K("o_loc", hh)], "ost")
        if MOBA_FAKE:
            P.op("sp", _NOP, r=[K("o_loc", 0), K("o_loc", 1)], w=[])
            return
        P.op("pool", lambda e: e.collective_compute("AllGather", ALU.bypass, replica_groups=RG,
                                                    ins=[o_loc.opt()], outs=[o_full.opt()]),
             r=[K("o_loc", 0), K("o_loc", 1)], w=[K("o_full")], dma="o_g", inc=1)
        P.barrier()

        sb.release(phase_mark)
        Om = sb.alloc("Om", [128, 4, D], BF16)
        Wo = sb.alloc("mwo", [128, 8, D], BF16)
        tmp = [sb.alloc(f"mtmp{i}", [128, 512], F32) for i in range(2)]
        gate_bcast(l, s, 1.0)
        w_o = mb_w_o.rearrange("(f p) d -> p f d", p=128)
        for f0 in range(8):
            dma("act", Wo[:, f0, :], w_o[:, f0, :], [K("moba_w_o")], [K("mwo", f0)], f"mwo{f0}")
        o_mine = nc.dram_tensor("o_mine", [NCORES, TOK, 128], BF16).ap()
        P.op("sp", lambda e: e.dma_start(
            out=o_mine, in_=o_full.rearrange("(r t) c -> r t c", r=NCORES)[:, bass.ds(pidx(e) * TOK, TOK), :]),
             r=[K("o_full")], w=[K("o_mine")], dma="omine")
        for tg in range(NT // 4):
            for a in range(4):
                dma("sp" if a % 2 == 0 else "act", Om[:, a, :].rearrange("p (r c) -> p r c", r=NCORES),
                    o_mine[:, tg * 512 + a * 128:tg * 512 + (a + 1) * 128, :].rearrange("r p c -> p r c"),
                    [K("o_mine")], [K("Om", a)], f"om{a}")
            for c in range(8):
                j = cnt["psT"] % 2
                cnt["psT"] += 1
                for tt in range(4):
                    P.op("pe", lambda e, tt=tt, c=c, j=j: e.transpose(
                        out=psTb[j][:, tt * 128:(tt + 1) * 128], in_=Om[:, tt, c * 128:(c + 1) * 128],
                        identity=ident_b[:, :]), r=[K("Om", tt), K("identb")], w=[K("psT", j)])
                if c % 2 == 0:
                    P.op("dve", lambda e, c=c, j=j, tg=tg: e.tensor_copy(out=hT[:, c, tg * 512:(tg + 1) * 512],
                                                                         in_=psTb[j][:, 0:512]),
                         r=[K("psT", j)], w=[K("hT", c, tg)])
                else:
                    P.op("act", lambda e, c=c, j=j, tg=tg: e.copy(out=hT[:, c, tg * 512:(tg + 1) * 512],
                                                                  in_=psTb[j][:, 0:512]),
                         r=[K("psT", j)], w=[K("hT", c, tg)])
        residual_matmul(hT, lambda fi, tgx: K("hT", fi, tgx), 8, Wo, lambda fi: K("mwo", fi), tmp)
        P.barrier()

    def write_out(final):
        sb.release(phase_mark)
        xnf = [sb.alloc(f"xnf{i}", [128, D], F32) for i in range(2)]
        if final:
            rms_stats()
            dma("sp", gdiag[:, :], final_norm.to_broadcast([128, D]), [], [K("gdiag", c) for c in range(8)], "fn")
        for t in range(NT):
            if final:
                j = t % 2
                P.op("dve", lambda e, t=t, j=j: e.tensor_scalar(
                    out=xnf[j][:, :], in0=X[:, t, :], scalar1=rstd[:, t:t + 1], scalar2=None, op0=ALU.mult),
                     r=[K("X", t), K("rstd")], w=[K("xnf", j)])
                P.op("pool", lambda e, t=t, j=j: e.tensor_tensor(
                    out=xnf[j][:, :], in0=xnf[j][:, :], in1=gdiag[:, :], op=ALU.mult),
                     r=[K("xnf", j)] + [K("gdiag", c) for c in range(8)], w=[K("xnf", j)])
                dma("sp", y_out[t * 128:(t + 1) * 128, :], xnf[j][:, :], [K("xnf", j)], [K("y", t)], f"y{j}")
            else:
                dma("sp", y_out[t * 128:(t + 1) * 128, :], X[:, t, :], [K("X", t)], [K("y", t)], f"y{t % 2}")
        P.op("sp", _NOP, r=[K("y", t) for t in range(NT)], w=[])

    if MOBA_FAKE:
        P.barrier()
        moba()
        return nc, P
    P.barrier()
    done = False
    stages = [("ffn", 0, 0), ("mix", 0), ("ffn", 0, 1), ("ffn", 1, 0), ("mix", 1), ("ffn", 1, 1)]
    if dbg < 99:
        if dbg >= 3:
            sb.release(phase_mark)
            norm_to_hT(0, 0)
            gate_bcast(0, 0, 0.5)
        if dbg == 4:
            for t in range(NT):
                P.op("dve", lambda e, t=t: e.tensor_copy(out=X[:, t, 0:512], in_=hT[:, t % 8, 0:512]),
                     r=[K("hT", t % 8, 0)], w=[K("X", t)])
        write_out(False)
        return nc, P
    for st in stages:
        if st == ("ffn", 1, 0):
            for s_ in range(3):
                mod_sublayer(1, s_)
        if st[0] == "ffn":
            ffn(st[1], st[2])
        elif st == ("mix", 0):
            gmlp()
        elif st == ("mix", 1):
            moba()
        if stop_after == st:
            write_out(False)
            done = True
            break
    if not done:
        write_out(True)

    return nc, P


def emit_program(nc, P):
    from contextlib import ExitStack
    with ExitStack() as stack:
        block = stack.enter_context(nc.Block())
        P.emit(nc, block, stack)
    return nc


_ID = np.eye(128, dtype=np.float32)
_SEL = (np.arange(GMD)[None, :] // 192 == np.arange(16)[:, None]).astype(np.float32)
_MASK = (np.arange(128)[:, None] <= np.arange(128)[None, :]).astype(np.float32)


def _t5_bucket(rel):
    n = np.maximum(rel, 0)
    is_small = n < 16
    nf = np.maximum(n, 16).astype(np.float32)
    large = 16 + (np.log(nf / np.float32(16)) / np.float32(math.log(128 / 16)) * np.float32(16)).astype(np.int32)
    large = np.minimum(large, 31)
    return np.where(is_small, n, large)


def _moba_consts(rel_bias, core):
    rb = np.asarray(rel_bias, np.float32)
    kk = np.arange(128)[:, None]
    qq = np.arange(256)[None, :]
    T = np.zeros((8, 128, 256), np.float32)
    cm = np.zeros((2, 128, 256), np.float32)
    for hh in range(2):
        h = 2 * core + hh
        for kind in range(2):
            for kh in range(2):
                rel = qq - (kk + kh * 128) + (256 if kind == 0 else 0)
                T[hh * 4 + kind * 2 + kh] = rb[_t5_bucket(rel), h]
                if kind == 1:
                    cm[kh] = (rel >= 0).astype(np.float32)
    cmneg = (cm - 1.0) * 262144.0
    rb31 = np.broadcast_to(rb[31, 2 * core:2 * core + 2][None, :], (128, 2)).astype(np.float32).copy()
    return T, cm, cmneg, rb31


_IND = np.repeat(np.eye(64, dtype=np.float32), 256, axis=1).astype(ml_dtypes.bfloat16)


def make_in_maps(inputs, names=None):
    x = np.ascontiguousarray(np.asarray(inputs["x"], dtype=np.float32).reshape(SEQ, D))
    shared = {
        "c": np.ascontiguousarray(np.asarray(inputs["c"], np.float32).reshape(8, 128)),
        "mod_b": np.ascontiguousarray(np.asarray(inputs["mod_b"], np.float32).reshape(144, 128)),
        "norm_g": np.ascontiguousarray(np.asarray(inputs["norm_g"], np.float32).reshape(48, 128)),
        "final_norm": np.ascontiguousarray(np.asarray(inputs["final_norm"], np.float32).reshape(1, D)),
        "ident": _ID,
        "gmlp_w_s": np.ascontiguousarray(np.asarray(inputs["gmlp_w_s"], np.float32).reshape(16, 128, 128)),
        "gmlp_b_s": np.ascontiguousarray(np.asarray(inputs["gmlp_b_s"], np.float32).reshape(16, 128)),
        "gmlp_v_norm": np.ascontiguousarray(np.asarray(inputs["gmlp_v_norm"], np.float32).reshape(24, 128)),
        "gm_sel": _SEL,
        "gm_mask": _MASK,
    }
    big = {}
    for l in range(2):
        big[f"mod_w{l}"] = np.asarray(inputs["mod_w"], np.float32)[l]
        for s2 in range(2):
            big[f"ffn_w_in{l}{s2}"] = np.asarray(inputs["ffn_w_in"], np.float32)[l, s2]
            big[f"ffn_w_out{l}{s2}"] = np.asarray(inputs["ffn_w_out"], np.float32)[l, s2]
    big["gmlp_w_in"] = np.asarray(inputs["gmlp_w_in"], np.float32)[0]
    big["gmlp_w_out"] = np.asarray(inputs["gmlp_w_out"], np.float32)[0]
    big["moba_w_qkv"] = np.asarray(inputs["moba_w_qkv"], np.float32)[0]
    big["moba_w_o"] = np.asarray(inputs["moba_w_o"], np.float32)[0]
    in_maps = []
    for i in range(NCORES):
        m = dict(shared)
        m["x"] = np.ascontiguousarray(x[i * TOK:(i + 1) * TOK])
        T, cm, cmneg, rb31 = _moba_consts(inputs["rel_bias"], i)
        m["mb_T"], m["mb_cm"], m["mb_cmneg"], m["mb_rb31"] = T, cm, cmneg, rb31
        m["mb_ind"] = _IND
        for name, w in big.items():
            if names is not None and name not in names:
                continue
            r = w.shape[0] // NCORES
            m[name] = np.ascontiguousarray(w[i * r:(i + 1) * r])
        in_maps.append(m)
    return in_maps


def run(inputs, stop_after=None, trace=False):
    nc, P = build_program(stop_after)
    emit_program(nc, P)
    names = {t.name for t in nc.main_func.allocations if hasattr(t, "name")} if False else None
    in_maps = make_in_maps(inputs, None if stop_after is None or stop_after[0] != "dbg" else ({"mod_w0"} if stop_after[1] > 0 else set()))
    res = run_bass_kernel_spmd(nc, in_maps, core_ids=list(range(NCORES)), trace=trace)
    out = np.concatenate([np.asarray(r["y"]) for r in res.results], axis=0)
    return out.reshape(1, SEQ, D).astype(np.float32), res


def kernel(**inputs):
    out, _ = run(inputs)
    return out
```

```python
import math
import numpy as np
import ml_dtypes
import concourse.bass as bass
import concourse.mybir as mybir
from concourse.bass_utils import run_bass_kernel_spmd

F32 = mybir.dt.float32
BF16 = mybir.dt.bfloat16
AF = mybir.ActivationFunctionType
ALU = mybir.AluOpType
AX = mybir.AxisListType

import os
NOAG = bool(os.environ.get('NOAG'))
MOBA_FAKE = bool(os.environ.get('MOBA_FAKE'))
NCORES = 8
D = 1024
SEQ = 16384
TOK = SEQ // NCORES
NT = TOK // 128
DFF = 2816
NFF = DFF // 128
GMD = 3072
EPS = 1e-6
FFPARTS = [list(range(0, 6)), list(range(6, 12)), list(range(12, 17)), list(range(17, 22))]


def _NOP(eng):
    return eng.nop()


class Op:
    __slots__ = ("eng", "fn", "waits", "is_dma", "sem", "val", "needs_inc", "rank", "inc")


class Prog:
    ENGS = ("pe", "act", "dve", "pool", "sp")

    def __init__(self):
        self.ops = {e: [] for e in self.ENGS}
        self.last_w = {}
        self.readers = {}
        self.dma_cnt = {}

    def op(self, eng, fn, r=(), w=(), dma=None, inc=16):
        o = Op()
        o.eng = eng
        o.fn = fn
        o.is_dma = dma is not None
        o.needs_inc = False
        o.waits = []
        o.sem = None
        o.val = 0
        o.rank = 0
        o.inc = inc
        deps = {}
        for k in r:
            d = self.last_w.get(k)
            if d is not None:
                deps[id(d)] = (d, True)
        for k in w:
            d = self.last_w.get(k)
            if d is not None and id(d) not in deps:
                deps[id(d)] = (d, False)
            rd = self.readers.get(k)
            if rd:
                for x in rd[0].values():
                    if id(x) not in deps:
                        deps[id(x)] = (x, False)
                for x in rd[1]:
                    if id(x) not in deps:
                        deps[id(x)] = (x, False)
        for d, raw in deps.values():
            if d.is_dma:
                o.waits.append(("dma", d.sem, d.val))
            elif d.eng == eng:
                if eng != "pe":
                    d.needs_inc = True
                    o.waits.append(("eng", d))
            else:
                d.needs_inc = True
                o.waits.append(("eng", d))
        if dma is not None:
            self.dma_cnt[dma] = self.dma_cnt.get(dma, 0) + inc
            o.inc = inc
            o.sem = dma
            o.val = self.dma_cnt[dma]
        for k in w:
            self.last_w[k] = o
            self.readers[k] = [{}, []]
        for k in r:
            if k in w:
                continue
            rd = self.readers.setdefault(k, [{}, []])
            if o.is_dma:
                rd[1].append(o)
            else:
                rd[0][eng] = o
        self.ops[eng].append(o)
        return o

    def barrier(self):
        lasts = {}
        for e in self.ENGS:
            for o in reversed(self.ops[e]):
                if not o.is_dma and o.fn is not _NOP:
                    lasts[e] = o
                    break
        dmas = [o for e in self.ENGS for o in self.ops[e]
                if o.is_dma and o.inc != 1 and o.val == self.dma_cnt[o.sem]]
        for e in self.ENGS:
            o = Op()
            o.eng = e
            o.fn = _NOP
            o.is_dma = False
            o.needs_inc = False
            o.sem = None
            o.val = 0
            o.rank = 0
            o.inc = 16
            o.waits = []
            for f, d in lasts.items():
                if f != e:
                    d.needs_inc = True
                    o.waits.append(("eng", d))
            for d in dmas:
                o.waits.append(("dma", d.sem, d.val))
            self.ops[e].append(o)
        self.last_w = {k: o for k, o in self.last_w.items() if o.is_dma and o.inc == 1}
        self.readers = {}

    def emit(self, nc, block, stack):
        engsem = {e: stack.enter_context(nc.semaphore(f"s_{e}")) for e in self.ENGS}
        dmasem = {n: stack.enter_context(nc.semaphore(f"d_{n}")) for n in self.dma_cnt}
        for e in self.ENGS:
            c = 0
            for o in self.ops[e]:
                if (not o.is_dma) and o.needs_inc:
                    c += 1
                    o.rank = c
        prog = self

        def run(e, engobj):
            waited = {}
            for o in prog.ops[e]:
                red = {}
                for w in o.waits:
                    kk = ("d", w[1]) if w[0] == "dma" else ("e", w[1].eng)
                    vv = w[2] if w[0] == "dma" else w[1].rank
                    if kk not in red or vv > red[kk][0]:
                        red[kk] = (vv, w)
                for vv, w in red.values():
                    if w[0] == "dma":
                        key = ("d", w[1])
                        val = w[2]
                        sem = dmasem[w[1]]
                    else:
                        d = w[1]
                        key = ("e", d.eng)
                        val = d.rank
                        sem = engsem[d.eng]
                    if waited.get(key, 0) >= val:
                        continue
                    waited[key] = val
                    engobj.wait_ge(sem, val)
                ins = o.fn(engobj)
                if o.is_dma:
                    if o.inc == 1:
                        ins.then_inc(dmasem[o.sem])
                    else:
                        ins.then_inc(dmasem[o.sem], o.inc)
                elif o.needs_inc:
                    ins.then_inc(engsem[e], 1)

        @block.tensor
        def _(eng):
            run("pe", eng)

        @block.scalar
        def _(eng):
            run("act", eng)

        @block.vector
        def _(eng):
            run("dve", eng)

        @block.gpsimd
        def _(eng):
            run("pool", eng)

        @block.sync
        def _(eng):
            run("sp", eng)


class SBAlloc:
    def __init__(self, nc):
        self.nc = nc
        self.off = 16640
        self.n = 0
        self.peak = 0

    def alloc(self, name, shape, dtype):
        esz = 4 if dtype == F32 else 2
        size = esz
        for s in shape[1:]:
            size *= s
        off = (self.off + 63) // 64 * 64
        self.off = off + size
        self.peak = max(self.peak, self.off)
        self.n += 1
        assert self.off <= 229376, (name, self.off)
        return self.nc.alloc_sbuf_tensor_at(f"{name}_{self.n}", list(shape), dtype, offset=off)

    def mark(self):
        return self.off

    def release(self, m):
        self.off = m


def build_program(stop_after=None):
    nc = bass.Bass("TRN2", target_bir_lowering=False)
    P = Prog()
    sb = SBAlloc(nc)

    def K(name, *idx):
        return (name,) + idx

    def din(name, shape, dt=F32):
        return nc.dram_tensor(name, list(shape), dt, kind="ExternalInput").ap()

    x_in = din("x", [TOK, D])
    c_in = din("c", [8, 128])
    RG = [list(range(NCORES))]

    def gathered(name, R, C):
        if NOAG:
            return din(name, [R, C])
        sh = din(name, [R // NCORES, C])
        loc = nc.dram_tensor(name + "_loc", [R // NCORES, C], F32).ap()
        full = nc.dram_tensor(name + "_full", [R, C], F32, addr_space="Shared").ap()
        P.op("sp", lambda e: e.dma_start(out=loc, in_=sh), r=[], w=[K(name, "loc")], dma=name + "_l")
        P.op("pool", lambda e: e.collective_compute("AllGather", ALU.bypass, replica_groups=RG,
                                                    ins=[loc.opt()], outs=[full.opt()]),
             r=[K(name, "loc")], w=[K(name)], dma=name + "_g", inc=1)
        return full

    mod_b = din("mod_b", [2 * 72, 128])
    norm_g = din("norm_g", [48, 128])
    final_norm = din("final_norm", [1, D])
    mb_ind_in = din("mb_ind", [64, SEQ], BF16)
    mb_T_in = din("mb_T", [8, 128, 256])
    mb_cm_in = din("mb_cm", [2, 128, 256])
    mb_cmneg_in = din("mb_cmneg", [2, 128, 256])
    mb_rb31_in = din("mb_rb31", [128, 2])
    gm_ws_in = din("gmlp_w_s", [16, 128, 128])
    gm_bs_in = din("gmlp_b_s", [16, 128])
    gm_vn_in = din("gmlp_v_norm", [24, 128])
    gm_sel_in = din("gm_sel", [16, GMD])
    gm_mask_in = din("gm_mask", [128, 128])
    ident_in = din("ident", [128, 128])
    y_out = nc.dram_tensor("y", [TOK, D], F32, kind="ExternalOutput").ap()

    X = sb.alloc("X", [128, NT, D], F32)
    ident_f = sb.alloc("identf", [128, 128], F32)
    ident_b = sb.alloc("identb", [128, 128], BF16)
    ones_f = sb.alloc("onesf", [128, 128], F32)
    modT = sb.alloc("modT", [128, 144], F32)
    modbT = sb.alloc("modbT", [128, 144], F32)
    gT = sb.alloc("gT", [128, 48], F32)
    Acol = sb.alloc("Acol", [128, 48], F32)
    cT = sb.alloc("cT", [128, 8], F32)
    ssq = sb.alloc("ssq", [128, NT], F32)
    rstd = sb.alloc("rstd", [128, NT], F32)
    rtmp = sb.alloc("rtmp", [128, NT], F32)
    gate_b = sb.alloc("gateb", [128, D], F32)
    gdiag = sb.alloc("gdiag", [128, D], F32)
    rows = sb.alloc("rows", [128, 128], F32)
    junk = sb.alloc("junk", [128, D], BF16)

    PQ = [nc.alloc_psum_tensor(f"pq{i}", [128, 1024], F32) for i in range(4)]
    ps = [PQ[i // 2][:, (i % 2) * 512:(i % 2 + 1) * 512] for i in range(8)]
    _pq3b = PQ[3].bitcast(BF16)
    psTb = [_pq3b[:, i * 1024:(i + 1) * 1024] for i in range(2)]

    def dma(q, out, in_, r, w, sem):
        P.op(q, lambda e: e.dma_start(out=out, in_=in_), r=r, w=w, dma=sem)

    def transpose_small(rows_n, src_dram, dst, dst_key, tag):
        dma("sp", rows[0:rows_n, :], src_dram, [], [K("rows")], f"rows")
        P.op("pe", lambda e: e.transpose(out=ps[6][:, 0:rows_n], in_=rows[0:rows_n, :],
                                         identity=ident_f[0:rows_n, 0:rows_n]),
             r=[K("rows"), K("identf")], w=[K("ps", 5)])
        P.op("dve", lambda e: e.tensor_copy(out=dst, in_=ps[6][:, 0:rows_n]),
             r=[K("ps", 5)], w=[dst_key])

    dma("sp", ident_f[:, :], ident_in, [], [K("identf")], "c0")
    P.op("dve", lambda e: e.tensor_copy(out=ident_b[:, :], in_=ident_f[:, :]),
         r=[K("identf")], w=[K("identb")])
    P.op("pool", lambda e: e.memset(ones_f[:, :], 1.0), w=[K("onesf")])

    for t in range(NT):
        dma("sp" if t % 2 == 0 else "act", X[:, t, :], x_in[t * 128:(t + 1) * 128, :],
            [], [K("X", t)], f"x{t}")

    prep_mark = sb.mark()
    stF = [sb.alloc(f"stF{i}", [128, 6144], F32) for i in range(2)]
    stB = [sb.alloc(f"stB{i}", [128, 6144], BF16) for i in range(2)]
    prep_cnt = [0]

    def gathered_bf(name, R, C):
        if NOAG:
            return din(name, [R, C], BF16)
        sh = din(name, [R // NCORES, C])
        n = (R // NCORES) * C // 128
        loc = nc.dram_tensor(name + "_loc", [R // NCORES, C], BF16).ap()
        full = nc.dram_tensor(name + "_full", [R, C], BF16, addr_space="Shared").ap()
        i = prep_cnt[0] % 2
        prep_cnt[0] += 1
        shf = sh.rearrange("r c -> (r c)").rearrange("(p f) -> p f", p=128)
        locf = loc.rearrange("r c -> (r c)").rearrange("(p f) -> p f", p=128)
        dma("sp", stF[i][:, 0:n], shf, [], [K("stF", i)], f"stF{i}")
        a = (n * 9 // 20) // 64 * 64
        b = (n * 16 // 20) // 64 * 64
        P.op("act", lambda e: e.copy(out=stB[i][:, 0:a], in_=stF[i][:, 0:a]), r=[K("stF", i)], w=[K("stB", i, 0)])
        P.op("dve", lambda e: e.tensor_copy(out=stB[i][:, a:b], in_=stF[i][:, a:b]), r=[K("stF", i)], w=[K("stB", i, 1)])
        P.op("pool", lambda e: e.tensor_copy(out=stB[i][:, b:n], in_=stF[i][:, b:n]), r=[K("stF", i)], w=[K("stB", i, 2)])
        dma("act", locf, stB[i][:, 0:n], [K("stB", i, 0), K("stB", i, 1), K("stB", i, 2)], [K(name, "loc")], f"stB{i}")
        P.op("pool", lambda e: e.collective_compute("AllGather", ALU.bypass, replica_groups=RG,
                                                    ins=[loc.opt()], outs=[full.opt()]),
             r=[K(name, "loc")], w=[K(name)], dma=name + "_g", inc=1)
        return full

    dbg = stop_after[1] if (stop_after and stop_after[0] == "dbg") else 99
    mod_w = [None, None]
    ffn_w_in = {}
    ffn_w_out = {}
    mod_w[0] = gathered("mod_w0", D, 9216)
    ffn_w_in[(0, 0)] = gathered_bf("ffn_w_in00", D, 2 * DFF)
    ffn_w_out[(0, 0)] = gathered_bf("ffn_w_out00", DFF, D)
    gm_w_in = gathered_bf("gmlp_w_in", D, 2 * GMD)
    gm_w_out = gathered_bf("gmlp_w_out", GMD, D)
    ffn_w_in[(0, 1)] = gathered_bf("ffn_w_in01", D, 2 * DFF)
    ffn_w_out[(0, 1)] = gathered_bf("ffn_w_out01", DFF, D)
    mod_w[1] = gathered("mod_w1", D, 9216)
    ffn_w_in[(1, 0)] = gathered_bf("ffn_w_in10", D, 2 * DFF)
    ffn_w_out[(1, 0)] = gathered_bf("ffn_w_out10", DFF, D)
    mb_w_qkv = gathered_bf("moba_w_qkv", D, 3 * D)
    mb_w_o = gathered_bf("moba_w_o", D, D)
    ffn_w_in[(1, 1)] = gathered_bf("ffn_w_in11", D, 2 * DFF)
    ffn_w_out[(1, 1)] = gathered_bf("ffn_w_out11", DFF, D)
    P.barrier()
    sb.release(prep_mark)

    transpose_small(8, c_in, cT[:, :], K("cT"), "c")
    P.op("act", lambda e: e.activation(out=cT[:, :], in_=cT[:, :], func=AF.Silu),
         r=[K("cT")], w=[K("cT")])
    for half in range(2):
        transpose_small(72, mod_b[half * 72:(half + 1) * 72, :], modbT[:, half * 72:(half + 1) * 72],
                        K("modbT", half), "mb")
    transpose_small(48, norm_g, gT[:, :], K("gT"), "g")

    eps_t = sb.alloc("eps", [128, 1], F32)
    EPS_AP = eps_t[:, 0:1]
    P.op("pool", lambda e: e.memset(eps_t[:, :], EPS), w=[K("eps")])

    attn_mark = sb.mark()
    WST = [sb.alloc(f"wst{i}", [128, 8, 256], F32) for i in range(2)]
    cnt = {"wst": 0, "wp": 0, "sg": 0, "tmp": 0, "psA": 0, "psO": 0, "psT": 0, "wos": 0}

    def mod_piece(l, n):
        i = cnt["wst"] % 2
        cnt["wst"] += 1
        src = mod_w[l].rearrange("(k p) n -> p k n", p=128)[:, :, n * 256:(n + 1) * 256]
        dma("sp", WST[i][:, :, :], src, [K(f"mod_w{l}")], [K("wst", i, 0), K("wst", i, 1)], f"wst{i}a")
        for j in range(2):
            for k in range(8):
                P.op("pe", lambda e, k=k, j=j, i=i: e.matmul(
                    ps[6][:, j:j + 1], lhsT=WST[i][:, k, j * 128:(j + 1) * 128], rhs=cT[:, k:k + 1],
                    start=(k == 0), stop=(k == 7)),
                     r=[K("wst", i, j), K("cT")], w=[K("ps", 5)])
        col0 = l * 72 + 2 * n
        P.op("dve", lambda e: e.tensor_tensor(out=modT[:, col0:col0 + 2], in0=ps[6][:, 0:2],
                                              in1=modbT[:, col0:col0 + 2], op=ALU.add),
             r=[K("ps", 5), K("modbT", l)], w=[K("modT", col0), K("modT", col0 + 1)])

    def mod_sublayer(l, s):
        for n in range(12):
            mod_piece(l, s * 12 + n)
        mod_finalize(l, s)

    def mod_finalize(l, s):
        base = l * 72 + s * 24
        a0 = (l * 3 + s) * 8
        P.op("dve", lambda e: e.scalar_tensor_tensor(
            out=Acol[:, a0:a0 + 8], in0=modT[:, base + 8:base + 16], scalar=1.0,
            in1=gT[:, a0:a0 + 8], op0=ALU.add, op1=ALU.mult),
             r=[K("modT", base + 8 + i) for i in range(8)] + [K("gT")], w=[K("Acol", l, s)])

    def gate_bcast(l, s, mul):
        base = l * 72 + s * 24 + 16
        for c in range(8):
            P.op("dve", lambda e, c=c: e.tensor_scalar(
                out=gdiag[:, c * 128:(c + 1) * 128], in0=ident_f[:, :], scalar1=modT[:, base + c:base + c + 1],
                scalar2=float(mul), op0=ALU.mult, op1=ALU.mult),
                 r=[K("identf"), K("modT", base + c)], w=[K("gdiag", c)])
        for h in range(2):
            P.op("pe", lambda e, h=h: e.matmul(ps[6][:, :], lhsT=ones_f[:, :], rhs=gdiag[:, h * 512:(h + 1) * 512],
                                               start=True, stop=True),
                 r=[K("onesf")] + [K("gdiag", c) for c in range(h * 4, h * 4 + 4)], w=[K("ps", 5)])
            P.op("act", lambda e, h=h: e.copy(out=gate_b[:, h * 512:(h + 1) * 512], in_=ps[6][:, :]),
                 r=[K("ps", 5)], w=[K("gateb", h)])

    import collections
    pending = collections.deque()

    def queue_mod(l, s):
        for n in range(12):
            pending.append((lambda l=l, s=s, n=n: mod_piece(l, s * 12 + n)))
        pending.append((lambda l=l, s=s: mod_finalize(l, s)))

    def drain(k):
        for _ in range(k):
            if pending:
                pending.popleft()()

    def drain_all():
        while pending:
            pending.popleft()()

    mod_sublayer(0, 0)
    queue_mod(0, 1)
    queue_mod(0, 2)

    hT = sb.alloc("hT", [128, 8, TOK], BF16)
    xn = sb.alloc("xn", [128, 4, D], BF16)
    phase_mark = sb.mark()

    def rms_stats():
        for t in range(NT):
            P.op("act", lambda e, t=t: e.activation(out=junk[:, :], in_=X[:, t, :], func=AF.Square,
                                                    accum_out=ssq[:, t:t + 1]),
                 r=[K("X", t)], w=[K("junk"), K("ssq")])
        P.op("act", lambda e: e.activation(out=rtmp[:, :], in_=ssq[:, :], func=AF.Sqrt, scale=1.0 / D, bias=EPS_AP),
             r=[K("ssq"), K("eps")], w=[K("rtmp")])
        P.op("dve", lambda e: e.reciprocal(out=rstd[:, :], in_=rtmp[:, :]), r=[K("rtmp")], w=[K("rstd")])

    def norm_to_hT(l, s):
        rms_stats()
        for tg in range(NT // 4):
            norm_group(l, s, tg, hT, tg)

    def norm_group(l, s, tg, hT, tgd):
        a0 = (l * 3 + s) * 8
        b0 = l * 72 + s * 24
        if True:
            for tt in range(4):
                t = tg * 4 + tt
                if tt % 2 == 0:
                    P.op("dve", lambda e, t=t, tt=tt: e.tensor_scalar(
                        out=xn[:, tt, :], in0=X[:, t, :], scalar1=rstd[:, t:t + 1], scalar2=None, op0=ALU.mult),
                         r=[K("X", t), K("rstd")], w=[K("xn", tt)])
                else:
                    P.op("act", lambda e, t=t, tt=tt: e.activation(
                        out=xn[:, tt, :], in_=X[:, t, :], func=AF.Copy, scale=rstd[:, t:t + 1]),
                         r=[K("X", t), K("rstd")], w=[K("xn", tt)])
            for c in range(8):
                j = cnt["psT"] % 2
                cnt["psT"] += 1
                for tt in range(4):
                    P.op("pe", lambda e, tt=tt, c=c, j=j: e.transpose(
                        out=psTb[j][:, tt * 128:(tt + 1) * 128],
                        in_=xn[:, tt, c * 128:(c + 1) * 128], identity=ident_b[:, :]),
                         r=[K("xn", tt), K("identb")], w=[K("psT", j)])
                if c % 2 == 0:
                    P.op("dve", lambda e, c=c, j=j, tg=tgd: e.tensor_scalar(
                        out=hT[:, c, tg * 512:(tg + 1) * 512], in0=psTb[j][:, 0:512],
                        scalar1=Acol[:, a0 + c:a0 + c + 1], scalar2=modT[:, b0 + c:b0 + c + 1],
                        op0=ALU.mult, op1=ALU.add),
                         r=[K("psT", j), K("Acol", l, s), K("modT", b0 + c)], w=[K("hT", c, tgd)])
                else:
                    P.op("act", lambda e, c=c, j=j, tg=tgd: e.activation(
                        out=hT[:, c, tg * 512:(tg + 1) * 512], in_=psTb[j][:, 0:512],
                        func=AF.Identity, scale=Acol[:, a0 + c:a0 + c + 1], bias=modT[:, b0 + c:b0 + c + 1]),
                         r=[K("psT", j), K("Acol", l, s), K("modT", b0 + c)], w=[K("hT", c, tgd)])

    def residual_matmul(lhs_tile, lhs_key, nk, rhs_tile, rhs_key, tmp, tlist=None, t0=0, tk="tmp"):
        for t in (tlist if tlist is not None else range(NT)):
            tl = t - t0
            for dh in range(2):
                po = 4 + cnt["psO"] % 2
                cnt["psO"] += 1
                for fi in range(nk):
                    P.op("pe", lambda e, fi=fi, tl=tl, dh=dh, po=po: e.matmul(
                        ps[po][:, :], lhsT=(lhs_tile(fi, tl) if callable(lhs_tile) else lhs_tile[:, fi, tl * 128:(tl + 1) * 128]),
                        rhs=rhs_tile[:, fi, dh * 512:(dh + 1) * 512], start=(fi == 0), stop=(fi == nk - 1)),
                         r=[lhs_key(fi, t // 4), rhs_key(fi)], w=[K("ps", po)])
                j = cnt["tmp"] % 2
                cnt["tmp"] += 1
                P.op("dve", lambda e, po=po, j=j, dh=dh: e.tensor_tensor(
                    out=tmp[j][:, :], in0=ps[po][:, :], in1=gate_b[:, dh * 512:(dh + 1) * 512], op=ALU.mult),
                     r=[K("ps", po), K("gateb", dh)], w=[K(tk, j)])
                P.op("pool", lambda e, j=j, t=t, dh=dh: e.tensor_tensor(
                    out=X[:, t, dh * 512:(dh + 1) * 512], in0=X[:, t, dh * 512:(dh + 1) * 512], in1=tmp[j][:, :],
                    op=ALU.add),
                     r=[K(tk, j), K("X", t)], w=[K("X", t)])

    def ffn(l, s2):
        s = 0 if s2 == 0 else 2
        sb.release(phase_mark)
        aT = sb.alloc("aT", [128, 6, TOK], BF16)
        Wp = [sb.alloc(f"wp{i}", [128, 8, 256], BF16) for i in range(3)]
        Wo = sb.alloc("wo", [128, 6, D], BF16)
        sg = [sb.alloc(f"sg{i}", [128, 512], BF16) for i in range(2)]
        tmp = [sb.alloc(f"tmp{i}", [128, 512], F32) for i in range(2)]
        norm_to_hT(l, s)
        gate_bcast(l, s, 0.5)
        w_in = ffn_w_in[(l, s2)].rearrange("(k p) n -> p k n", p=128)
        w_out = ffn_w_out[(l, s2)].rearrange("(f p) d -> p f d", p=128)
        kin = K(f"ffn_w_in{l}{s2}")
        kout = K(f"ffn_w_out{l}{s2}")
        for part in FFPARTS:
            nf = len(part)
            for fi, f in enumerate(part):
                dma("act", Wo[:, fi, :], w_out[:, f, :], [kout], [K("wo", fi)], f"wo{fi}")
            for fi, f in enumerate(part):
                i = cnt["wp"] % 3
                cnt["wp"] += 1
                dma("sp", Wp[i][:, :, 0:128], w_in[:, :, f * 128:(f + 1) * 128], [kin], [K("wp", i, 0)], f"wp{i}a")
                dma("sp", Wp[i][:, :, 128:256], w_in[:, :, DFF + f * 128:DFF + (f + 1) * 128], [kin],
                    [K("wp", i, 1)], f"wp{i}b")
                for tg in range(4):
                    a = cnt["psA"] % 2
                    cnt["psA"] += 1
                    for gu in range(2):
                        pb = a * 2 + gu
                        for k in range(8):
                            P.op("pe", lambda e, k=k, i=i, gu=gu, tg=tg, pb=pb: e.matmul(
                                ps[pb][:, :], lhsT=Wp[i][:, k, gu * 128:(gu + 1) * 128],
                                rhs=hT[:, k, tg * 512:(tg + 1) * 512], start=(k == 0), stop=(k == 7)),
                                 r=[K("wp", i, gu), K("hT", k, tg)], w=[K("ps", pb)])
                    j = cnt["sg"] % 2
                    cnt["sg"] += 1
                    P.op("act", lambda e, a=a, j=j: e.activation(out=sg[j][:, :], in_=ps[a * 2][:, :], func=AF.Silu),
                         r=[K("ps", a * 2)], w=[K("sg", j)])
                    P.op("dve", lambda e, a=a, j=j, fi=fi, tg=tg: e.tensor_tensor(
                        out=aT[:, fi, tg * 512:(tg + 1) * 512], in0=sg[j][:, :], in1=ps[a * 2 + 1][:, :], op=ALU.mult),
                         r=[K("sg", j), K("ps", a * 2 + 1)], w=[K("aT", fi, tg)])
                drain(2)
            residual_matmul(aT, lambda fi, tg: K("aT", fi, tg), nf, Wo, lambda fi: K("wo", fi), tmp)
        P.barrier()

    def gmlp():
        l, s = 0, 1
        sb.release(phase_mark)
        vtok = sb.alloc("vtok", [128, 4, GMD], BF16)

        def prodT(c):
            return hT[:, c // 3, 512 + (c % 3) * 512: 512 + (c % 3 + 1) * 512]
        Wp = [sb.alloc(f"gwp{i}", [128, 8, 256], BF16) for i in range(3)]
        wsT = sb.alloc("wsT", [128, 16, 128], BF16)
        bsT = sb.alloc("bsT", [128, 24, 128], BF16)
        uT = [sb.alloc(f"uT{i}", [128, 512], BF16) for i in range(2)]
        t1 = [sb.alloc(f"t1{i}", [128, 512], F32) for i in range(2)]
        Wo = sb.alloc("gwo", [128, 6, D], BF16)
        vnT = sb.alloc("vnT", [128, 24], F32)
        ssqv = sb.alloc("ssqv", [128, 4], F32)
        rstdv = sb.alloc("rstdv", [128, 4], F32)
        hTg = hT
        transpose_small(24, gm_vn_in, vnT[:, :], K("vnT"), "vn")
        wsS = WST[0]
        dma("sp", wsS[:, :, :].rearrange("p a (b c) -> p (a b) c", b=2), gm_ws_in.rearrange("g t s -> t g s"),
            [], [K("wst", 0, 0), K("wst", 0, 1)], "wst0a")
        dma("sp", WST[1][:, 0, 0:128], gm_mask_in, [], [K("wst", 1, 0), K("wst", 1, 1)], "wst1a")
        for g in range(16):
            P.op("pe", lambda e, g=g: e.transpose(out=ps[5][:, 0:128], in_=wsS[:, g // 2, (g % 2) * 128:(g % 2 + 1) * 128],
                                                  identity=ident_f[:, :]),
                 r=[K("wst", 0, 0), K("wst", 0, 1), K("identf")], w=[K("ps", 5)])
            P.op("dve", lambda e, g=g: e.tensor_tensor(out=wsT[:, g, :], in0=ps[5][:, 0:128], in1=WST[1][:, 0, 0:128],
                                                       op=ALU.mult),
                 r=[K("ps", 5), K("wst", 1, 0)], w=[K("wsT")])
        selS = t1[0]
        dma("sp", rows[0:16, :], gm_bs_in, [], [K("rows")], "rows")
        for c in range(24):
            dma("sp", selS[0:16, 0:128], gm_sel_in[:, c * 128:(c + 1) * 128], [], [K("t1", 0)], "sel")
            P.op("pe", lambda e: e.matmul(ps[5][:, 0:128], lhsT=selS[0:16, 0:128], rhs=rows[0:16, :], start=True, stop=True),
                 r=[K("t1", 0), K("rows")], w=[K("ps", 5)])
            P.op("dve", lambda e, c=c: e.tensor_copy(out=bsT[:, c, :], in_=ps[5][:, 0:128]),
                 r=[K("ps", 5)], w=[K("bsT")])
        gate_bcast(l, s, 1.0)
        rms_stats()
        w_in = gm_w_in.rearrange("(k p) n -> p k n", p=128)
        w_out = gm_w_out.rearrange("(f p) d -> p f d", p=128)
        kin, kout = K("gmlp_w_in"), K("gmlp_w_out")
        wpc = [0]

        def load_piece(col0):
            i = wpc[0] % 3
            wpc[0] += 1
            dma("sp", Wp[i][:, :, :], w_in[:, :, col0:col0 + 256], [kin], [K("gwp", i)], f"gwp{i}")
            return i

        for tg in range(NT // 4):
            norm_group(l, s, tg, hTg, 0)
            for vb in range(12):
                i = load_piece(GMD + vb * 256)
                for tt in range(4):
                    pb = cnt["psA"] % 4
                    cnt["psA"] += 1
                    for k in range(8):
                        P.op("pe", lambda e, k=k, i=i, tt=tt, pb=pb: e.matmul(
                            ps[pb][:, 0:256], lhsT=hTg[:, k, tt * 128:(tt + 1) * 128], rhs=Wp[i][:, k, :],
                            start=(k == 0), stop=(k == 7)),
                             r=[K("gwp", i), K("hT", k, 0)], w=[K("ps", pb)])
                    P.op("act", lambda e, pb=pb, tt=tt, vb=vb: e.activation(
                        out=vtok[:, tt, vb * 256:(vb + 1) * 256], in_=ps[pb][:, 0:256], func=AF.Gelu),
                         r=[K("ps", pb)], w=[K("vtok", tt, vb)])
            for tt in range(4):
                P.op("act", lambda e, tt=tt: e.activation(out=junk[:, 0:1024], in_=vtok[:, tt, 0:1024], func=AF.Square,
                                                          accum_out=ssqv[:, tt:tt + 1]),
                     r=[K("vtok", tt, vb) for vb in range(4)], w=[K("junk"), K("ssqv", tt)])
                for part in range(1, 3):
                    P.op("act", lambda e, tt=tt, part=part: e.activation(
                        out=junk[:, 0:1024], in_=vtok[:, tt, part * 1024:(part + 1) * 1024], func=AF.Square,
                        accum_out=rtmp[:, part:part + 1]),
                         r=[K("vtok", tt, vb) for vb in range(part * 4, part * 4 + 4)], w=[K("junk"), K("rtmp")])
                    P.op("dve", lambda e, tt=tt, part=part: e.tensor_tensor(
                        out=ssqv[:, tt:tt + 1], in0=ssqv[:, tt:tt + 1], in1=rtmp[:, part:part + 1], op=ALU.add),
                         r=[K("ssqv", tt), K("rtmp")], w=[K("ssqv", tt)])
            P.op("act", lambda e: e.activation(out=rstdv[:, :], in_=ssqv[:, :], func=AF.Sqrt, scale=1.0 / GMD, bias=EPS_AP),
                 r=[K("ssqv", tt) for tt in range(4)] + [K("eps")], w=[K("rstdv")])
            P.op("dve", lambda e: e.reciprocal(out=rstdv[:, :], in_=rstdv[:, :]), r=[K("rstdv")], w=[K("rstdv")])
            for tt in range(4):
                P.op("dve", lambda e, tt=tt: e.tensor_scalar(
                    out=vtok[:, tt, :], in0=vtok[:, tt, :], scalar1=rstdv[:, tt:tt + 1], scalar2=None, op0=ALU.mult),
                     r=[K("vtok", tt, vb) for vb in range(12)] + [K("rstdv")], w=[K("vtok", tt, vb) for vb in range(12)])
            for up in range(12):
                i = load_piece(up * 256)
                for cc in range(2):
                    c = up * 2 + cc
                    pu = cnt["psA"] % 4
                    cnt["psA"] += 1
                    for k in range(8):
                        P.op("pe", lambda e, k=k, i=i, cc=cc, pu=pu: e.matmul(
                            ps[pu][:, :], lhsT=Wp[i][:, k, cc * 128:(cc + 1) * 128], rhs=hTg[:, k, 0:512],
                            start=(k == 0), stop=(k == 7)),
                             r=[K("gwp", i), K("hT", k, 0)], w=[K("ps", pu)])
                    j = cnt["sg"] % 2
                    cnt["sg"] += 1
                    P.op("act", lambda e, pu=pu, j=j: e.activation(out=uT[j][:, :], in_=ps[pu][:, :], func=AF.Gelu),
                         r=[K("ps", pu)], w=[K("uT", j)])
                    pss = cnt["psA"] % 4
                    cnt["psA"] += 1
                    f0 = c * 128
                    if c % 3 == 1:
                        segs = [(f0 // 192, 0, 64), (f0 // 192 + 1, 64, 128)]
                    else:
                        segs = [(f0 // 192, 0, 128)]
                    for tt in range(4):
                        for (g, p0, p1) in segs:
                            P.op("pe", lambda e, g=g, p0=p0, p1=p1, tt=tt, pss=pss, f0=f0: e.matmul(
                                ps[pss][p0:p1, tt * 128:(tt + 1) * 128], lhsT=vtok[:, tt, f0 + p0:f0 + p1],
                                rhs=wsT[:, g, :], start=True, stop=True),
                                 r=[K("vtok", tt, f0 // 256), K("wsT")], w=[K("ps", pss)])
                    for tt in range(4):
                        P.op("dve", lambda e, tt=tt, pss=pss, c=c, j=j: e.scalar_tensor_tensor(
                            out=t1[j][:, tt * 128:(tt + 1) * 128], in0=ps[pss][:, tt * 128:(tt + 1) * 128],
                            scalar=vnT[:, c:c + 1], in1=bsT[:, c, :], op0=ALU.mult, op1=ALU.add),
                             r=[K("ps", pss), K("vnT"), K("bsT")], w=[K("t1", j)])
                    P.op("pool", lambda e, j=j, c=c: e.tensor_tensor(out=prodT(c), in0=t1[j][:, :], in1=uT[j][:, :],
                                                                    op=ALU.mult),
                         r=[K("t1", j), K("uT", j)], w=[K("prodT", c)])
            for part in range(4):
                for fi in range(6):
                    f = part * 6 + fi
                    dma("act", Wo[:, fi, :], w_out[:, f, :], [kout], [K("gwo", fi)], f"wo{fi}")
                residual_matmul(lambda fi, tl, part=part: prodT(part * 6 + fi)[:, tl * 128:(tl + 1) * 128],
                                lambda fi, tgx, part=part: K("prodT", part * 6 + fi),
                                6, Wo, lambda fi: K("gwo", fi), t1, tlist=range(tg * 4, tg * 4 + 4), t0=tg * 4, tk="t1")
        P.barrier()

    NJ = int(os.environ.get("MOBA_NJ", "64"))
    MBIG = 262144.0

    def moba():
        l, s = 1, 1
        if not MOBA_FAKE:
            qk_loc = nc.dram_tensor("qk_loc", [2048, TOK], BF16).ap()
            qk_full = nc.dram_tensor("qk_full", [NCORES * 2048, TOK], BF16, addr_space="Shared").ap()
            v_loc = nc.dram_tensor("v_loc", [TOK, D], BF16).ap()
            v_full = nc.dram_tensor("v_full", [SEQ, D], BF16, addr_space="Shared").ap()
            o_full = nc.dram_tensor("o_full", [NCORES * SEQ, 128], BF16, addr_space="Shared").ap()
        else:
            qk_full = din("qk_full", [NCORES * 2048, TOK], BF16)
            v_full = din("v_full", [SEQ, D], BF16)
        o_loc = nc.dram_tensor("o_loc", [SEQ, 128], BF16, kind=("ExternalOutput" if MOBA_FAKE else "Internal")).ap()

        if not MOBA_FAKE:
            sb.release(phase_mark)
            Wp = [sb.alloc(f"mwp{i}", [128, 8, 256], BF16) for i in range(2)]
            stg = [sb.alloc(f"stg{i}", [128, 512], BF16) for i in range(8)]
            norm_to_hT(l, s)
            w_in = mb_w_qkv.rearrange("(k p) n -> p k n", p=128)
            kin = K("moba_w_qkv")
            stc = [0]
            loc_keys = []
            for p in range(12):
                i = p % 2
                dma("act", Wp[i][:, :, :], w_in[:, :, p * 256:(p + 1) * 256], [kin], [K("mwp", i)], f"gwp{i}")
                if p < 8:
                    for cc in range(2):
                        fc = p * 2 + cc
                        for tg in range(4):
                            pb = cnt["psA"] % 4
                            cnt["psA"] += 1
                            for k in range(8):
                                P.op("pe", lambda e, k=k, i=i, cc=cc, tg=tg, pb=pb: e.matmul(
                                    ps[pb][:, :], lhsT=Wp[i][:, k, cc * 128:(cc + 1) * 128],
                                    rhs=hT[:, k, tg * 512:(tg + 1) * 512], start=(k == 0), stop=(k == 7)),
                                     r=[K("mwp", i), K("hT", k, tg)], w=[K("ps", pb)])
                            j = stc[0] % 8
                            stc[0] += 1
                            P.op("act", lambda e, pb=pb, j=j: e.copy(out=stg[j][:, :], in_=ps[pb][:, :]),
                                 r=[K("ps", pb)], w=[K("stg", j)])
                            kk = K("qk_loc", fc, tg)
                            loc_keys.append(kk)
                            dma("sp", qk_loc[fc * 128:(fc + 1) * 128, tg * 512:(tg + 1) * 512], stg[j][:, :],
                                [K("stg", j)], [kk], f"stg{j}")
                else:
                    for t in range(NT):
                        pb = cnt["psA"] % 4
                        cnt["psA"] += 1
                        for k in range(8):
                            P.op("pe", lambda e, k=k, i=i, t=t, pb=pb: e.matmul(
                                ps[pb][:, 0:256], lhsT=hT[:, k, t * 128:(t + 1) * 128], rhs=Wp[i][:, k, :],
                                start=(k == 0), stop=(k == 7)),
                                 r=[K("mwp", i), K("hT", k, t // 4)], w=[K("ps", pb)])
                        j = stc[0] % 8
                        stc[0] += 1
                        P.op("act", lambda e, pb=pb, j=j: e.copy(out=stg[j][:, 0:256], in_=ps[pb][:, 0:256]),
                             r=[K("ps", pb)], w=[K("stg", j)])
                        kk = K("v_loc", p, t)
                        loc_keys.append(kk)
                        dma("sp", v_loc[t * 128:(t + 1) * 128, (p - 8) * 256:(p - 7) * 256], stg[j][:, 0:256],
                            [K("stg", j)], [kk], f"stg{j}")
            P.op("pool", lambda e: e.collective_compute("AllGather", ALU.bypass, replica_groups=RG,
                                                        ins=[qk_loc.opt()], outs=[qk_full.opt()]),
                 r=[k_ for k_ in loc_keys if k_[0] == "qk_loc"], w=[K("qk_full")], dma="qk_g", inc=1)
            P.op("pool", lambda e: e.collective_compute("AllGather", ALU.bypass, replica_groups=RG,
                                                        ins=[v_loc.opt()], outs=[v_full.opt()]),
                 r=[k_ for k_ in loc_keys if k_[0] == "v_loc"], w=[K("v_full")], dma="v_g", inc=1)
            P.barrier()

        sb.release(attn_mark)
        KA = sb.alloc("KA", [128, SEQ], BF16)
        QA = sb.alloc("QA", [128, SEQ], BF16)
        VA = sb.alloc("VA", [128, 128, 130], BF16)
        _m_ost = sb.mark()
        Ost = sb.alloc("Ost", [128, 128, 64], BF16)
        _m_after = sb.mark()
        sb.release(_m_ost)
        Traw = sb.alloc("Traw", [128, 256], F32)
        Tcm = sb.alloc("Tcm", [128, 2, 256], F32)
        Tcn = sb.alloc("Tcn", [128, 2, 256], F32)
        sb.release(_m_after)
        PT = [sb.alloc(f"PT{i}", [128, 1024], BF16) for i in range(2)]
        Tb = sb.alloc("Tb", [128, 8, 256], BF16)
        rb31 = sb.alloc("rb31", [128, 2], F32)
        negb = sb.alloc("negb", [128, 1], F32)
        km = sb.alloc("km", [128, 64], F32)
        kmh = sb.alloc("kmh", [128, 64], BF16)
        kml = sb.alloc("kml", [128, 64], BF16)
        kmt = sb.alloc("kmt", [128, 64], F32)
        gsb = [sb.alloc(f"gsb{i}", [128, 64], F32) for i in range(8)]
        m8 = [sb.alloc(f"m8{i}", [128, 8], F32) for i in range(8)]
        mbw = [sb.alloc(f"mbw{i}", [128, 128], BF16) for i in range(8)]
        rl = [sb.alloc(f"rl{i}", [128, 1], F32) for i in range(2)]

        pid_cache = {}

        def pidx(e):
            if id(e) not in pid_cache:
                pid_cache[id(e)] = e.snap(e.partition_id())
            return pid_cache[id(e)]

        dma("sp", KA[64:128, :], mb_ind_in, [], [K("KAind")], "kaind")
        P.op("pool", lambda e: e.memset(VA[:, :, 0:1], 1.0), w=[K("VA1")])
        P.op("pool", lambda e: e.memset(VA[:, :, 129:130], 1.0), w=[K("VA1")])
        P.op("pool", lambda e: e.memset(negb[:, :], -MBIG), w=[K("negb")])
        for i in range(8):
            P.op("pool", lambda e, i=i: e.memset(mbw[i][:, :], 0.0), w=[K("mbw", i)])
        dma("sp", rb31[:, :], mb_rb31_in, [], [K("rb31")], "rb31")
        dma("sp", Tcm[:, :, :], mb_cm_in.rearrange("a p q -> p a q"), [], [K("Tcm")], "tcm")
        dma("sp", Tcn[:, :, :], mb_cmneg_in.rearrange("a p q -> p a q"), [], [K("Tcn")], "tcn")
        P.op("sp", lambda e: e.dma_start(
            out=VA[:, :, 1:129],
            in_=v_full.rearrange("(hb p) c -> p hb c", p=128)[:, :, bass.ds(pidx(e) * 128, 128)]),
             r=[K("v_full")], w=[K("VA")], dma="va")
        for ti in range(8):
            hh, kind, kh = ti // 4, (ti // 2) % 2, ti % 2
            dma("sp", Traw[:, :], mb_T_in[ti], [], [K("Traw")], "traw")
            if kind == 0:
                P.op("dve", lambda e, ti=ti, hh=hh: e.tensor_scalar(
                    out=Tb[:, ti, :], in0=Traw[:, :], scalar1=rb31[:, hh:hh + 1], scalar2=8.0,
                    op0=ALU.subtract, op1=ALU.mult), r=[K("Traw"), K("rb31")], w=[K("Tb", ti), K("Ost")])
            else:
                P.op("dve", lambda e, ti=ti, hh=hh: e.tensor_scalar(
                    out=Traw[:, :], in0=Traw[:, :], scalar1=rb31[:, hh:hh + 1], scalar2=8.0,
                    op0=ALU.subtract, op1=ALU.mult), r=[K("Traw"), K("rb31")], w=[K("Traw")])
                P.op("dve", lambda e, kh=kh: e.tensor_tensor(out=Traw[:, :], in0=Traw[:, :], in1=Tcm[:, kh, :], op=ALU.mult),
                     r=[K("Traw"), K("Tcm")], w=[K("Traw")])
                P.op("dve", lambda e, ti=ti, kh=kh: e.tensor_tensor(out=Tb[:, ti, :], in0=Traw[:, :], in1=Tcn[:, kh, :],
                                                                  op=ALU.add),
                     r=[K("Traw"), K("Tcn")], w=[K("Tb", ti), K("Ost")])

        qk_v = qk_full.rearrange("(r f) t -> f r t", f=2048)
        for hh in range(2):
            P.op("sp", lambda e, hh=hh: e.dma_start(
                out=KA[0:64, :].rearrange("p (r t) -> p r t", r=NCORES),
                in_=qk_v[bass.ds(1024 + pidx(e) * 128 + hh * 64, 64), :, :]),
                 r=[K("qk_full")], w=[K("KA")], dma="ka")
            P.op("act", lambda e, hh=hh: e.dma_start(
                out=QA[0:64, :].rearrange("p (r t) -> p r t", r=NCORES),
                in_=qk_v[bass.ds(pidx(e) * 128 + hh * 64, 64), :, :]),
                 r=[K("qk_full")], w=[K("QAq")], dma="qa")
            P.op("dve", lambda e: e.tensor_reduce(out=km[0:64, :], in_=KA[0:64, :].rearrange("p (n k) -> p n k", k=256),
                                                  axis=AX.X, op=ALU.add), r=[K("KA")], w=[K("km")])
            P.op("dve", lambda e: e.tensor_copy(out=kmh[0:64, :], in_=km[0:64, :]), r=[K("km")], w=[K("kmh")])
            P.op("dve", lambda e: e.tensor_copy(out=kmt[0:64, :], in_=kmh[0:64, :]), r=[K("kmh")], w=[K("kmt")])
            P.op("dve", lambda e: e.tensor_tensor(out=kmt[0:64, :], in0=km[0:64, :], in1=kmt[0:64, :], op=ALU.subtract),
                 r=[K("km"), K("kmt")], w=[K("kmt")])
            P.op("dve", lambda e: e.tensor_copy(out=kml[0:64, :], in_=kmt[0:64, :]), r=[K("kmt")], w=[K("kml")])
            for st_ in range(2):
                for b_ in range(4):
                    P.op("pool", lambda e, st_=st_, b_=b_: e.memset(gsb[st_ * 4 + b_][:, :], -1e30),
                         w=[K("gsb", st_ * 4 + b_)])
            for bt in range(2 * NJ // 4):
                st_ = bt % 2
                qcs = [bt * 4 + b_ for b_ in range(4)]
                pg = 4 + st_
                pm = 6 + st_
                dyn = [qc for qc in qcs if qc // 2 >= 4]
                for b_, qc in enumerate(qcs):
                    if qc in dyn:
                        P.op("pe", lambda e, qc=qc, b_=b_, pg=pg: e.matmul(
                            ps[pg][:, b_ * 64:(b_ + 1) * 64], lhsT=QA[0:64, qc * 128:(qc + 1) * 128],
                            rhs=kmh[0:64, :], start=True, stop=False),
                             r=[K("QAq"), K("kmh")], w=[K("ps", pg)])
                        P.op("pe", lambda e, qc=qc, b_=b_, pg=pg: e.matmul(
                            ps[pg][:, b_ * 64:(b_ + 1) * 64], lhsT=QA[0:64, qc * 128:(qc + 1) * 128],
                            rhs=kml[0:64, :], start=False, stop=True),
                             r=[K("QAq"), K("kml")], w=[K("ps", pg)])
                for b_, qc in enumerate(qcs):
                    g = st_ * 4 + b_
                    J = qc // 2
                    if qc in dyn:
                        P.op("dve", lambda e, g=g, J=J, b_=b_, pg=pg: e.tensor_copy(
                            out=gsb[g][:, 0:J], in_=ps[pg][:, b_ * 64:b_ * 64 + J]),
                             r=[K("ps", pg)], w=[K("gsb", g)])
                for b_, qc in enumerate(qcs):
                    g = st_ * 4 + b_
                    J = qc // 2
                    if qc in dyn:
                        P.op("dve", lambda e, g=g, J=J: e.max(out=m8[g][:, :], in_=gsb[g][:, 0:max(J, 8)]),
                             r=[K("gsb", g)], w=[K("m8", g)])
                for b_, qc in enumerate(qcs):
                    g = st_ * 4 + b_
                    J = qc // 2
                    if qc in dyn:
                        P.op("dve", lambda e, g=g: e.tensor_scalar(
                            out=mbw[g][:, 64:128], in0=gsb[g][:, :], scalar1=m8[g][:, 2:3], scalar2=MBIG,
                            op0=ALU.is_ge, op1=ALU.mult), r=[K("gsb", g), K("m8", g)], w=[K("mbw", g)])
                        P.op("pool", lambda e, g=g, J=J: e.memset(mbw[g][:, 64 + J:65 + J], MBIG), r=[], w=[K("mbw", g)])
                    else:
                        P.op("pool", lambda e, g=g: e.memset(mbw[g][:, 64:128], 0.0), w=[K("mbw", g)])
                        P.op("pool", lambda e, g=g, J=J: e.memset(mbw[g][:, 64:65 + J], MBIG), w=[K("mbw", g)])
                for b_, qc in enumerate(qcs):
                    g = st_ * 4 + b_
                    P.op("pe", lambda e, g=g, b_=b_, st_=st_: e.transpose(out=psTb[st_][:, b_ * 128:(b_ + 1) * 128],
                                                                         in_=mbw[g][:, :], identity=ident_b[:, :]),
                         r=[K("mbw", g), K("identb")], w=[K("ps", pm)])
                q0 = qcs[0]
                P.op("act", lambda e, q0=q0, pm=pm: e.activation(
                    out=QA[64:128, q0 * 128:(q0 + 4) * 128], in_=psTb[pm - 6][64:128, 0:512], func=AF.Identity,
                    bias=negb[64:128, 0:1], scale=1.0), r=[K("ps", pm), K("negb")], w=[K("QAm", qc) for qc in qcs])
            vo = 0 if hh == 0 else 65
            dcol = 0 if hh == 0 else 64
            v0 = 1 if hh == 0 else 0
            units = [(J, n) for J in range(NJ) for n in range(J + 1)]

            pairs = [units[i:i + 2] for i in range(0, len(units), 2)]

            def emit_qk(p):
                Dp = PQ[p % 2]
                bank = 2 * (p % 2)
                pj = p % 2
                for ui, (J, n) in enumerate(pairs[p]):
                    kind = 1 if n == J else (0 if n == J - 1 else -1)
                    for kh in range(2):
                        c0 = ui * 512 + kh * 256
                        P.op("pe", lambda e, n=n, kh=kh, J=J, Dp=Dp, c0=c0, kind=kind: e.matmul(
                            Dp[:, c0:c0 + 256], lhsT=KA[:, n * 256 + kh * 128:n * 256 + (kh + 1) * 128],
                            rhs=QA[:, J * 256:(J + 1) * 256], start=True, stop=(kind < 0)),
                             r=[K("KA"), K("KAind"), K("QAq"), K("QAm", 2 * J), K("QAm", 2 * J + 1)],
                             w=[K("ps", bank + ui)])
                        if kind >= 0:
                            ti = hh * 4 + kind * 2 + kh
                            P.op("pe", lambda e, Dp=Dp, c0=c0, ti=ti: e.matmul(
                                Dp[:, c0:c0 + 256], lhsT=ident_b[:, :], rhs=Tb[:, ti, :],
                                start=False, stop=True), r=[K("identb"), K("Tb", ti)], w=[K("ps", bank + ui)])
                wd = 512 * len(pairs[p])
                P.op("act", lambda e, Dp=Dp, pj=pj, wd=wd: e.activation(out=PT[pj][:, 0:wd], in_=Dp[:, 0:wd], func=AF.Exp,
                                                                       scale=0.125),
                     r=[K("ps", bank + ui) for ui in range(len(pairs[p]))], w=[K("PT", pj)])

            def emit_pv(p):
                pj = p % 2
                for ui, (J, n) in enumerate(pairs[p]):
                    ob = 2 * (J % 2)
                    for q2 in range(2):
                        for kh in range(2):
                            c0 = ui * 512 + kh * 256 + q2 * 128
                            P.op("pe", lambda e, q2=q2, kh=kh, n=n, J=J, pj=pj, ob=ob, vo=vo, c0=c0: e.matmul(
                                ps[4 + ob + q2][:, 0:65], lhsT=PT[pj][:, c0:c0 + 128],
                                rhs=VA[:, n * 2 + kh, vo:vo + 65], start=(n == 0 and kh == 0),
                                stop=(n == J and kh == 1)),
                                 r=[K("PT", pj), K("VA"), K("VA1")], w=[K("ps", 4 + ob + q2)])
                    if n == J:
                        for q2 in range(2):
                            pb = 4 + ob + q2
                            rj = q2
                            P.op("dve", lambda e, pb=pb, rj=rj, dcol=dcol: e.reciprocal(out=rl[rj][:, :],
                                                                                       in_=ps[pb][:, dcol:dcol + 1]),
                                 r=[K("ps", pb)], w=[K("rl", rj)])
                            P.op("dve", lambda e, pb=pb, rj=rj, J=J, q2=q2, v0=v0: e.tensor_scalar(
                                out=Ost[:, 2 * J + q2, :], in0=ps[pb][:, v0:v0 + 64], scalar1=rl[rj][:, 0:1],
                                scalar2=None, op0=ALU.mult), r=[K("ps", pb), K("rl", rj)], w=[K("Ost")])

            for p in range(len(pairs) + 1):
                if p < len(pairs):
                    emit_qk(p)
                if p >= 1:
                    emit_pv(p - 1)
            dma("sp", o_loc.rearrange("(qc p) c -> p qc c", p=128)[:, 0:2 * NJ, hh * 64:(hh + 1) * 64], Ost[:, 0:2 * NJ, :],
                [K("Ost")], [K("o_loc", hh)], "ost")
        if MOBA_FAKE:
            P.op("sp", _NOP, r=[K("o_loc", 0), K("o_loc", 1)], w=[])
            return
        P.op("pool", lambda e: e.collective_compute("AllGather", ALU.bypass, replica_groups=RG,
                                                    ins=[o_loc.opt()], outs=[o_full.opt()]),
             r=[K("o_loc", 0), K("o_loc", 1)], w=[K("o_full")], dma="o_g", inc=1)
        P.barrier()

        sb.release(phase_mark)
        Om = sb.alloc("Om", [128, 4, D], BF16)
        Wo = sb.alloc("mwo", [128, 8, D], BF16)
        tmp = [sb.alloc(f"mtmp{i}", [128, 512], F32) for i in range(2)]
        gate_bcast(l, s, 1.0)
        w_o = mb_w_o.rearrange("(f p) d -> p f d", p=128)
        for f0 in range(8):
            dma("act", Wo[:, f0, :], w_o[:, f0, :], [K("moba_w_o")], [K("mwo", f0)], f"mwo{f0}")
        o_mine = nc.dram_tensor("o_mine", [NCORES, TOK, 128], BF16).ap()
        P.op("sp", lambda e: e.dma_start(
            out=o_mine, in_=o_full.rearrange("(r t) c -> r t c", r=NCORES)[:, bass.ds(pidx(e) * TOK, TOK), :]),
             r=[K("o_full")], w=[K("o_mine")], dma="omine")
        for tg in range(NT // 4):
            for a in range(4):
                dma("sp" if a % 2 == 0 else "act", Om[:, a, :].rearrange("p (r c) -> p r c", r=NCORES),
                    o_mine[:, tg * 512 + a * 128:tg * 512 + (a + 1) * 128, :].rearrange("r p c -> p r c"),
                    [K("o_mine")], [K("Om", a)], f"om{a}")
            for c in range(8):
                j = cnt["psT"] % 2
                cnt["psT"] += 1
                for tt in range(4):
                    P.op("pe", lambda e, tt=tt, c=c, j=j: e.transpose(
                        out=psTb[j][:, tt * 128:(tt + 1) * 128], in_=Om[:, tt, c * 128:(c + 1) * 128],
                        identity=ident_b[:, :]), r=[K("Om", tt), K("identb")], w=[K("psT", j)])
                if c % 2 == 0:
                    P.op("dve", lambda e, c=c, j=j, tg=tg: e.tensor_copy(out=hT[:, c, tg * 512:(tg + 1) * 512],
                                                                         in_=psTb[j][:, 0:512]),
                         r=[K("psT", j)], w=[K("hT", c, tg)])
                else:
                    P.op("act", lambda e, c=c, j=j, tg=tg: e.copy(out=hT[:, c, tg * 512:(tg + 1) * 512],
                                                                  in_=psTb[j][:, 0:512]),
                         r=[K("psT", j)], w=[K("hT", c, tg)])
        residual_matmul(hT, lambda fi, tgx: K("hT", fi, tgx), 8, Wo, lambda fi: K("mwo", fi), tmp)
        P.barrier()

    def write_out(final):
        sb.release(phase_mark)
        xnf = [sb.alloc(f"xnf{i}", [128, D], F32) for i in range(2)]
        if final:
            rms_stats()
            dma("sp", gdiag[:, :], final_norm.to_broadcast([128, D]), [], [K("gdiag", c) for c in range(8)], "fn")
        for t in range(NT):
            if final:
                j = t % 2
                P.op("dve", lambda e, t=t, j=j: e.tensor_scalar(
                    out=xnf[j][:, :], in0=X[:, t, :], scalar1=rstd[:, t:t + 1], scalar2=None, op0=ALU.mult),
                     r=[K("X", t), K("rstd")], w=[K("xnf", j)])
                P.op("pool", lambda e, t=t, j=j: e.tensor_tensor(
                    out=xnf[j][:, :], in0=xnf[j][:, :], in1=gdiag[:, :], op=ALU.mult),
                     r=[K("xnf", j)] + [K("gdiag", c) for c in range(8)], w=[K("xnf", j)])
                dma("sp", y_out[t * 128:(t + 1) * 128, :], xnf[j][:, :], [K("xnf", j)], [K("y", t)], f"y{j}")
            else:
                dma("sp", y_out[t * 128:(t + 1) * 128, :], X[:, t, :], [K("X", t)], [K("y", t)], f"y{t % 2}")
        P.op("sp", _NOP, r=[K("y", t) for t in range(NT)], w=[])

    if MOBA_FAKE:
        P.barrier()
        moba()
        return nc, P
    P.barrier()
    done = False
    stages = [("ffn", 0, 0), ("mix", 0), ("ffn", 0, 1), ("ffn", 1, 0), ("mix", 1), ("ffn", 1, 1)]
    if dbg < 99:
        if dbg >= 3:
            sb.release(phase_mark)
            norm_to_hT(0, 0)
            gate_bcast(0, 0, 0.5)
        if dbg == 4:
            for t in range(NT):
                P.op("dve", lambda e, t=t: e.tensor_copy(out=X[:, t, 0:512], in_=hT[:, t % 8, 0:512]),
                     r=[K("hT", t % 8, 0)], w=[K("X", t)])
        write_out(False)
        return nc, P
    for st in stages:
        if st == ("ffn", 0, 1):
            drain_all()
            for s_ in range(3):
                queue_mod(1, s_)
        if st == ("mix", 0) or st == ("ffn", 1, 0):
            drain_all()
        if st[0] == "ffn":
            ffn(st[1], st[2])
        elif st == ("mix", 0):
            gmlp()
        elif st == ("mix", 1):
            moba()
        if stop_after == st:
            write_out(False)
            done = True
            break
    if not done:
        write_out(True)

    return nc, P


def emit_program(nc, P):
    from contextlib import ExitStack
    with ExitStack() as stack:
        block = stack.enter_context(nc.Block())
        P.emit(nc, block, stack)
    return nc


_ID = np.eye(128, dtype=np.float32)
_SEL = (np.arange(GMD)[None, :] // 192 == np.arange(16)[:, None]).astype(np.float32)
_MASK = (np.arange(128)[:, None] <= np.arange(128)[None, :]).astype(np.float32)


def _t5_bucket(rel):
    n = np.maximum(rel, 0)
    is_small = n < 16
    nf = np.maximum(n, 16).astype(np.float32)
    large = 16 + (np.log(nf / np.float32(16)) / np.float32(math.log(128 / 16)) * np.float32(16)).astype(np.int32)
    large = np.minimum(large, 31)
    return np.where(is_small, n, large)


def _moba_consts(rel_bias, core):
    rb = np.asarray(rel_bias, np.float32)
    kk = np.arange(128)[:, None]
    qq = np.arange(256)[None, :]
    T = np.zeros((8, 128, 256), np.float32)
    cm = np.zeros((2, 128, 256), np.float32)
    for hh in range(2):
        h = 2 * core + hh
        for kind in range(2):
            for kh in range(2):
                rel = qq - (kk + kh * 128) + (256 if kind == 0 else 0)
                T[hh * 4 + kind * 2 + kh] = rb[_t5_bucket(rel), h]
                if kind == 1:
                    cm[kh] = (rel >= 0).astype(np.float32)
    cmneg = (cm - 1.0) * 262144.0
    rb31 = np.broadcast_to(rb[31, 2 * core:2 * core + 2][None, :], (128, 2)).astype(np.float32).copy()
    return T, cm, cmneg, rb31


_IND = np.repeat(np.eye(64, dtype=np.float32), 256, axis=1).astype(ml_dtypes.bfloat16)


def make_in_maps(inputs, names=None):
    x = np.ascontiguousarray(np.asarray(inputs["x"], dtype=np.float32).reshape(SEQ, D))
    shared = {
        "c": np.ascontiguousarray(np.asarray(inputs["c"], np.float32).reshape(8, 128)),
        "mod_b": np.ascontiguousarray(np.asarray(inputs["mod_b"], np.float32).reshape(144, 128)),
        "norm_g": np.ascontiguousarray(np.asarray(inputs["norm_g"], np.float32).reshape(48, 128)),
        "final_norm": np.ascontiguousarray(np.asarray(inputs["final_norm"], np.float32).reshape(1, D)),
        "ident": _ID,
        "gmlp_w_s": np.ascontiguousarray(np.asarray(inputs["gmlp_w_s"], np.float32).reshape(16, 128, 128)),
        "gmlp_b_s": np.ascontiguousarray(np.asarray(inputs["gmlp_b_s"], np.float32).reshape(16, 128)),
        "gmlp_v_norm": np.ascontiguousarray(np.asarray(inputs["gmlp_v_norm"], np.float32).reshape(24, 128)),
        "gm_sel": _SEL,
        "gm_mask": _MASK,
    }
    big = {}
    for l in range(2):
        big[f"mod_w{l}"] = np.asarray(inputs["mod_w"], np.float32)[l]
        for s2 in range(2):
            big[f"ffn_w_in{l}{s2}"] = np.asarray(inputs["ffn_w_in"], np.float32)[l, s2]
            big[f"ffn_w_out{l}{s2}"] = np.asarray(inputs["ffn_w_out"], np.float32)[l, s2]
    big["gmlp_w_in"] = np.asarray(inputs["gmlp_w_in"], np.float32)[0]
    big["gmlp_w_out"] = np.asarray(inputs["gmlp_w_out"], np.float32)[0]
    big["moba_w_qkv"] = np.asarray(inputs["moba_w_qkv"], np.float32)[0]
    big["moba_w_o"] = np.asarray(inputs["moba_w_o"], np.float32)[0]
    in_maps = []
    for i in range(NCORES):
        m = dict(shared)
        m["x"] = np.ascontiguousarray(x[i * TOK:(i + 1) * TOK])
        T, cm, cmneg, rb31 = _moba_consts(inputs["rel_bias"], i)
        m["mb_T"], m["mb_cm"], m["mb_cmneg"], m["mb_rb31"] = T, cm, cmneg, rb31
        m["mb_ind"] = _IND
        for name, w in big.items():
            if names is not None and name not in names:
                continue
            r = w.shape[0] // NCORES
            m[name] = np.ascontiguousarray(w[i * r:(i + 1) * r])
        in_maps.append(m)
    return in_maps


def run(inputs, stop_after=None, trace=False):
    nc, P = build_program(stop_after)
    emit_program(nc, P)
    names = {t.name for t in nc.main_func.allocations if hasattr(t, "name")} if False else None
    in_maps = make_in_maps(inputs, None if stop_after is None or stop_after[0] != "dbg" else ({"mod_w0"} if stop_after[1] > 0 else set()))
    res = run_bass_kernel_spmd(nc, in_maps, core_ids=list(range(NCORES)), trace=trace)
    out = np.concatenate([np.asarray(r["y"]) for r in res.results], axis=0)
    return out.reshape(1, SEQ, D).astype(np.float32), res


def kernel(**inputs):
    out, _ = run(inputs)
    return out
```

```python
import math
import numpy as np
import ml_dtypes
import concourse.bass as bass
import concourse.mybir as mybir
from concourse.bass_utils import run_bass_kernel_spmd

F32 = mybir.dt.float32
BF16 = mybir.dt.bfloat16
AF = mybir.ActivationFunctionType
ALU = mybir.AluOpType
AX = mybir.AxisListType

import os
NOAG = bool(os.environ.get('NOAG'))
MOBA_FAKE = bool(os.environ.get('MOBA_FAKE'))
NCORES = 8
D = 1024
SEQ = 16384
TOK = SEQ // NCORES
NT = TOK // 128
DFF = 2816
NFF = DFF // 128
GMD = 3072
EPS = 1e-6
FFPARTS = [list(range(0, 6)), list(range(6, 12)), list(range(12, 17)), list(range(17, 22))]


def _NOP(eng):
    return eng.nop()


class Op:
    __slots__ = ("eng", "fn", "waits", "is_dma", "sem", "val", "needs_inc", "rank", "inc")


class Prog:
    ENGS = ("pe", "act", "dve", "pool", "sp")

    def __init__(self):
        self.ops = {e: [] for e in self.ENGS}
        self.last_w = {}
        self.readers = {}
        self.dma_cnt = {}

    def op(self, eng, fn, r=(), w=(), dma=None, inc=16):
        o = Op()
        o.eng = eng
        o.fn = fn
        o.is_dma = dma is not None
        o.needs_inc = False
        o.waits = []
        o.sem = None
        o.val = 0
        o.rank = 0
        o.inc = inc
        deps = {}
        for k in r:
            d = self.last_w.get(k)
            if d is not None:
                deps[id(d)] = (d, True)
        for k in w:
            d = self.last_w.get(k)
            if d is not None and id(d) not in deps:
                deps[id(d)] = (d, False)
            rd = self.readers.get(k)
            if rd:
                for x in rd[0].values():
                    if id(x) not in deps:
                        deps[id(x)] = (x, False)
                for x in rd[1]:
                    if id(x) not in deps:
                        deps[id(x)] = (x, False)
        for d, raw in deps.values():
            if d.is_dma:
                o.waits.append(("dma", d.sem, d.val))
            elif d.eng == eng:
                if eng != "pe":
                    d.needs_inc = True
                    o.waits.append(("eng", d))
            else:
                d.needs_inc = True
                o.waits.append(("eng", d))
        if dma is not None:
            self.dma_cnt[dma] = self.dma_cnt.get(dma, 0) + inc
            o.inc = inc
            o.sem = dma
            o.val = self.dma_cnt[dma]
        for k in w:
            self.last_w[k] = o
            self.readers[k] = [{}, []]
        for k in r:
            if k in w:
                continue
            rd = self.readers.setdefault(k, [{}, []])
            if o.is_dma:
                rd[1].append(o)
            else:
                rd[0][eng] = o
        self.ops[eng].append(o)
        return o

    def barrier(self):
        lasts = {}
        for e in self.ENGS:
            for o in reversed(self.ops[e]):
                if not o.is_dma and o.fn is not _NOP:
                    lasts[e] = o
                    break
        dmas = [o for e in self.ENGS for o in self.ops[e]
                if o.is_dma and o.inc != 1 and o.val == self.dma_cnt[o.sem]]
        for e in self.ENGS:
            o = Op()
            o.eng = e
            o.fn = _NOP
            o.is_dma = False
            o.needs_inc = False
            o.sem = None
            o.val = 0
            o.rank = 0
            o.inc = 16
            o.waits = []
            for f, d in lasts.items():
                if f != e:
                    d.needs_inc = True
                    o.waits.append(("eng", d))
            for d in dmas:
                o.waits.append(("dma", d.sem, d.val))
            self.ops[e].append(o)
        self.last_w = {k: o for k, o in self.last_w.items() if o.is_dma and o.inc == 1}
        self.readers = {}

    def emit(self, nc, block, stack):
        engsem = {e: stack.enter_context(nc.semaphore(f"s_{e}")) for e in self.ENGS}
        dmasem = {n: stack.enter_context(nc.semaphore(f"d_{n}")) for n in self.dma_cnt}
        for e in self.ENGS:
            c = 0
            for o in self.ops[e]:
                if (not o.is_dma) and o.needs_inc:
                    c += 1
                    o.rank = c
        prog = self

        def run(e, engobj):
            waited = {}
            for o in prog.ops[e]:
                red = {}
                for w in o.waits:
                    kk = ("d", w[1]) if w[0] == "dma" else ("e", w[1].eng)
                    vv = w[2] if w[0] == "dma" else w[1].rank
                    if kk not in red or vv > red[kk][0]:
                        red[kk] = (vv, w)
                for vv, w in red.values():
                    if w[0] == "dma":
                        key = ("d", w[1])
                        val = w[2]
                        sem = dmasem[w[1]]
                    else:
                        d = w[1]
                        key = ("e", d.eng)
                        val = d.rank
                        sem = engsem[d.eng]
                    if waited.get(key, 0) >= val:
                        continue
                    waited[key] = val
                    engobj.wait_ge(sem, val)
                ins = o.fn(engobj)
                if o.is_dma:
                    if o.inc == 1:
                        ins.then_inc(dmasem[o.sem])
                    else:
                        ins.then_inc(dmasem[o.sem], o.inc)
                elif o.needs_inc:
                    ins.then_inc(engsem[e], 1)

        @block.tensor
        def _(eng):
            run("pe", eng)

        @block.scalar
        def _(eng):
            run("act", eng)

        @block.vector
        def _(eng):
            run("dve", eng)

        @block.gpsimd
        def _(eng):
            run("pool", eng)

        @block.sync
        def _(eng):
            run("sp", eng)


class SBAlloc:
    def __init__(self, nc):
        self.nc = nc
        self.off = 16640
        self.n = 0
        self.peak = 0

    def alloc(self, name, shape, dtype):
        esz = 4 if dtype == F32 else 2
        size = esz
        for s in shape[1:]:
            size *= s
        off = (self.off + 63) // 64 * 64
        self.off = off + size
        self.peak = max(self.peak, self.off)
        self.n += 1
        assert self.off <= 229376, (name, self.off)
        return self.nc.alloc_sbuf_tensor_at(f"{name}_{self.n}", list(shape), dtype, offset=off)

    def mark(self):
        return self.off

    def release(self, m):
        self.off = m


def build_program(stop_after=None):
    nc = bass.Bass("TRN2", target_bir_lowering=False)
    P = Prog()
    sb = SBAlloc(nc)

    def K(name, *idx):
        return (name,) + idx

    def din(name, shape, dt=F32):
        return nc.dram_tensor(name, list(shape), dt, kind="ExternalInput").ap()

    x_in = din("x", [TOK, D])
    c_in = din("c", [8, 128])
    RG = [list(range(NCORES))]

    def gathered(name, R, C):
        if NOAG:
            return din(name, [R, C])
        sh = din(name, [R // NCORES, C])
        loc = nc.dram_tensor(name + "_loc", [R // NCORES, C], F32).ap()
        full = nc.dram_tensor(name + "_full", [R, C], F32, addr_space="Shared").ap()
        P.op("sp", lambda e: e.dma_start(out=loc, in_=sh), r=[], w=[K(name, "loc")], dma=name + "_l")
        P.op("pool", lambda e: e.collective_compute("AllGather", ALU.bypass, replica_groups=RG,
                                                    ins=[loc.opt()], outs=[full.opt()]),
             r=[K(name, "loc")], w=[K(name)], dma=name + "_g", inc=1)
        return full

    mod_b = din("mod_b", [2 * 72, 128])
    norm_g = din("norm_g", [48, 128])
    final_norm = din("final_norm", [1, D])
    mb_ind_in = din("mb_ind", [64, SEQ], BF16)
    mb_T_in = din("mb_T", [8, 128, 256])
    mb_cm_in = din("mb_cm", [2, 128, 256])
    mb_cmneg_in = din("mb_cmneg", [2, 128, 256])
    mb_rb31_in = din("mb_rb31", [128, 2])
    gm_ws_in = din("gmlp_w_s", [16, 128, 128])
    gm_bs_in = din("gmlp_b_s", [16, 128])
    gm_vn_in = din("gmlp_v_norm", [24, 128])
    gm_sel_in = din("gm_sel", [16, GMD])
    gm_mask_in = din("gm_mask", [128, 128])
    ident_in = din("ident", [128, 128])
    y_out = nc.dram_tensor("y", [TOK, D], F32, kind="ExternalOutput").ap()

    X = sb.alloc("X", [128, NT, D], F32)
    ident_f = sb.alloc("identf", [128, 128], F32)
    ident_b = sb.alloc("identb", [128, 128], BF16)
    ones_f = sb.alloc("onesf", [128, 128], F32)
    modT = sb.alloc("modT", [128, 144], F32)
    modbT = sb.alloc("modbT", [128, 144], F32)
    gT = sb.alloc("gT", [128, 48], F32)
    Acol = sb.alloc("Acol", [128, 48], F32)
    cT = sb.alloc("cT", [128, 8], F32)
    ssq = sb.alloc("ssq", [128, NT], F32)
    rstd = sb.alloc("rstd", [128, NT], F32)
    rtmp = sb.alloc("rtmp", [128, NT], F32)
    gate_b = sb.alloc("gateb", [128, D], F32)
    gdiag = sb.alloc("gdiag", [128, D], F32)
    rows = sb.alloc("rows", [128, 128], F32)
    junk = sb.alloc("junk", [128, D], BF16)

    PQ = [nc.alloc_psum_tensor(f"pq{i}", [128, 1024], F32) for i in range(4)]
    ps = [PQ[i // 2][:, (i % 2) * 512:(i % 2 + 1) * 512] for i in range(8)]
    _pq3b = PQ[3].bitcast(BF16)
    psTb = [_pq3b[:, i * 1024:(i + 1) * 1024] for i in range(2)]

    def dma(q, out, in_, r, w, sem):
        P.op(q, lambda e: e.dma_start(out=out, in_=in_), r=r, w=w, dma=sem)

    def transpose_small(rows_n, src_dram, dst, dst_key, tag):
        dma("sp", rows[0:rows_n, :], src_dram, [], [K("rows")], f"rows")
        P.op("pe", lambda e: e.transpose(out=ps[5][:, 0:rows_n], in_=rows[0:rows_n, :],
                                         identity=ident_f[0:rows_n, 0:rows_n]),
             r=[K("rows"), K("identf")], w=[K("ps", 5)])
        P.op("dve", lambda e: e.tensor_copy(out=dst, in_=ps[5][:, 0:rows_n]),
             r=[K("ps", 5)], w=[dst_key])

    dma("sp", ident_f[:, :], ident_in, [], [K("identf")], "c0")
    P.op("dve", lambda e: e.tensor_copy(out=ident_b[:, :], in_=ident_f[:, :]),
         r=[K("identf")], w=[K("identb")])
    P.op("pool", lambda e: e.memset(ones_f[:, :], 1.0), w=[K("onesf")])

    for t in range(NT):
        dma("sp" if t % 2 == 0 else "act", X[:, t, :], x_in[t * 128:(t + 1) * 128, :],
            [], [K("X", t)], f"x{t}")

    prep_mark = sb.mark()
    stF = [sb.alloc(f"stF{i}", [128, 9216], F32) for i in range(2)]
    stB = [sb.alloc(f"stB{i}", [128, 9216], BF16) for i in range(2)]
    prep_cnt = [0]

    def gathered_bf(name, R, C):
        if NOAG:
            return din(name, [R, C], BF16)
        sh = din(name, [R // NCORES, C])
        n = (R // NCORES) * C // 128
        loc = nc.dram_tensor(name + "_loc", [R // NCORES, C], BF16).ap()
        full = nc.dram_tensor(name + "_full", [R, C], BF16, addr_space="Shared").ap()
        i = prep_cnt[0] % 2
        prep_cnt[0] += 1
        shf = sh.rearrange("r c -> (r c)").rearrange("(p f) -> p f", p=128)
        locf = loc.rearrange("r c -> (r c)").rearrange("(p f) -> p f", p=128)
        dma("sp", stF[i][:, 0:n], shf, [], [K("stF", i)], f"stF{i}")
        a = (n * 9 // 20) // 64 * 64
        b = (n * 16 // 20) // 64 * 64
        P.op("act", lambda e: e.copy(out=stB[i][:, 0:a], in_=stF[i][:, 0:a]), r=[K("stF", i)], w=[K("stB", i, 0)])
        P.op("dve", lambda e: e.tensor_copy(out=stB[i][:, a:b], in_=stF[i][:, a:b]), r=[K("stF", i)], w=[K("stB", i, 1)])
        P.op("pool", lambda e: e.tensor_copy(out=stB[i][:, b:n], in_=stF[i][:, b:n]), r=[K("stF", i)], w=[K("stB", i, 2)])
        dma("act", locf, stB[i][:, 0:n], [K("stB", i, 0), K("stB", i, 1), K("stB", i, 2)], [K(name, "loc")], f"stB{i}")
        P.op("pool", lambda e: e.collective_compute("AllGather", ALU.bypass, replica_groups=RG,
                                                    ins=[loc.opt()], outs=[full.opt()]),
             r=[K(name, "loc")], w=[K(name)], dma=name + "_g", inc=1)
        return full

    dbg = stop_after[1] if (stop_after and stop_after[0] == "dbg") else 99
    mod_w = [None, None]
    ffn_w_in = {}
    ffn_w_out = {}
    mod_w[0] = gathered_bf("mod_w0", D, 9216)
    ffn_w_in[(0, 0)] = gathered_bf("ffn_w_in00", D, 2 * DFF)
    ffn_w_out[(0, 0)] = gathered_bf("ffn_w_out00", DFF, D)
    gm_w_in = gathered_bf("gmlp_w_in", D, 2 * GMD)
    gm_w_out = gathered_bf("gmlp_w_out", GMD, D)
    ffn_w_in[(0, 1)] = gathered_bf("ffn_w_in01", D, 2 * DFF)
    ffn_w_out[(0, 1)] = gathered_bf("ffn_w_out01", DFF, D)
    mod_w[1] = gathered_bf("mod_w1", D, 9216)
    ffn_w_in[(1, 0)] = gathered_bf("ffn_w_in10", D, 2 * DFF)
    ffn_w_out[(1, 0)] = gathered_bf("ffn_w_out10", DFF, D)
    mb_w_qkv = gathered_bf("moba_w_qkv", D, 3 * D)
    mb_w_o = gathered_bf("moba_w_o", D, D)
    ffn_w_in[(1, 1)] = gathered_bf("ffn_w_in11", D, 2 * DFF)
    ffn_w_out[(1, 1)] = gathered_bf("ffn_w_out11", DFF, D)
    P.barrier()
    sb.release(prep_mark)

    transpose_small(8, c_in, cT[:, :], K("cT"), "c")
    P.op("act", lambda e: e.activation(out=cT[:, :], in_=cT[:, :], func=AF.Silu),
         r=[K("cT")], w=[K("cT")])
    P.op("dve", lambda e: e.tensor_copy(out=cTb[:, :], in_=cT[:, :]), r=[K("cT")], w=[K("cTb")])
    for half in range(2):
        transpose_small(72, mod_b[half * 72:(half + 1) * 72, :], modbT[:, half * 72:(half + 1) * 72],
                        K("modbT", half), "mb")
    transpose_small(48, norm_g, gT[:, :], K("gT"), "g")

    eps_t = sb.alloc("eps", [128, 1], F32)
    EPS_AP = eps_t[:, 0:1]
    P.op("pool", lambda e: e.memset(eps_t[:, :], EPS), w=[K("eps")])

    attn_mark = sb.mark()
    WSTb = [sb.alloc(f"wstb{i}", [128, 8, 256], BF16) for i in range(4)]
    cTb = sb.alloc("cTb", [128, 8], BF16)
    cnt = {"wst": 0, "wp": 0, "sg": 0, "tmp": 0, "psA": 0, "psO": 0, "psT": 0, "wos": 0}

    def mod_piece(l, n):
        i = cnt["wst"] % 4
        cnt["wst"] += 1
        src = mod_w[l].rearrange("(k p) n -> p k n", p=128)[:, :, n * 256:(n + 1) * 256]
        dma("act", WSTb[i][:, :, :], src, [K(f"mod_w{l}")], [K("wstb", i)], f"wstb{i}")
        for j in range(2):
            for k in range(8):
                P.op("pe", lambda e, k=k, j=j, i=i: e.matmul(
                    ps[5][:, j:j + 1], lhsT=WSTb[i][:, k, j * 128:(j + 1) * 128], rhs=cTb[:, k:k + 1],
                    start=(k == 0), stop=(k == 7)),
                     r=[K("wstb", i), K("cTb")], w=[K("ps", 5)])
        col0 = l * 72 + 2 * n
        P.op("dve", lambda e: e.tensor_tensor(out=modT[:, col0:col0 + 2], in0=ps[5][:, 0:2],
                                              in1=modbT[:, col0:col0 + 2], op=ALU.add),
             r=[K("ps", 5), K("modbT", l)], w=[K("modT", col0), K("modT", col0 + 1)])

    def mod_sublayer(l, s):
        for n in range(12):
            mod_piece(l, s * 12 + n)
        mod_finalize(l, s)

    def mod_finalize(l, s):
        base = l * 72 + s * 24
        a0 = (l * 3 + s) * 8
        P.op("dve", lambda e: e.scalar_tensor_tensor(
            out=Acol[:, a0:a0 + 8], in0=modT[:, base + 8:base + 16], scalar=1.0,
            in1=gT[:, a0:a0 + 8], op0=ALU.add, op1=ALU.mult),
             r=[K("modT", base + 8 + i) for i in range(8)] + [K("gT")], w=[K("Acol", l, s)])

    def gate_bcast(l, s, mul):
        base = l * 72 + s * 24 + 16
        for c in range(8):
            P.op("dve", lambda e, c=c: e.tensor_scalar(
                out=gdiag[:, c * 128:(c + 1) * 128], in0=ident_f[:, :], scalar1=modT[:, base + c:base + c + 1],
                scalar2=float(mul), op0=ALU.mult, op1=ALU.mult),
                 r=[K("identf"), K("modT", base + c)], w=[K("gdiag", c)])
        for h in range(2):
            P.op("pe", lambda e, h=h: e.matmul(ps[5][:, :], lhsT=ones_f[:, :], rhs=gdiag[:, h * 512:(h + 1) * 512],
                                               start=True, stop=True),
                 r=[K("onesf")] + [K("gdiag", c) for c in range(h * 4, h * 4 + 4)], w=[K("ps", 5)])
            P.op("act", lambda e, h=h: e.copy(out=gate_b[:, h * 512:(h + 1) * 512], in_=ps[5][:, :]),
                 r=[K("ps", 5)], w=[K("gateb", h)])

    import collections
    pending = collections.deque()

    def queue_mod(l, s):
        for n in range(12):
            pending.append((lambda l=l, s=s, n=n: mod_piece(l, s * 12 + n)))
        pending.append((lambda l=l, s=s: mod_finalize(l, s)))

    def drain(k):
        for _ in range(k):
            if pending:
                pending.popleft()()

    def drain_all():
        while pending:
            pending.popleft()()

    mod_sublayer(0, 0)
    queue_mod(0, 1)
    queue_mod(0, 2)

    hT = sb.alloc("hT", [128, 8, TOK], BF16)
    xn = sb.alloc("xn", [128, 4, D], BF16)
    phase_mark = sb.mark()

    def rms_stats():
        for t in range(NT):
            P.op("act", lambda e, t=t: e.activation(out=junk[:, :], in_=X[:, t, :], func=AF.Square,
                                                    accum_out=ssq[:, t:t + 1]),
                 r=[K("X", t)], w=[K("junk"), K("ssq")])
        P.op("act", lambda e: e.activation(out=rtmp[:, :], in_=ssq[:, :], func=AF.Sqrt, scale=1.0 / D, bias=EPS_AP),
             r=[K("ssq"), K("eps")], w=[K("rtmp")])
        P.op("dve", lambda e: e.reciprocal(out=rstd[:, :], in_=rtmp[:, :]), r=[K("rtmp")], w=[K("rstd")])

    def norm_to_hT(l, s):
        rms_stats()
        for tg in range(NT // 4):
            norm_group(l, s, tg, hT, tg)

    def norm_group(l, s, tg, hT, tgd):
        a0 = (l * 3 + s) * 8
        b0 = l * 72 + s * 24
        if True:
            for tt in range(4):
                t = tg * 4 + tt
                if tt % 2 == 0:
                    P.op("dve", lambda e, t=t, tt=tt: e.tensor_scalar(
                        out=xn[:, tt, :], in0=X[:, t, :], scalar1=rstd[:, t:t + 1], scalar2=None, op0=ALU.mult),
                         r=[K("X", t), K("rstd")], w=[K("xn", tt)])
                else:
                    P.op("act", lambda e, t=t, tt=tt: e.activation(
                        out=xn[:, tt, :], in_=X[:, t, :], func=AF.Copy, scale=rstd[:, t:t + 1]),
                         r=[K("X", t), K("rstd")], w=[K("xn", tt)])
            for c in range(8):
                j = cnt["psT"] % 2
                cnt["psT"] += 1
                for tt in range(4):
                    P.op("pe", lambda e, tt=tt, c=c, j=j: e.transpose(
                        out=psTb[j][:, tt * 128:(tt + 1) * 128],
                        in_=xn[:, tt, c * 128:(c + 1) * 128], identity=ident_b[:, :]),
                         r=[K("xn", tt), K("identb")], w=[K("psT", j)])
                if c % 2 == 0:
                    P.op("dve", lambda e, c=c, j=j, tg=tgd: e.tensor_scalar(
                        out=hT[:, c, tg * 512:(tg + 1) * 512], in0=psTb[j][:, 0:512],
                        scalar1=Acol[:, a0 + c:a0 + c + 1], scalar2=modT[:, b0 + c:b0 + c + 1],
                        op0=ALU.mult, op1=ALU.add),
                         r=[K("psT", j), K("Acol", l, s), K("modT", b0 + c)], w=[K("hT", c, tgd)])
                else:
                    P.op("act", lambda e, c=c, j=j, tg=tgd: e.activation(
                        out=hT[:, c, tg * 512:(tg + 1) * 512], in_=psTb[j][:, 0:512],
                        func=AF.Identity, scale=Acol[:, a0 + c:a0 + c + 1], bias=modT[:, b0 + c:b0 + c + 1]),
                         r=[K("psT", j), K("Acol", l, s), K("modT", b0 + c)], w=[K("hT", c, tgd)])

    def residual_matmul(lhs_tile, lhs_key, nk, rhs_tile, rhs_key, tmp, tlist=None, t0=0, tk="tmp"):
        for t in (tlist if tlist is not None else range(NT)):
            tl = t - t0
            for dh in range(2):
                po = 4 + cnt["psO"] % 2
                cnt["psO"] += 1
                for fi in range(nk):
                    P.op("pe", lambda e, fi=fi, tl=tl, dh=dh, po=po: e.matmul(
                        ps[po][:, :], lhsT=(lhs_tile(fi, tl) if callable(lhs_tile) else lhs_tile[:, fi, tl * 128:(tl + 1) * 128]),
                        rhs=rhs_tile[:, fi, dh * 512:(dh + 1) * 512], start=(fi == 0), stop=(fi == nk - 1)),
                         r=[lhs_key(fi, t // 4), rhs_key(fi)], w=[K("ps", po)])
                j = cnt["tmp"] % 2
                cnt["tmp"] += 1
                P.op("dve", lambda e, po=po, j=j, dh=dh: e.tensor_tensor(
                    out=tmp[j][:, :], in0=ps[po][:, :], in1=gate_b[:, dh * 512:(dh + 1) * 512], op=ALU.mult),
                     r=[K("ps", po), K("gateb", dh)], w=[K(tk, j)])
                P.op("pool", lambda e, j=j, t=t, dh=dh: e.tensor_tensor(
                    out=X[:, t, dh * 512:(dh + 1) * 512], in0=X[:, t, dh * 512:(dh + 1) * 512], in1=tmp[j][:, :],
                    op=ALU.add),
                     r=[K(tk, j), K("X", t)], w=[K("X", t)])

    def ffn(l, s2):
        s = 0 if s2 == 0 else 2
        sb.release(phase_mark)
        aT = sb.alloc("aT", [128, 6, TOK], BF16)
        Wp = [sb.alloc(f"wp{i}", [128, 8, 256], BF16) for i in range(3)]
        Wo = sb.alloc("wo", [128, 6, D], BF16)
        sg = [sb.alloc(f"sg{i}", [128, 512], BF16) for i in range(2)]
        tmp = [sb.alloc(f"tmp{i}", [128, 512], F32) for i in range(2)]
        norm_to_hT(l, s)
        gate_bcast(l, s, 0.5)
        w_in = ffn_w_in[(l, s2)].rearrange("(k p) n -> p k n", p=128)
        w_out = ffn_w_out[(l, s2)].rearrange("(f p) d -> p f d", p=128)
        kin = K(f"ffn_w_in{l}{s2}")
        kout = K(f"ffn_w_out{l}{s2}")
        for part in FFPARTS:
            nf = len(part)
            for fi, f in enumerate(part):
                dma("act", Wo[:, fi, :], w_out[:, f, :], [kout], [K("wo", fi)], f"wo{fi}")
            for fi, f in enumerate(part):
                i = cnt["wp"] % 3
                cnt["wp"] += 1
                dma("sp", Wp[i][:, :, 0:128], w_in[:, :, f * 128:(f + 1) * 128], [kin], [K("wp", i, 0)], f"wp{i}a")
                dma("sp", Wp[i][:, :, 128:256], w_in[:, :, DFF + f * 128:DFF + (f + 1) * 128], [kin],
                    [K("wp", i, 1)], f"wp{i}b")
                for tg in range(4):
                    a = cnt["psA"] % 2
                    cnt["psA"] += 1
                    for gu in range(2):
                        pb = a * 2 + gu
                        for k in range(8):
                            P.op("pe", lambda e, k=k, i=i, gu=gu, tg=tg, pb=pb: e.matmul(
                                ps[pb][:, :], lhsT=Wp[i][:, k, gu * 128:(gu + 1) * 128],
                                rhs=hT[:, k, tg * 512:(tg + 1) * 512], start=(k == 0), stop=(k == 7)),
                                 r=[K("wp", i, gu), K("hT", k, tg)], w=[K("ps", pb)])
                    j = cnt["sg"] % 2
                    cnt["sg"] += 1
                    P.op("act", lambda e, a=a, j=j: e.activation(out=sg[j][:, :], in_=ps[a * 2][:, :], func=AF.Silu),
                         r=[K("ps", a * 2)], w=[K("sg", j)])
                    P.op("dve", lambda e, a=a, j=j, fi=fi, tg=tg: e.tensor_tensor(
                        out=aT[:, fi, tg * 512:(tg + 1) * 512], in0=sg[j][:, :], in1=ps[a * 2 + 1][:, :], op=ALU.mult),
                         r=[K("sg", j), K("ps", a * 2 + 1)], w=[K("aT", fi, tg)])
                drain(2)
            residual_matmul(aT, lambda fi, tg: K("aT", fi, tg), nf, Wo, lambda fi: K("wo", fi), tmp)
        P.barrier()

    def gmlp():
        l, s = 0, 1
        sb.release(phase_mark)
        wsS = sb.alloc("wsS", [128, 8, 256], F32)
        mskS = sb.alloc("mskS", [128, 128], F32)
        sb.release(phase_mark)
        vtok = sb.alloc("vtok", [128, 4, GMD], BF16)

        def prodT(c):
            return hT[:, c // 3, 512 + (c % 3) * 512: 512 + (c % 3 + 1) * 512]
        Wp = [sb.alloc(f"gwp{i}", [128, 8, 256], BF16) for i in range(3)]
        wsT = sb.alloc("wsT", [128, 16, 128], BF16)
        bsT = sb.alloc("bsT", [128, 24, 128], BF16)
        uT = [sb.alloc(f"uT{i}", [128, 512], BF16) for i in range(2)]
        t1 = [sb.alloc(f"t1{i}", [128, 512], F32) for i in range(2)]
        Wo = sb.alloc("gwo", [128, 6, D], BF16)
        vnT = sb.alloc("vnT", [128, 24], F32)
        ssqv = sb.alloc("ssqv", [128, 4], F32)
        rstdv = sb.alloc("rstdv", [128, 4], F32)
        hTg = hT
        transpose_small(24, gm_vn_in, vnT[:, :], K("vnT"), "vn")

        dma("sp", wsS[:, :, :].rearrange("p a (b c) -> p (a b) c", b=2), gm_ws_in.rearrange("g t s -> t g s"),
            [], [K("wst", 0, 0), K("wst", 0, 1)], "wst0a")
        dma("sp", mskS[:, :], gm_mask_in, [], [K("wst", 1, 0), K("wst", 1, 1)], "wst1a")
        for g in range(16):
            P.op("pe", lambda e, g=g: e.transpose(out=ps[5][:, 0:128], in_=wsS[:, g // 2, (g % 2) * 128:(g % 2 + 1) * 128],
                                                  identity=ident_f[:, :]),
                 r=[K("wst", 0, 0), K("wst", 0, 1), K("identf")], w=[K("ps", 5)])
            P.op("dve", lambda e, g=g: e.tensor_tensor(out=wsT[:, g, :], in0=ps[5][:, 0:128], in1=mskS[:, :],
                                                       op=ALU.mult),
                 r=[K("ps", 5), K("wst", 1, 0)], w=[K("wsT")])
        selS = t1[0]
        dma("sp", rows[0:16, :], gm_bs_in, [], [K("rows")], "rows")
        for c in range(24):
            dma("sp", selS[0:16, 0:128], gm_sel_in[:, c * 128:(c + 1) * 128], [], [K("t1", 0)], "sel")
            P.op("pe", lambda e: e.matmul(ps[5][:, 0:128], lhsT=selS[0:16, 0:128], rhs=rows[0:16, :], start=True, stop=True),
                 r=[K("t1", 0), K("rows")], w=[K("ps", 5)])
            P.op("dve", lambda e, c=c: e.tensor_copy(out=bsT[:, c, :], in_=ps[5][:, 0:128]),
                 r=[K("ps", 5)], w=[K("bsT")])
        P.barrier()
        gate_bcast(l, s, 1.0)
        rms_stats()
        w_in = gm_w_in.rearrange("(k p) n -> p k n", p=128)
        w_out = gm_w_out.rearrange("(f p) d -> p f d", p=128)
        kin, kout = K("gmlp_w_in"), K("gmlp_w_out")
        wpc = [0]

        def load_piece(col0):
            i = wpc[0] % 3
            wpc[0] += 1
            dma("sp", Wp[i][:, :, :], w_in[:, :, col0:col0 + 256], [kin], [K("gwp", i)], f"gwp{i}")
            return i

        for tg in range(NT // 4):
            norm_group(l, s, tg, hTg, 0)
            for vb in range(12):
                i = load_piece(GMD + vb * 256)
                for tt in range(4):
                    pb = cnt["psA"] % 4
                    cnt["psA"] += 1
                    for k in range(8):
                        P.op("pe", lambda e, k=k, i=i, tt=tt, pb=pb: e.matmul(
                            ps[pb][:, 0:256], lhsT=hTg[:, k, tt * 128:(tt + 1) * 128], rhs=Wp[i][:, k, :],
                            start=(k == 0), stop=(k == 7)),
                             r=[K("gwp", i), K("hT", k, 0)], w=[K("ps", pb)])
                    P.op("act", lambda e, pb=pb, tt=tt, vb=vb: e.activation(
                        out=vtok[:, tt, vb * 256:(vb + 1) * 256], in_=ps[pb][:, 0:256], func=AF.Gelu),
                         r=[K("ps", pb)], w=[K("vtok", tt, vb)])
            for tt in range(4):
                P.op("act", lambda e, tt=tt: e.activation(out=junk[:, 0:1024], in_=vtok[:, tt, 0:1024], func=AF.Square,
                                                          accum_out=ssqv[:, tt:tt + 1]),
                     r=[K("vtok", tt, vb) for vb in range(4)], w=[K("junk"), K("ssqv", tt)])
                for part in range(1, 3):
                    P.op("act", lambda e, tt=tt, part=part: e.activation(
                        out=junk[:, 0:1024], in_=vtok[:, tt, part * 1024:(part + 1) * 1024], func=AF.Square,
                        accum_out=rtmp[:, part:part + 1]),
                         r=[K("vtok", tt, vb) for vb in range(part * 4, part * 4 + 4)], w=[K("junk"), K("rtmp")])
                    P.op("dve", lambda e, tt=tt, part=part: e.tensor_tensor(
                        out=ssqv[:, tt:tt + 1], in0=ssqv[:, tt:tt + 1], in1=rtmp[:, part:part + 1], op=ALU.add),
                         r=[K("ssqv", tt), K("rtmp")], w=[K("ssqv", tt)])
            P.op("act", lambda e: e.activation(out=rstdv[:, :], in_=ssqv[:, :], func=AF.Sqrt, scale=1.0 / GMD, bias=EPS_AP),
                 r=[K("ssqv", tt) for tt in range(4)] + [K("eps")], w=[K("rstdv")])
            P.op("dve", lambda e: e.reciprocal(out=rstdv[:, :], in_=rstdv[:, :]), r=[K("rstdv")], w=[K("rstdv")])
            for tt in range(4):
                P.op("dve", lambda e, tt=tt: e.tensor_scalar(
                    out=vtok[:, tt, :], in0=vtok[:, tt, :], scalar1=rstdv[:, tt:tt + 1], scalar2=None, op0=ALU.mult),
                     r=[K("vtok", tt, vb) for vb in range(12)] + [K("rstdv")], w=[K("vtok", tt, vb) for vb in range(12)])
            for up in range(12):
                i = load_piece(up * 256)
                for cc in range(2):
                    c = up * 2 + cc
                    pu = cnt["psA"] % 4
                    cnt["psA"] += 1
                    for k in range(8):
                        P.op("pe", lambda e, k=k, i=i, cc=cc, pu=pu: e.matmul(
                            ps[pu][:, :], lhsT=Wp[i][:, k, cc * 128:(cc + 1) * 128], rhs=hTg[:, k, 0:512],
                            start=(k == 0), stop=(k == 7)),
                             r=[K("gwp", i), K("hT", k, 0)], w=[K("ps", pu)])
                    j = cnt["sg"] % 2
                    cnt["sg"] += 1
                    P.op("act", lambda e, pu=pu, j=j: e.activation(out=uT[j][:, :], in_=ps[pu][:, :], func=AF.Gelu),
                         r=[K("ps", pu)], w=[K("uT", j)])
                    pss = cnt["psA"] % 4
                    cnt["psA"] += 1
                    f0 = c * 128
                    if c % 3 == 1:
                        segs = [(f0 // 192, 0, 64), (f0 // 192 + 1, 64, 128)]
                    else:
                        segs = [(f0 // 192, 0, 128)]
                    for tt in range(4):
                        for (g, p0, p1) in segs:
                            P.op("pe", lambda e, g=g, p0=p0, p1=p1, tt=tt, pss=pss, f0=f0: e.matmul(
                                ps[pss][p0:p1, tt * 128:(tt + 1) * 128], lhsT=vtok[:, tt, f0 + p0:f0 + p1],
                                rhs=wsT[:, g, :], start=True, stop=True),
                                 r=[K("vtok", tt, f0 // 256), K("wsT")], w=[K("ps", pss)])
                    for tt in range(4):
                        P.op("dve", lambda e, tt=tt, pss=pss, c=c, j=j: e.scalar_tensor_tensor(
                            out=t1[j][:, tt * 128:(tt + 1) * 128], in0=ps[pss][:, tt * 128:(tt + 1) * 128],
                            scalar=vnT[:, c:c + 1], in1=bsT[:, c, :], op0=ALU.mult, op1=ALU.add),
                             r=[K("ps", pss), K("vnT"), K("bsT")], w=[K("t1", j)])
                    P.op("pool", lambda e, j=j, c=c: e.tensor_tensor(out=prodT(c), in0=t1[j][:, :], in1=uT[j][:, :],
                                                                    op=ALU.mult),
                         r=[K("t1", j), K("uT", j)], w=[K("prodT", c)])
            for part in range(4):
                for fi in range(6):
                    f = part * 6 + fi
                    dma("act", Wo[:, fi, :], w_out[:, f, :], [kout], [K("gwo", fi)], f"wo{fi}")
                residual_matmul(lambda fi, tl, part=part: prodT(part * 6 + fi)[:, tl * 128:(tl + 1) * 128],
                                lambda fi, tgx, part=part: K("prodT", part * 6 + fi),
                                6, Wo, lambda fi: K("gwo", fi), t1, tlist=range(tg * 4, tg * 4 + 4), t0=tg * 4, tk="t1")
        P.barrier()

    NJ = int(os.environ.get("MOBA_NJ", "64"))
    MBIG = 262144.0

    def moba():
        l, s = 1, 1
        if not MOBA_FAKE:
            qk_loc = nc.dram_tensor("qk_loc", [2048, TOK], BF16).ap()
            qk_full = nc.dram_tensor("qk_full", [NCORES * 2048, TOK], BF16, addr_space="Shared").ap()
            v_loc = nc.dram_tensor("v_loc", [TOK, D], BF16).ap()
            v_full = nc.dram_tensor("v_full", [SEQ, D], BF16, addr_space="Shared").ap()
            o_full = nc.dram_tensor("o_full", [NCORES * SEQ, 128], BF16, addr_space="Shared").ap()
        else:
            qk_full = din("qk_full", [NCORES * 2048, TOK], BF16)
            v_full = din("v_full", [SEQ, D], BF16)
        o_loc = nc.dram_tensor("o_loc", [SEQ, 128], BF16, kind=("ExternalOutput" if MOBA_FAKE else "Internal")).ap()

        if not MOBA_FAKE:
            sb.release(phase_mark)
            Wp = [sb.alloc(f"mwp{i}", [128, 8, 256], BF16) for i in range(2)]
            stg = [sb.alloc(f"stg{i}", [128, 512], BF16) for i in range(8)]
            norm_to_hT(l, s)
            w_in = mb_w_qkv.rearrange("(k p) n -> p k n", p=128)
            kin = K("moba_w_qkv")
            stc = [0]
            loc_keys = []
            for p in range(12):
                i = p % 2
                dma("act", Wp[i][:, :, :], w_in[:, :, p * 256:(p + 1) * 256], [kin], [K("mwp", i)], f"gwp{i}")
                if p < 8:
                    for cc in range(2):
                        fc = p * 2 + cc
                        for tg in range(4):
                            pb = cnt["psA"] % 4
                            cnt["psA"] += 1
                            for k in range(8):
                                P.op("pe", lambda e, k=k, i=i, cc=cc, tg=tg, pb=pb: e.matmul(
                                    ps[pb][:, :], lhsT=Wp[i][:, k, cc * 128:(cc + 1) * 128],
                                    rhs=hT[:, k, tg * 512:(tg + 1) * 512], start=(k == 0), stop=(k == 7)),
                                     r=[K("mwp", i), K("hT", k, tg)], w=[K("ps", pb)])
                            j = stc[0] % 8
                            stc[0] += 1
                            P.op("act", lambda e, pb=pb, j=j: e.copy(out=stg[j][:, :], in_=ps[pb][:, :]),
                                 r=[K("ps", pb)], w=[K("stg", j)])
                            kk = K("qk_loc", fc, tg)
                            loc_keys.append(kk)
                            dma("sp", qk_loc[fc * 128:(fc + 1) * 128, tg * 512:(tg + 1) * 512], stg[j][:, :],
                                [K("stg", j)], [kk], f"stg{j}")
                else:
                    for t in range(NT):
                        pb = cnt["psA"] % 4
                        cnt["psA"] += 1
                        for k in range(8):
                            P.op("pe", lambda e, k=k, i=i, t=t, pb=pb: e.matmul(
                                ps[pb][:, 0:256], lhsT=hT[:, k, t * 128:(t + 1) * 128], rhs=Wp[i][:, k, :],
                                start=(k == 0), stop=(k == 7)),
                                 r=[K("mwp", i), K("hT", k, t // 4)], w=[K("ps", pb)])
                        j = stc[0] % 8
                        stc[0] += 1
                        P.op("act", lambda e, pb=pb, j=j: e.copy(out=stg[j][:, 0:256], in_=ps[pb][:, 0:256]),
                             r=[K("ps", pb)], w=[K("stg", j)])
                        kk = K("v_loc", p, t)
                        loc_keys.append(kk)
                        dma("sp", v_loc[t * 128:(t + 1) * 128, (p - 8) * 256:(p - 7) * 256], stg[j][:, 0:256],
                            [K("stg", j)], [kk], f"stg{j}")
            P.op("pool", lambda e: e.collective_compute("AllGather", ALU.bypass, replica_groups=RG,
                                                        ins=[qk_loc.opt()], outs=[qk_full.opt()]),
                 r=[k_ for k_ in loc_keys if k_[0] == "qk_loc"], w=[K("qk_full")], dma="qk_g", inc=1)
            P.op("pool", lambda e: e.collective_compute("AllGather", ALU.bypass, replica_groups=RG,
                                                        ins=[v_loc.opt()], outs=[v_full.opt()]),
                 r=[k_ for k_ in loc_keys if k_[0] == "v_loc"], w=[K("v_full")], dma="v_g", inc=1)
            P.barrier()

        sb.release(attn_mark)
        KA = sb.alloc("KA", [128, SEQ], BF16)
        QA = sb.alloc("QA", [128, SEQ], BF16)
        VA = sb.alloc("VA", [128, 128, 130], BF16)
        _m_ost = sb.mark()
        Ost = sb.alloc("Ost", [128, 128, 64], BF16)
        _m_after = sb.mark()
        sb.release(_m_ost)
        Traw = sb.alloc("Traw", [128, 256], F32)
        Tcm = sb.alloc("Tcm", [128, 2, 256], F32)
        Tcn = sb.alloc("Tcn", [128, 2, 256], F32)
        sb.release(_m_after)
        PT = [sb.alloc(f"PT{i}", [128, 1024], BF16) for i in range(2)]
        Tb = sb.alloc("Tb", [128, 8, 256], BF16)
        rb31 = sb.alloc("rb31", [128, 2], F32)
        negb = sb.alloc("negb", [128, 1], F32)
        km = sb.alloc("km", [128, 64], F32)
        kmh = sb.alloc("kmh", [128, 64], BF16)
        kml = sb.alloc("kml", [128, 64], BF16)
        kmt = sb.alloc("kmt", [128, 64], F32)
        gsb = [sb.alloc(f"gsb{i}", [128, 64], F32) for i in range(8)]
        m8 = [sb.alloc(f"m8{i}", [128, 8], F32) for i in range(8)]
        mbw = [sb.alloc(f"mbw{i}", [128, 128], BF16) for i in range(8)]
        rl = [sb.alloc(f"rl{i}", [128, 1], F32) for i in range(2)]

        pid_cache = {}

        def pidx(e):
            if id(e) not in pid_cache:
                pid_cache[id(e)] = e.snap(e.partition_id())
            return pid_cache[id(e)]

        dma("sp", KA[64:128, :], mb_ind_in, [], [K("KAind")], "kaind")
        P.op("pool", lambda e: e.memset(VA[:, :, 0:1], 1.0), w=[K("VA1")])
        P.op("pool", lambda e: e.memset(VA[:, :, 129:130], 1.0), w=[K("VA1")])
        P.op("pool", lambda e: e.memset(negb[:, :], -MBIG), w=[K("negb")])
        for i in range(8):
            P.op("pool", lambda e, i=i: e.memset(mbw[i][:, :], 0.0), w=[K("mbw", i)])
        dma("sp", rb31[:, :], mb_rb31_in, [], [K("rb31")], "rb31")
        dma("sp", Tcm[:, :, :], mb_cm_in.rearrange("a p q -> p a q"), [], [K("Tcm")], "tcm")
        dma("sp", Tcn[:, :, :], mb_cmneg_in.rearrange("a p q -> p a q"), [], [K("Tcn")], "tcn")
        P.op("sp", lambda e: e.dma_start(
            out=VA[:, :, 1:129],
            in_=v_full.rearrange("(hb p) c -> p hb c", p=128)[:, :, bass.ds(pidx(e) * 128, 128)]),
             r=[K("v_full")], w=[K("VA")], dma="va")
        for ti in range(8):
            hh, kind, kh = ti // 4, (ti // 2) % 2, ti % 2
            dma("sp", Traw[:, :], mb_T_in[ti], [], [K("Traw")], "traw")
            if kind == 0:
                P.op("dve", lambda e, ti=ti, hh=hh: e.tensor_scalar(
                    out=Tb[:, ti, :], in0=Traw[:, :], scalar1=rb31[:, hh:hh + 1], scalar2=8.0,
                    op0=ALU.subtract, op1=ALU.mult), r=[K("Traw"), K("rb31")], w=[K("Tb", ti), K("Ost")])
            else:
                P.op("dve", lambda e, ti=ti, hh=hh: e.tensor_scalar(
                    out=Traw[:, :], in0=Traw[:, :], scalar1=rb31[:, hh:hh + 1], scalar2=8.0,
                    op0=ALU.subtract, op1=ALU.mult), r=[K("Traw"), K("rb31")], w=[K("Traw")])
                P.op("dve", lambda e, kh=kh: e.tensor_tensor(out=Traw[:, :], in0=Traw[:, :], in1=Tcm[:, kh, :], op=ALU.mult),
                     r=[K("Traw"), K("Tcm")], w=[K("Traw")])
                P.op("dve", lambda e, ti=ti, kh=kh: e.tensor_tensor(out=Tb[:, ti, :], in0=Traw[:, :], in1=Tcn[:, kh, :],
                                                                  op=ALU.add),
                     r=[K("Traw"), K("Tcn")], w=[K("Tb", ti), K("Ost")])

        qk_v = qk_full.rearrange("(r f) t -> f r t", f=2048)
        for hh in range(2):
            P.op("sp", lambda e, hh=hh: e.dma_start(
                out=KA[0:64, :].rearrange("p (r t) -> p r t", r=NCORES),
                in_=qk_v[bass.ds(1024 + pidx(e) * 128 + hh * 64, 64), :, :]),
                 r=[K("qk_full")], w=[K("KA")], dma="ka")
            P.op("act", lambda e, hh=hh: e.dma_start(
                out=QA[0:64, :].rearrange("p (r t) -> p r t", r=NCORES),
                in_=qk_v[bass.ds(pidx(e) * 128 + hh * 64, 64), :, :]),
                 r=[K("qk_full")], w=[K("QAq")], dma="qa")
            P.op("dve", lambda e: e.tensor_reduce(out=km[0:64, :], in_=KA[0:64, :].rearrange("p (n k) -> p n k", k=256),
                                                  axis=AX.X, op=ALU.add), r=[K("KA")], w=[K("km")])
            P.op("dve", lambda e: e.tensor_copy(out=kmh[0:64, :], in_=km[0:64, :]), r=[K("km")], w=[K("kmh")])
            P.op("dve", lambda e: e.tensor_copy(out=kmt[0:64, :], in_=kmh[0:64, :]), r=[K("kmh")], w=[K("kmt")])
            P.op("dve", lambda e: e.tensor_tensor(out=kmt[0:64, :], in0=km[0:64, :], in1=kmt[0:64, :], op=ALU.subtract),
                 r=[K("km"), K("kmt")], w=[K("kmt")])
            P.op("dve", lambda e: e.tensor_copy(out=kml[0:64, :], in_=kmt[0:64, :]), r=[K("kmt")], w=[K("kml")])
            for st_ in range(2):
                for b_ in range(4):
                    P.op("pool", lambda e, st_=st_, b_=b_: e.memset(gsb[st_ * 4 + b_][:, :], -1e30),
                         w=[K("gsb", st_ * 4 + b_)])
            for bt in range(2 * NJ // 4):
                st_ = bt % 2
                qcs = [bt * 4 + b_ for b_ in range(4)]
                pg = 4 + st_
                pm = 6 + st_
                dyn = [qc for qc in qcs if qc // 2 >= 4]
                for b_, qc in enumerate(qcs):
                    if qc in dyn:
                        P.op("pe", lambda e, qc=qc, b_=b_, pg=pg: e.matmul(
                            ps[pg][:, b_ * 64:(b_ + 1) * 64], lhsT=QA[0:64, qc * 128:(qc + 1) * 128],
                            rhs=kmh[0:64, :], start=True, stop=False),
                             r=[K("QAq"), K("kmh")], w=[K("ps", pg)])
                        P.op("pe", lambda e, qc=qc, b_=b_, pg=pg: e.matmul(
                            ps[pg][:, b_ * 64:(b_ + 1) * 64], lhsT=QA[0:64, qc * 128:(qc + 1) * 128],
                            rhs=kml[0:64, :], start=False, stop=True),
                             r=[K("QAq"), K("kml")], w=[K("ps", pg)])
                for b_, qc in enumerate(qcs):
                    g = st_ * 4 + b_
                    J = qc // 2
                    if qc in dyn:
                        P.op("dve", lambda e, g=g, J=J, b_=b_, pg=pg: e.tensor_copy(
                            out=gsb[g][:, 0:J], in_=ps[pg][:, b_ * 64:b_ * 64 + J]),
                             r=[K("ps", pg)], w=[K("gsb", g)])
                for b_, qc in enumerate(qcs):
                    g = st_ * 4 + b_
                    J = qc // 2
                    if qc in dyn:
                        P.op("dve", lambda e, g=g, J=J: e.max(out=m8[g][:, :], in_=gsb[g][:, 0:max(J, 8)]),
                             r=[K("gsb", g)], w=[K("m8", g)])
                for b_, qc in enumerate(qcs):
                    g = st_ * 4 + b_
                    J = qc // 2
                    if qc in dyn:
                        P.op("dve", lambda e, g=g: e.tensor_scalar(
                            out=mbw[g][:, 64:128], in0=gsb[g][:, :], scalar1=m8[g][:, 2:3], scalar2=MBIG,
                            op0=ALU.is_ge, op1=ALU.mult), r=[K("gsb", g), K("m8", g)], w=[K("mbw", g)])
                        P.op("pool", lambda e, g=g, J=J: e.memset(mbw[g][:, 64 + J:65 + J], MBIG), r=[], w=[K("mbw", g)])
                    else:
                        P.op("pool", lambda e, g=g: e.memset(mbw[g][:, 64:128], 0.0), w=[K("mbw", g)])
                        P.op("pool", lambda e, g=g, J=J: e.memset(mbw[g][:, 64:65 + J], MBIG), w=[K("mbw", g)])
                for b_, qc in enumerate(qcs):
                    g = st_ * 4 + b_
                    P.op("pe", lambda e, g=g, b_=b_, st_=st_: e.transpose(out=psTb[st_][:, b_ * 128:(b_ + 1) * 128],
                                                                         in_=mbw[g][:, :], identity=ident_b[:, :]),
                         r=[K("mbw", g), K("identb")], w=[K("ps", pm)])
                q0 = qcs[0]
                P.op("act", lambda e, q0=q0, pm=pm: e.activation(
                    out=QA[64:128, q0 * 128:(q0 + 4) * 128], in_=psTb[pm - 6][64:128, 0:512], func=AF.Identity,
                    bias=negb[64:128, 0:1], scale=1.0), r=[K("ps", pm), K("negb")], w=[K("QAm", qc) for qc in qcs])
            vo = 0 if hh == 0 else 65
            dcol = 0 if hh == 0 else 64
            v0 = 1 if hh == 0 else 0
            units = [(J, n) for J in range(NJ) for n in range(J + 1)]

            pairs = [units[i:i + 2] for i in range(0, len(units), 2)]

            def emit_qk(p):
                Dp = PQ[p % 2]
                bank = 2 * (p % 2)
                pj = p % 2
                for ui, (J, n) in enumerate(pairs[p]):
                    kind = 1 if n == J else (0 if n == J - 1 else -1)
                    for kh in range(2):
                        c0 = ui * 512 + kh * 256
                        P.op("pe", lambda e, n=n, kh=kh, J=J, Dp=Dp, c0=c0, kind=kind: e.matmul(
                            Dp[:, c0:c0 + 256], lhsT=KA[:, n * 256 + kh * 128:n * 256 + (kh + 1) * 128],
                            rhs=QA[:, J * 256:(J + 1) * 256], start=True, stop=(kind < 0)),
                             r=[K("KA"), K("KAind"), K("QAq"), K("QAm", 2 * J), K("QAm", 2 * J + 1)],
                             w=[K("ps", bank + ui)])
                        if kind >= 0:
                            ti = hh * 4 + kind * 2 + kh
                            P.op("pe", lambda e, Dp=Dp, c0=c0, ti=ti: e.matmul(
                                Dp[:, c0:c0 + 256], lhsT=ident_b[:, :], rhs=Tb[:, ti, :],
                                start=False, stop=True), r=[K("identb"), K("Tb", ti)], w=[K("ps", bank + ui)])
                wd = 512 * len(pairs[p])
                P.op("act", lambda e, Dp=Dp, pj=pj, wd=wd: e.activation(out=PT[pj][:, 0:wd], in_=Dp[:, 0:wd], func=AF.Exp,
                                                                       scale=0.125),
                     r=[K("ps", bank + ui) for ui in range(len(pairs[p]))], w=[K("PT", pj)])

            def emit_pv(p):
                pj = p % 2
                for ui, (J, n) in enumerate(pairs[p]):
                    ob = 2 * (J % 2)
                    for q2 in range(2):
                        for kh in range(2):
                            c0 = ui * 512 + kh * 256 + q2 * 128
                            P.op("pe", lambda e, q2=q2, kh=kh, n=n, J=J, pj=pj, ob=ob, vo=vo, c0=c0: e.matmul(
                                ps[4 + ob + q2][:, 0:65], lhsT=PT[pj][:, c0:c0 + 128],
                                rhs=VA[:, n * 2 + kh, vo:vo + 65], start=(n == 0 and kh == 0),
                                stop=(n == J and kh == 1)),
                                 r=[K("PT", pj), K("VA"), K("VA1")], w=[K("ps", 4 + ob + q2)])
                    if n == J:
                        for q2 in range(2):
                            pb = 4 + ob + q2
                            rj = q2
                            P.op("dve", lambda e, pb=pb, rj=rj, dcol=dcol: e.reciprocal(out=rl[rj][:, :],
                                                                                       in_=ps[pb][:, dcol:dcol + 1]),
                                 r=[K("ps", pb)], w=[K("rl", rj)])
                            P.op("dve", lambda e, pb=pb, rj=rj, J=J, q2=q2, v0=v0: e.tensor_scalar(
                                out=Ost[:, 2 * J + q2, :], in0=ps[pb][:, v0:v0 + 64], scalar1=rl[rj][:, 0:1],
                                scalar2=None, op0=ALU.mult), r=[K("ps", pb), K("rl", rj)], w=[K("Ost")])

            for p in range(len(pairs) + 1):
                if p < len(pairs):
                    emit_qk(p)
                if p >= 1:
                    emit_pv(p - 1)
            dma("sp", o_loc.rearrange("(qc p) c -> p qc c", p=128)[:, 0:2 * NJ, hh * 64:(hh + 1) * 64], Ost[:, 0:2 * NJ, :],
                [K("Ost")], [K("o_loc", hh)], "ost")
        if MOBA_FAKE:
            P.op("sp", _NOP, r=[K("o_loc", 0), K("o_loc", 1)], w=[])
            return
        P.op("pool", lambda e: e.collective_compute("AllGather", ALU.bypass, replica_groups=RG,
                                                    ins=[o_loc.opt()], outs=[o_full.opt()]),
             r=[K("o_loc", 0), K("o_loc", 1)], w=[K("o_full")], dma="o_g", inc=1)
        P.barrier()

        sb.release(phase_mark)
        Om = sb.alloc("Om", [128, 4, D], BF16)
        Wo = sb.alloc("mwo", [128, 8, D], BF16)
        tmp = [sb.alloc(f"mtmp{i}", [128, 512], F32) for i in range(2)]
        gate_bcast(l, s, 1.0)
        w_o = mb_w_o.rearrange("(f p) d -> p f d", p=128)
        for f0 in range(8):
            dma("act", Wo[:, f0, :], w_o[:, f0, :], [K("moba_w_o")], [K("mwo", f0)], f"mwo{f0}")
        o_mine = nc.dram_tensor("o_mine", [NCORES, TOK, 128], BF16).ap()
        P.op("sp", lambda e: e.dma_start(
            out=o_mine, in_=o_full.rearrange("(r t) c -> r t c", r=NCORES)[:, bass.ds(pidx(e) * TOK, TOK), :]),
             r=[K("o_full")], w=[K("o_mine")], dma="omine")
        for tg in range(NT // 4):
            for a in range(4):
                dma("sp" if a % 2 == 0 else "act", Om[:, a, :].rearrange("p (r c) -> p r c", r=NCORES),
                    o_mine[:, tg * 512 + a * 128:tg * 512 + (a + 1) * 128, :].rearrange("r p c -> p r c"),
                    [K("o_mine")], [K("Om", a)], f"om{a}")
            for c in range(8):
                j = cnt["psT"] % 2
                cnt["psT"] += 1
                for tt in range(4):
                    P.op("pe", lambda e, tt=tt, c=c, j=j: e.transpose(
                        out=psTb[j][:, tt * 128:(tt + 1) * 128], in_=Om[:, tt, c * 128:(c + 1) * 128],
                        identity=ident_b[:, :]), r=[K("Om", tt), K("identb")], w=[K("psT", j)])
                if c % 2 == 0:
                    P.op("dve", lambda e, c=c, j=j, tg=tg: e.tensor_copy(out=hT[:, c, tg * 512:(tg + 1) * 512],
                                                                         in_=psTb[j][:, 0:512]),
                         r=[K("psT", j)], w=[K("hT", c, tg)])
                else:
                    P.op("act", lambda e, c=c, j=j, tg=tg: e.copy(out=hT[:, c, tg * 512:(tg + 1) * 512],
                                                                  in_=psTb[j][:, 0:512]),
                         r=[K("psT", j)], w=[K("hT", c, tg)])
        residual_matmul(hT, lambda fi, tgx: K("hT", fi, tgx), 8, Wo, lambda fi: K("mwo", fi), tmp)
        P.barrier()

    def write_out(final):
        sb.release(phase_mark)
        xnf = [sb.alloc(f"xnf{i}", [128, D], F32) for i in range(2)]
        if final:
            rms_stats()
            dma("sp", gdiag[:, :], final_norm.to_broadcast([128, D]), [], [K("gdiag", c) for c in range(8)], "fn")
        for t in range(NT):
            if final:
                j = t % 2
                P.op("dve", lambda e, t=t, j=j: e.tensor_scalar(
                    out=xnf[j][:, :], in0=X[:, t, :], scalar1=rstd[:, t:t + 1], scalar2=None, op0=ALU.mult),
                     r=[K("X", t), K("rstd")], w=[K("xnf", j)])
                P.op("pool", lambda e, t=t, j=j: e.tensor_tensor(
                    out=xnf[j][:, :], in0=xnf[j][:, :], in1=gdiag[:, :], op=ALU.mult),
                     r=[K("xnf", j)] + [K("gdiag", c) for c in range(8)], w=[K("xnf", j)])
                dma("sp", y_out[t * 128:(t + 1) * 128, :], xnf[j][:, :], [K("xnf", j)], [K("y", t)], f"y{j}")
            else:
                dma("sp", y_out[t * 128:(t + 1) * 128, :], X[:, t, :], [K("X", t)], [K("y", t)], f"y{t % 2}")
        P.op("sp", _NOP, r=[K("y", t) for t in range(NT)], w=[])

    if MOBA_FAKE:
        P.barrier()
        moba()
        return nc, P
    P.barrier()
    done = False
    stages = [("ffn", 0, 0), ("mix", 0), ("ffn", 0, 1), ("ffn", 1, 0), ("mix", 1), ("ffn", 1, 1)]
    if dbg < 99:
        if dbg >= 3:
            sb.release(phase_mark)
            norm_to_hT(0, 0)
            gate_bcast(0, 0, 0.5)
        if dbg == 4:
            for t in range(NT):
                P.op("dve", lambda e, t=t: e.tensor_copy(out=X[:, t, 0:512], in_=hT[:, t % 8, 0:512]),
                     r=[K("hT", t % 8, 0)], w=[K("X", t)])
        write_out(False)
        return nc, P
    for st in stages:
        if st == ("ffn", 0, 1):
            drain_all()
            for s_ in range(3):
                queue_mod(1, s_)
        if st == ("mix", 0) or st == ("ffn", 1, 0):
            drain_all()
        if st[0] == "ffn":
            ffn(st[1], st[2])
        elif st == ("mix", 0):
            gmlp()
        elif st == ("mix", 1):
            moba()
        if stop_after == st:
            write_out(False)
            done = True
            break
    if not done:
        write_out(True)

    return nc, P


def emit_program(nc, P):
    from contextlib import ExitStack
    with ExitStack() as stack:
        block = stack.enter_context(nc.Block())
        P.emit(nc, block, stack)
    return nc


_ID = np.eye(128, dtype=np.float32)
_SEL = (np.arange(GMD)[None, :] // 192 == np.arange(16)[:, None]).astype(np.float32)
_MASK = (np.arange(128)[:, None] <= np.arange(128)[None, :]).astype(np.float32)


def _t5_bucket(rel):
    n = np.maximum(rel, 0)
    is_small = n < 16
    nf = np.maximum(n, 16).astype(np.float32)
    large = 16 + (np.log(nf / np.float32(16)) / np.float32(math.log(128 / 16)) * np.float32(16)).astype(np.int32)
    large = np.minimum(large, 31)
    return np.where(is_small, n, large)


def _moba_consts(rel_bias, core):
    rb = np.asarray(rel_bias, np.float32)
    kk = np.arange(128)[:, None]
    qq = np.arange(256)[None, :]
    T = np.zeros((8, 128, 256), np.float32)
    cm = np.zeros((2, 128, 256), np.float32)
    for hh in range(2):
        h = 2 * core + hh
        for kind in range(2):
            for kh in range(2):
                rel = qq - (kk + kh * 128) + (256 if kind == 0 else 0)
                T[hh * 4 + kind * 2 + kh] = rb[_t5_bucket(rel), h]
                if kind == 1:
                    cm[kh] = (rel >= 0).astype(np.float32)
    cmneg = (cm - 1.0) * 262144.0
    rb31 = np.broadcast_to(rb[31, 2 * core:2 * core + 2][None, :], (128, 2)).astype(np.float32).copy()
    return T, cm, cmneg, rb31


_IND = np.repeat(np.eye(64, dtype=np.float32), 256, axis=1).astype(ml_dtypes.bfloat16)


def make_in_maps(inputs, names=None):
    x = np.ascontiguousarray(np.asarray(inputs["x"], dtype=np.float32).reshape(SEQ, D))
    shared = {
        "c": np.ascontiguousarray(np.asarray(inputs["c"], np.float32).reshape(8, 128)),
        "mod_b": np.ascontiguousarray(np.asarray(inputs["mod_b"], np.float32).reshape(144, 128)),
        "norm_g": np.ascontiguousarray(np.asarray(inputs["norm_g"], np.float32).reshape(48, 128)),
        "final_norm": np.ascontiguousarray(np.asarray(inputs["final_norm"], np.float32).reshape(1, D)),
        "ident": _ID,
        "gmlp_w_s": np.ascontiguousarray(np.asarray(inputs["gmlp_w_s"], np.float32).reshape(16, 128, 128)),
        "gmlp_b_s": np.ascontiguousarray(np.asarray(inputs["gmlp_b_s"], np.float32).reshape(16, 128)),
        "gmlp_v_norm": np.ascontiguousarray(np.asarray(inputs["gmlp_v_norm"], np.float32).reshape(24, 128)),
        "gm_sel": _SEL,
        "gm_mask": _MASK,
    }
    big = {}
    for l in range(2):
        big[f"mod_w{l}"] = np.asarray(inputs["mod_w"], np.float32)[l]
        for s2 in range(2):
            big[f"ffn_w_in{l}{s2}"] = np.asarray(inputs["ffn_w_in"], np.float32)[l, s2]
            big[f"ffn_w_out{l}{s2}"] = np.asarray(inputs["ffn_w_out"], np.float32)[l, s2]
    big["gmlp_w_in"] = np.asarray(inputs["gmlp_w_in"], np.float32)[0]
    big["gmlp_w_out"] = np.asarray(inputs["gmlp_w_out"], np.float32)[0]
    big["moba_w_qkv"] = np.asarray(inputs["moba_w_qkv"], np.float32)[0]
    big["moba_w_o"] = np.asarray(inputs["moba_w_o"], np.float32)[0]
    in_maps = []
    for i in range(NCORES):
        m = dict(shared)
        m["x"] = np.ascontiguousarray(x[i * TOK:(i + 1) * TOK])
        T, cm, cmneg, rb31 = _moba_consts(inputs["rel_bias"], i)
        m["mb_T"], m["mb_cm"], m["mb_cmneg"], m["mb_rb31"] = T, cm, cmneg, rb31
        m["mb_ind"] = _IND
        for name, w in big.items():
            if names is not None and name not in names:
                continue
            r = w.shape[0] // NCORES
            m[name] = np.ascontiguousarray(w[i * r:(i + 1) * r])
        in_maps.append(m)
    return in_maps


def run(inputs, stop_after=None, trace=False):
    nc, P = build_program(stop_after)
    emit_program(nc, P)
    names = {t.name for t in nc.main_func.allocations if hasattr(t, "name")} if False else None
    in_maps = make_in_maps(inputs, None if stop_after is None or stop_after[0] != "dbg" else ({"mod_w0"} if stop_after[1] > 0 else set()))
    res = run_bass_kernel_spmd(nc, in_maps, core_ids=list(range(NCORES)), trace=trace)
    out = np.concatenate([np.asarray(r["y"]) for r in res.results], axis=0)
    return out.reshape(1, SEQ, D).astype(np.float32), res


def kernel(**inputs):
    out, _ = run(inputs)
    return out
```
